# Optimizing a Trainium2 kernel written in Bass

```python
import math
import jax, jax.numpy as jnp
from jax import lax
import numpy as np

D_MODEL = 2048
BATCH = 4
SEQ = 2048
DEPTH = 2

D_MIX = D_MODEL
D_GROUP = D_MIX // 4
D_FF = 4 * D_MODEL
ROPE_THETA = 10000.0
EPS = 1e-6
Q_BLOCK = 128
NEG_INF = -1e30

S5_CH = D_GROUP
S5_GROUP = 16
S5_G = S5_CH // S5_GROUP
S5_P = 64
S5_DT_MIN = 0.001
S5_DT_MAX = 0.1

RET_HEADS = 4
RET_DK = D_GROUP // RET_HEADS
RET_CHUNK = 128

NSA_HEADS = 4
NSA_DH = D_GROUP // NSA_HEADS
NSA_CMP_LEN = 32
NSA_CMP_STRIDE = 16
NSA_SEL_LEN = 64
NSA_TOPK = 8
NSA_WINDOW = 256
NSA_FORCE_BONUS = 1e4

DIFF_HEADS = 4
DIFF_DV = D_GROUP // DIFF_HEADS
DIFF_DH = DIFF_DV // 2

SPLIT_WIDTHS = (
    S5_CH,
    D_GROUP, D_GROUP, D_GROUP, D_GROUP,
    D_GROUP,
    NSA_DH, NSA_DH, NSA_DH, NSA_DH, NSA_DH, NSA_DH,
    NSA_HEADS * 3,
    D_GROUP, D_GROUP, D_GROUP,
)
N_IN = sum(SPLIT_WIDTHS)

kernel_name = "hybrid_s5_retnet_nsa_diffattn_block"


def _rms(x, gain=None):
    xf = x.astype(jnp.float32)
    y = xf * lax.rsqrt(jnp.mean(xf * xf, axis=-1, keepdims=True) + EPS)
    if gain is not None:
        y = y * gain.astype(jnp.float32)
    return y.astype(x.dtype)


def _rope(x, pos):
    d = x.shape[-1]
    inv = ROPE_THETA ** (-jnp.arange(0, d, 2, dtype=jnp.float32) / d)
    ang = pos.astype(jnp.float32)[..., None] * inv
    cos = jnp.cos(ang)[:, :, None, :]
    sin = jnp.sin(ang)[:, :, None, :]
    xf = x.astype(jnp.float32)
    x1, x2 = xf[..., : d // 2], xf[..., d // 2:]
    return jnp.concatenate([x1 * cos - x2 * sin, x2 * cos + x1 * sin], axis=-1).astype(x.dtype)


def _masked_softmax(s, mask):
    p = jax.nn.softmax(jnp.where(mask, s, NEG_INF), axis=-1)
    return jnp.where(mask, p, 0.0)


def _s5_combine(e1, e2):
    a1r, a1i, b1r, b1i = e1
    a2r, a2i, b2r, b2i = e2
    return (a2r * a1r - a2i * a1i,
            a2r * a1i + a2i * a1r,
            a2r * b1r - a2i * b1i + b2r,
            a2r * b1i + a2i * b1r + b2i)


def _s5_mixer(u, lam_re, lam_im, log_dt, b_re, b_im, c_re, c_im, d_skip, w_glu):
    bsz, seq, _ = u.shape
    f32 = jnp.float32
    ug = u.astype(f32).reshape(bsz, seq, S5_G, S5_GROUP)
    lam_re = lam_re.astype(f32)
    lam_im = lam_im.astype(f32)
    dt = jnp.exp(log_dt.astype(f32))[:, None]
    mag = jnp.exp(lam_re * dt)
    lb_re = mag * jnp.cos(lam_im * dt)
    lb_im = mag * jnp.sin(lam_im * dt)
    den = lam_re * lam_re + lam_im * lam_im
    f_re = ((lb_re - 1.0) * lam_re + lb_im * lam_im) / den
    f_im = (lb_im * lam_re - (lb_re - 1.0) * lam_im) / den
    b_re = b_re.astype(f32)
    b_im = b_im.astype(f32)
    bb_re = f_re[..., None] * b_re - f_im[..., None] * b_im
    bb_im = f_re[..., None] * b_im + f_im[..., None] * b_re
    bu_re = jnp.einsum('blgh,gph->blgp', ug, bb_re)
    bu_im = jnp.einsum('blgh,gph->blgp', ug, bb_im)
    a_re = jnp.broadcast_to(lb_re, bu_re.shape)
    a_im = jnp.broadcast_to(lb_im, bu_im.shape)
    _, _, s_re, s_im = lax.associative_scan(_s5_combine, (a_re, a_im, bu_re, bu_im), axis=1)
    y = (jnp.einsum('gnp,blgp->blgn', c_re.astype(f32), s_re)
         - jnp.einsum('gnp,blgp->blgn', c_im.astype(f32), s_im)
         + d_skip.astype(f32) * ug)
    y = jax.nn.gelu(y.reshape(bsz, seq, S5_CH)).astype(u.dtype)
    return y * jax.nn.sigmoid(y @ w_glu)


def _retention(q, k, v, g, pos):
    bsz, seq, _ = q.shape
    H, dk, C = RET_HEADS, RET_DK, RET_CHUNK
    n_chunk = seq // C
    q = _rope(q.reshape(bsz, seq, H, dk), pos)
    k = _rope(k.reshape(bsz, seq, H, dk), pos) * (dk ** -0.5)
    v = v.reshape(bsz, seq, H, dk)
    log_g = jnp.log(1.0 - 2.0 ** (-5.0 - jnp.arange(H, dtype=jnp.float32)))
    qc = q.reshape(bsz, n_chunk, C, H, dk)
    kc = k.reshape(bsz, n_chunk, C, H, dk)
    vc = v.reshape(bsz, n_chunk, C, H, dk)
    idx = jnp.arange(C, dtype=jnp.float32)
    diff = idx[:, None] - idx[None, :]
    dmat = jnp.where(diff >= 0, jnp.exp(jnp.maximum(diff, 0.0)[None] * log_g[:, None, None]), 0.0)
    inner = jnp.einsum('bnqhd,bnkhd->bnhqk', qc, kc) * dmat[None, None]
    o_inner = jnp.einsum('bnhqk,bnkhe->bnqhe', inner, vc)
    zeta = jnp.exp((C - 1.0 - idx)[None, :] * log_g[:, None])
    kv = jnp.einsum('bnkhd,hk,bnkhe->nbhde', kc, zeta, vc)
    g_chunk = jnp.exp(C * log_g)[None, :, None, None]

    def step(r, kv_i):
        return r * g_chunk + kv_i, r

    _, r_prev = lax.scan(step, jnp.zeros((bsz, H, dk, dk), kv.dtype), kv)
    xi = jnp.exp((idx + 1.0)[None, :] * log_g[:, None])
    o_cross = jnp.einsum('bnqhd,nbhde,hq->bnqhe', qc, r_prev, xi)
    o = (o_inner + o_cross).reshape(bsz, seq, H, dk).astype(jnp.float32)
    mu = jnp.mean(o, axis=-1, keepdims=True)
    var = jnp.mean(jnp.square(o - mu), axis=-1, keepdims=True)
    o = ((o - mu) * lax.rsqrt(var + EPS)).reshape(bsz, seq, H * dk).astype(g.dtype)
    return jax.nn.silu(g) * o


def _nsa(q, kc, vc, ks, vs, kw, vw, gate, pos, pe_k, pe_v, w_cmp_k, w_cmp_v):
    bsz, seq, _ = q.shape
    H, dh, QB = NSA_HEADS, NSA_DH, Q_BLOCK
    scale = dh ** -0.5
    nb = seq // QB
    t = jnp.arange(seq)
    q = _rope(_rms(q.reshape(bsz, seq, H, dh)), pos)

    n_cmp = (seq - NSA_CMP_LEN) // NSA_CMP_STRIDE + 1
    blk_idx = np.arange(n_cmp)[:, None] * NSA_CMP_STRIDE + np.arange(NSA_CMP_LEN)[None, :]
    kc_r = _rope(kc[:, :, None, :], pos)[:, :, 0]
    k_cmp = _rms((kc_r[:, blk_idx] + pe_k).reshape(bsz, n_cmp, NSA_CMP_LEN * dh) @ w_cmp_k)
    v_cmp = (vc[:, blk_idx] + pe_v).reshape(bsz, n_cmp, NSA_CMP_LEN * dh) @ w_cmp_v
    mask_c = jnp.asarray(blk_idx[:, -1])[None, :] <= t[:, None]
    s_c = jnp.einsum('bqhd,bjd->bhqj', q, k_cmp).astype(jnp.float32) * scale
    p_cmp = _masked_softmax(s_c, mask_c[None, None])
    o_cmp = jnp.einsum('bhqj,bjd->bqhd', p_cmp.astype(v_cmp.dtype), v_cmp)

    n_sel = seq // NSA_SEL_LEN
    topk = min(NSA_TOPK, n_sel)
    cmp_start = np.arange(n_cmp) * NSA_CMP_STRIDE
    sel_start = np.arange(n_sel) * NSA_SEL_LEN
    overlap = ((cmp_start[None, :] < sel_start[:, None] + NSA_SEL_LEN)
               & (cmp_start[None, :] + NSA_CMP_LEN > sel_start[:, None])).astype(np.float32)
    imp = jnp.einsum('bhqi,ji->bqj', p_cmp, jnp.asarray(overlap))
    cur = t // NSA_SEL_LEN
    jj = jnp.arange(n_sel)
    valid = jj[None, :] <= cur[:, None]
    forced = (jj[None, :] == 0) | (jj[None, :] == cur[:, None]) | (jj[None, :] == cur[:, None] - 1)
    score = jnp.where(valid, imp + jnp.where(forced, NSA_FORCE_BONUS, 0.0), NEG_INF)
    _, sel = lax.top_k(score, topk)
    ks_blk = _rope(_rms(ks)[:, :, None, :], pos)[:, :, 0].reshape(bsz, n_sel, NSA_SEL_LEN, dh)
    vs_blk = vs.reshape(bsz, n_sel, NSA_SEL_LEN, dh)
    b_idx = jnp.arange(bsz)[:, None, None]
    l_idx = jnp.arange(NSA_SEL_LEN)

    def sel_block(args):
        q_b, sel_b, t_b = args
        kg = ks_blk[b_idx, sel_b]
        vg = vs_blk[b_idx, sel_b]
        s = jnp.einsum('bqhd,bqkld->bhqkl', q_b, kg).astype(jnp.float32) * scale
        kpos = sel_b[..., None] * NSA_SEL_LEN + l_idx
        m = (kpos <= t_b[None, :, None, None]).reshape(bsz, 1, QB, topk * NSA_SEL_LEN)
        p = _masked_softmax(s.reshape(bsz, H, QB, topk * NSA_SEL_LEN), m).reshape(s.shape)
        return jnp.einsum('bhqkl,bqkld->bqhd', p.astype(vg.dtype), vg)

    q_blocks = q.reshape(bsz, nb, QB, H, dh).transpose(1, 0, 2, 3, 4)
    sel_blocks = sel.reshape(bsz, nb, QB, topk).transpose(1, 0, 2, 3)
    o_sel = lax.map(sel_block, (q_blocks, sel_blocks, t.reshape(nb, QB)))
    o_sel = o_sel.transpose(1, 0, 2, 3, 4).reshape(bsz, seq, H, dh)

    n_prev = NSA_WINDOW // QB
    kw_r = _rope(_rms(kw)[:, :, None, :], pos)[:, :, 0]
    kw_pad = jnp.pad(kw_r, ((0, 0), (n_prev * QB, 0), (0, 0))).reshape(bsz, nb + n_prev, QB, dh)
    vw_pad = jnp.pad(vw, ((0, 0), (n_prev * QB, 0), (0, 0))).reshape(bsz, nb + n_prev, QB, dh)
    k_band = jnp.concatenate([kw_pad[:, i:i + nb] for i in range(n_prev + 1)], axis=2)
    v_band = jnp.concatenate([vw_pad[:, i:i + nb] for i in range(n_prev + 1)], axis=2)
    qb = q.reshape(bsz, nb, QB, H, dh)
    s_w = jnp.einsum('bnqhd,bnkd->bnhqk', qb, k_band).astype(jnp.float32) * scale
    qpos = t.reshape(nb, QB)
    kpos = (jnp.arange(nb)[:, None] - n_prev) * QB + jnp.arange((n_prev + 1) * QB)[None, :]
    dist = qpos[:, :, None] - kpos[:, None, :]
    m_w = (dist >= 0) & (dist < NSA_WINDOW) & (kpos[:, None, :] >= 0)
    p_w = _masked_softmax(s_w, m_w[None, :, None])
    o_win = jnp.einsum('bnhqk,bnkd->bnqhd', p_w.astype(v_band.dtype), v_band).reshape(bsz, seq, H, dh)

    gts = jax.nn.sigmoid(gate.reshape(bsz, seq, H, 3))
    o = gts[..., 0:1] * o_cmp + gts[..., 1:2] * o_sel + gts[..., 2:3] * o_win
    return o.reshape(bsz, seq, H * dh)


def _diff_attn(q, k, v, pos, lq1, lk1, lq2, lk2, lam_init):
    bsz, seq, _ = q.shape
    H, dh, dv, QB = DIFF_HEADS, DIFF_DH, DIFF_DV, Q_BLOCK
    nb = seq // QB
    scale = dh ** -0.5
    q = _rope(_rms(q.reshape(bsz, seq, 2 * H, dh)), pos).reshape(bsz, seq, H, 2, dh)
    k = _rope(_rms(k.reshape(bsz, seq, 2 * H, dh)), pos).reshape(bsz, seq, H, 2, dh)
    v = v.reshape(bsz, seq, H, dv)
    lam = (jnp.exp(jnp.sum(lq1.astype(jnp.float32) * lk1.astype(jnp.float32)))
           - jnp.exp(jnp.sum(lq2.astype(jnp.float32) * lk2.astype(jnp.float32))) + lam_init)
    kpos = jnp.arange(seq)

    def block(args):
        q_b, t_b = args
        s = jnp.einsum('bqhcd,bkhcd->bhcqk', q_b, k).astype(jnp.float32) * scale
        p = _masked_softmax(s, kpos[None, :] <= t_b[:, None])
        a = p[:, :, 0] - lam * p[:, :, 1]
        return jnp.einsum('bhqk,bkhe->bqhe', a.astype(v.dtype), v)

    q_blocks = q.reshape(bsz, nb, QB, H, 2, dh).transpose(1, 0, 2, 3, 4, 5)
    o = lax.map(block, (q_blocks, kpos.reshape(nb, QB)))
    o = o.transpose(1, 0, 2, 3, 4).reshape(bsz, seq, H, dv)
    o = _rms(o) * (1.0 - lam_init)
    return o.reshape(bsz, seq, H * dv)


def setup_inputs(seed: int = 0) -> dict:
    key = jax.random.key(seed)
    ks = jax.random.split(key, 32)
    f32 = jnp.float32
    nrm = lambda k, shape, s: jax.random.normal(k, shape, f32) * s
    x = jax.random.normal(ks[0], (BATCH, SEQ, D_MODEL), f32)
    positions = (jnp.arange(SEQ, dtype=jnp.int32)[None, :]
                 + jax.random.randint(ks[1], (BATCH, 1), 0, 1024, dtype=jnp.int32))
    norm1_g = 1.0 + nrm(ks[2], (DEPTH, D_MODEL), 0.02)
    w_in = nrm(ks[3], (DEPTH, D_MODEL, N_IN), D_MODEL ** -0.5)
    s5_lambda_re = -0.5 + nrm(ks[4], (DEPTH, S5_G, S5_P), 0.01)
    s5_lambda_im = math.pi * jnp.arange(S5_P, dtype=f32)[None, None, :] + nrm(ks[5], (DEPTH, S5_G, S5_P), 0.01)
    s5_log_dt = jax.random.uniform(ks[6], (DEPTH, S5_G), f32, math.log(S5_DT_MIN), math.log(S5_DT_MAX))
    s5_b_re = nrm(ks[7], (DEPTH, S5_G, S5_P, S5_GROUP), (2 * S5_GROUP) ** -0.5)
    s5_b_im = nrm(ks[8], (DEPTH, S5_G, S5_P, S5_GROUP), (2 * S5_GROUP) ** -0.5)
    s5_c_re = nrm(ks[9], (DEPTH, S5_G, S5_GROUP, S5_P), (2 * S5_P) ** -0.5)
    s5_c_im = nrm(ks[10], (DEPTH, S5_G, S5_GROUP, S5_P), (2 * S5_P) ** -0.5)
    s5_d = nrm(ks[11], (DEPTH, S5_G, S5_GROUP), 1.0)
    s5_w_glu = nrm(ks[12], (DEPTH, S5_CH, S5_CH), S5_CH ** -0.5)
    nsa_pe_k = nrm(ks[13], (DEPTH, NSA_CMP_LEN, NSA_DH), 0.1)
    nsa_pe_v = nrm(ks[14], (DEPTH, NSA_CMP_LEN, NSA_DH), 0.1)
    nsa_w_cmp_k = nrm(ks[15], (DEPTH, NSA_CMP_LEN * NSA_DH, NSA_DH), (NSA_CMP_LEN * NSA_DH) ** -0.5)
    nsa_w_cmp_v = nrm(ks[16], (DEPTH, NSA_CMP_LEN * NSA_DH, NSA_DH), (NSA_CMP_LEN * NSA_DH) ** -0.5)
    diff_lq1 = nrm(ks[17], (DEPTH, DIFF_DH), 0.1)
    diff_lk1 = nrm(ks[18], (DEPTH, DIFF_DH), 0.1)
    diff_lq2 = nrm(ks[19], (DEPTH, DIFF_DH), 0.1)
    diff_lk2 = nrm(ks[20], (DEPTH, DIFF_DH), 0.1)
    w_out = nrm(ks[21], (DEPTH, D_MIX, D_MODEL), D_MIX ** -0.5)
    norm2_g = 1.0 + nrm(ks[22], (DEPTH, D_MODEL), 0.02)
    mlp_w1 = nrm(ks[23], (DEPTH, D_MODEL, D_FF), D_MODEL ** -0.5)
    mlp_w2 = nrm(ks[24], (DEPTH, D_FF, D_MODEL), D_FF ** -0.5)
    return {"x": x, "positions": positions, "norm1_g": norm1_g, "w_in": w_in,
            "s5_lambda_re": s5_lambda_re, "s5_lambda_im": s5_lambda_im, "s5_log_dt": s5_log_dt,
            "s5_b_re": s5_b_re, "s5_b_im": s5_b_im, "s5_c_re": s5_c_re, "s5_c_im": s5_c_im,
            "s5_d": s5_d, "s5_w_glu": s5_w_glu, "nsa_pe_k": nsa_pe_k, "nsa_pe_v": nsa_pe_v,
            "nsa_w_cmp_k": nsa_w_cmp_k, "nsa_w_cmp_v": nsa_w_cmp_v,
            "diff_lq1": diff_lq1, "diff_lk1": diff_lk1, "diff_lq2": diff_lq2, "diff_lk2": diff_lk2,
            "w_out": w_out, "norm2_g": norm2_g, "mlp_w1": mlp_w1, "mlp_w2": mlp_w2}


def reference(x, positions, norm1_g, w_in, s5_lambda_re, s5_lambda_im, s5_log_dt,
              s5_b_re, s5_b_im, s5_c_re, s5_c_im, s5_d, s5_w_glu, nsa_pe_k, nsa_pe_v,
              nsa_w_cmp_k, nsa_w_cmp_v, diff_lq1, diff_lk1, diff_lq2, diff_lk2,
              w_out, norm2_g, mlp_w1, mlp_w2):
    split_points = []
    acc = 0
    for w in SPLIT_WIDTHS[:-1]:
        acc += w
        split_points.append(acc)
    for layer in range(DEPTH):
        h = _rms(x, norm1_g[layer])
        (u, rq, rk, rv, rg, nq, nkc, nvc, nks, nvs, nkw, nvw, ngate,
         dq, dk, dv) = jnp.split(h @ w_in[layer], split_points, axis=-1)
        y_a = _s5_mixer(u, s5_lambda_re[layer], s5_lambda_im[layer], s5_log_dt[layer],
                        s5_b_re[layer], s5_b_im[layer], s5_c_re[layer], s5_c_im[layer],
                        s5_d[layer], s5_w_glu[layer])
        y_b = _retention(rq, rk, rv, rg, positions)
        y_c = _nsa(nq, nkc, nvc, nks, nvs, nkw, nvw, ngate, positions,
                   nsa_pe_k[layer], nsa_pe_v[layer], nsa_w_cmp_k[layer], nsa_w_cmp_v[layer])
        lam_init = 0.8 - 0.6 * math.exp(-0.3 * layer)
        y_d = _diff_attn(dq, dk, dv, positions, diff_lq1[layer], diff_lk1[layer],
                         diff_lq2[layer], diff_lk2[layer], lam_init)
        mixed = jnp.concatenate([y_a.astype(x.dtype), y_b.astype(x.dtype),
                                 y_c.astype(x.dtype), y_d.astype(x.dtype)], axis=-1)
        x = x + mixed @ w_out[layer]
        h2 = _rms(x, norm2_g[layer])
        x = x + jnp.square(jax.nn.relu(h2 @ mlp_w1[layer])) @ mlp_w2[layer]
    return x
```

```python
import math
import numpy as np
from contextlib import ExitStack
import concourse.bass as bass
import concourse.mybir as mybir
from concourse.bass_utils import run_bass_kernel_spmd

dt = mybir.dt
F32, BF16, F32R, I32 = dt.float32, dt.bfloat16, dt.float32r, dt.int32
AF = mybir.ActivationFunctionType
ALU = mybir.AluOpType
AX = mybir.AxisListType

S = 2048
D = 2048
L = 2
NT = S // 128
N_IN = 5388
D_FF = 8192
EPS = 1e-6
PI = math.pi
S5L = 256

ENGS = ['sp', 'act', 'dve', 'pool', 'pe']
BLK = {'sp': 'sync', 'act': 'scalar', 'dve': 'vector', 'pool': 'gpsimd', 'pe': 'tensor'}
DSIZE = {F32: 4, BF16: 2, I32: 4, F32R: 4}
N_DMA_SEM = 12
SB_BASE = 16384 + 512
SB_END = 224 * 1024

OFF = {}
_o = 0
for _n, _w in [('u', 512), ('rq', 512), ('rk', 512), ('rv', 512), ('rg', 512), ('nq', 512),
               ('nkc', 128), ('nvc', 128), ('nks', 128), ('nvs', 128), ('nkw', 128), ('nvw', 128),
               ('ngate', 12), ('dq', 512), ('dk', 512), ('dv', 512)]:
    OFF[_n] = _o
    _o += _w
assert _o == N_IN


class T:
    __slots__ = ('ap', 'w', 'rs', 'name', 'psum')

    def __init__(self, ap, name=''):
        self.ap = ap
        self.w = None
        self.rs = []
        self.name = name
        self.psum = False

    def __getitem__(self, k):
        return self.ap[k]


class Prog:
    def __init__(self, nc):
        self.nc = nc
        self.ops = {e: [] for e in ENGS}
        self.tiles = []
        self.pending = {e: set() for e in ENGS}
        self.dma_cnt = {}
        self.dma_rr = {e: 0 for e in ENGS}
        self.sb_off = SB_BASE
        self.uid = 0
        self.sb_max = 0
        self.psum = []
        self.ps_rr = 0

    def sb(self, shape, dtype=F32, name='t'):
        per = int(np.prod(shape[1:])) * DSIZE[dtype]
        per = (per + 63) // 64 * 64
        off = self.sb_off
        self.sb_off += per
        self.sb_max = max(self.sb_max, self.sb_off)
        assert self.sb_off <= SB_END, f"SBUF overflow {self.sb_off} at {name}"
        self.uid += 1
        h = self.nc.alloc_sbuf_tensor_at(f"{name}_{self.uid}", list(shape), dtype, offset=off)
        t = T(h.ap(), name)
        self.tiles.append(t)
        return t

    def mark(self):
        return self.sb_off

    def dbg(self, name, t, ap, shape, dtype=F32):
        if not getattr(self, 'debug', False):
            return
        d = self.dram(name, list(shape), dtype, kind="ExternalOutput")
        self.dma('sp', d.ap, ap, reads=[t])

    def release(self, m):
        self.barrier()
        self.sb_off = m

    def track(self, ap, name=''):
        t = T(ap, name)
        self.tiles.append(t)
        return t

    def dram(self, name, shape, dtype=F32, kind="Internal"):
        h = self.nc.dram_tensor(name, list(shape), dtype, kind=kind)
        return self.track(h.ap(), name)

    def init_psum(self):
        for i in range(8):
            h = self.nc.alloc_psum_tensor(f"psb{i}", [128, 512], F32)
            self.psum.append(self.track(h.ap(), f"ps{i}"))
            self.psum[-1].psum = True
        return self.psum

    def ps(self, group=None):
        grp = group if group is not None else list(range(8))
        self.ps_rr += 1
        return self.psum[grp[self.ps_rr % len(grp)]]

    def _deps(self, reads, writes):
        deps = set()
        for t in reads:
            if t.w is not None:
                deps.add(t.w)
            if t.psum:
                deps.update(t.rs)
        for t in writes:
            if t.w is not None:
                deps.add(t.w)
            deps.update(t.rs)
        return deps

    def op(self, eng, fn, reads=(), writes=()):
        idx = len(self.ops[eng])
        tok = ('c', eng, idx)
        deps = self._deps(reads, writes)
        deps |= self.pending[eng]
        self.pending[eng] = set()
        self.ops[eng].append(dict(fn=fn, deps=deps, kind='c', marked=False))
        for t in reads:
            t.rs = [r for r in t.rs if not (r[0] == 'c' and r[1] == eng)]
            t.rs.append(tok)
        for t in writes:
            t.w = tok
            t.rs = []
        return tok

    def dma(self, q, out_ap, in_ap, reads=(), writes=(), **kw):
        i = self.dma_rr[q]
        self.dma_rr[q] = (i + 1) % N_DMA_SEM
        key = (q, i)
        prev = self.dma_cnt.get(key, 0)
        cnt = prev + 16
        self.dma_cnt[key] = cnt
        tok = ('d', key, cnt)
        deps = self._deps(reads, writes)
        deps |= self.pending[q]
        self.pending[q] = set()
        if prev > 0:
            deps.add(('d', key, prev))
        self.ops[q].append(dict(fn=lambda e: e.dma_start(out=out_ap, in_=in_ap, **kw), deps=deps,
                                kind='d', key=key, marked=True))
        for t in reads:
            t.rs.append(tok)
        for t in writes:
            t.w = tok
            t.rs = []
        return tok

    def barrier(self):
        toks = set()
        for e in ENGS:
            for j in range(len(self.ops[e]) - 1, -1, -1):
                if self.ops[e][j]['kind'] == 'c':
                    toks.add(('c', e, j))
                    break
        for key, cnt in self.dma_cnt.items():
            toks.add(('d', key, cnt))
        for e in ENGS:
            self.pending[e] |= toks
        for t in self.tiles:
            t.w = None
            t.rs = []

    def finalize(self):
        nc = self.nc
        def skip(e, idx, d):
            return d[0] == 'c' and d[1] == e and (e == 'pe' or e == 'sp' or idx - d[2] > 12)
        self._skip = skip
        for e in ENGS:
            for idx, o in enumerate(self.ops[e]):
                for d in o['deps']:
                    if d[0] == 'c' and not skip(e, idx, d):
                        self.ops[d[1]][d[2]]['marked'] = True
            for d in self.pending[e]:
                if d[0] == 'c' and not skip(e, 10 ** 9, d):
                    self.ops[d[1]][d[2]]['marked'] = True
        for e in ENGS:
            c = 0
            for o in self.ops[e]:
                if o['kind'] == 'c' and o['marked']:
                    c += 1
                    o['inc'] = c
        self.nwait = 0
        with ExitStack() as st:
            esem = {e: st.enter_context(nc.semaphore(f"s_{e}")) for e in ENGS}
            dsem = {}
            for key in self.dma_cnt:
                dsem[key] = st.enter_context(nc.semaphore(f"d_{key[0]}_{key[1]}"))
            block = st.enter_context(nc.Block())

            def resolve(d):
                if d[0] == 'c':
                    return esem[d[1]], self.ops[d[1]][d[2]]['inc'], d[1]
                return dsem[d[1]], d[2], d[1]

            def replay(e, eng):
                seen = {}
                ops = self.ops[e]

                def do_waits(deps, idx):
                    need = {}
                    for d in deps:
                        if skip(e, idx, d):
                            continue
                        sem, val, key = resolve(d)
                        k = (d[0], key)
                        if val > need.get(k, (None, 0))[1]:
                            need[k] = (sem, val)
                    for k, (sem, val) in need.items():
                        if seen.get(k, 0) >= val:
                            continue
                        seen[k] = val
                        eng.wait_ge(sem, val)
                        self.nwait += 1

                for idx, o in enumerate(ops):
                    do_waits(o['deps'], idx)
                    ins = o['fn'](eng)
                    if o['kind'] == 'd':
                        ins.then_inc(dsem[o['key']], 16)
                    elif o['marked']:
                        ins.then_inc(esem[e], 1)
                do_waits(self.pending[e], 10 ** 9)

            for e in ENGS:
                getattr(block, BLK[e])(lambda eng, e=e: replay(e, eng))
        return nc


def ACT(P, out, in_, func, r, w, **kw):
    return P.op('act', lambda e: e.activation(out=out, in_=in_, func=func, **kw), r, w)


def TT(P, eng, out, in0, in1, op, r, w):
    return P.op(eng, lambda e: e.tensor_tensor(out=out, in0=in0, in1=in1, op=op), r, w)


def TS(P, eng, out, in0, s1, s2, op0, op1, r, w, **kw):
    if op1 is None:
        return P.op(eng, lambda e: e.tensor_scalar(out=out, in0=in0, scalar1=s1, scalar2=None, op0=op0, **kw), r, w)
    return P.op(eng, lambda e: e.tensor_scalar(out=out, in0=in0, scalar1=s1, scalar2=s2, op0=op0, op1=op1, **kw), r, w)


def STT(P, out, in0, scalar, in1, op0, op1, r, w):
    return P.op('dve', lambda e: e.scalar_tensor_tensor(out=out, in0=in0, scalar=scalar, in1=in1, op0=op0, op1=op1), r, w)


def MM(P, out, lhsT, rhs, start, stop, r, w, **kw):
    return P.op('pe', lambda e: e.matmul(out, lhsT=lhsT, rhs=rhs, start=start, stop=stop, **kw), r, w)


def TR(P, out, in_, ident, r, w):
    return P.op('pe', lambda e: e.transpose(out=out, in_=in_, identity=ident), r, w)


def CP(P, eng, out, in_, r, w):
    if eng == 'act':
        return P.op('act', lambda e: e.activation(out=out, in_=in_, func=AF.Copy), r, w)
    return P.op(eng, lambda e: e.tensor_copy(out=out, in_=in_), r, w)


def RECIP(P, out, in_, r, w):
    return P.op('dve', lambda e: e.reciprocal(out=out, in_=in_), r, w)


class Ctx:
    pass


def norm_transpose_tile(P, C, xt, h, st, gbc, hT_t, dst_fn, evac_rr):
    ACT(P, h[:], xt[:], AF.Square, [xt], [h, st], accum_out=st[:, 0:1])
    ACT(P, st[:, 1:2], st[:, 0:1], AF.Sqrt, [st, C.epsT], [st], scale=1.0 / D, bias=C.epsT[:, 0:1])
    RECIP(P, st[:, 2:3], st[:, 1:2], [st], [st])
    STT(P, h[:], xt[:], st[:, 2:3], gbc[:], ALU.mult, ALU.mult, [xt, st, gbc, h], [h])
    for kq in range(4):
        bank = P.ps()
        for j in range(4):
            k = kq * 4 + j
            TR(P, bank[:, j * 128:(j + 1) * 128], h[:, k * 128:(k + 1) * 128], C.ident[:], [h, C.ident], [bank])
        eng = 'act' if (evac_rr + kq) % 2 == 0 else 'dve'
        CP(P, eng, dst_fn(kq), bank[:].rearrange("p (a b) -> p a b", a=4), [bank], [hT_t])


def phase_in_proj(P, C, l, xin):
    m0 = P.mark()
    gbc = P.sb([128, D], F32, 'gbc')
    P.dma('act', gbc[:], C.norm1_g.ap[l:l + 1, :].broadcast_to([128, D]), writes=[gbc])
    xts = [P.sb([128, D], F32, 'xt') for _ in range(2)]
    hs = [P.sb([128, D], F32, 'h') for _ in range(2)]
    sts = [P.sb([128, 4], F32, 'st') for _ in range(2)]
    hT = [P.sb([128, 16, 128], F32R, 'hT') for _ in range(8)]
    wb = [P.sb([128, 16, 512], F32R, 'wb') for _ in range(2)]
    stg = [P.sb([128, 512], F32, 'stg') for _ in range(4)]
    nblk = (N_IN + 511) // 512
    wi = 0
    si = 0
    for th in range(2):
        for i in range(8):
            tok0 = th * 1024 + i * 128
            xt, h, st = xts[i % 2], hs[i % 2], sts[i % 2]
            P.dma('sp', xt[:], xin[tok0:tok0 + 128, :], writes=[xt])
            norm_transpose_tile(P, C, xt, h, st, gbc, hT[i], (lambda kq, t=hT[i]: t[:, kq * 4:(kq + 1) * 4, :]), i)
        for cb in range(nblk):
            c0 = cb * 512
            nc_ = min(512, N_IN - c0)
            w = wb[wi % 2]
            wi += 1
            P.dma('pool', w[:, :, :nc_], C.w_in.ap[l, :, c0:c0 + nc_].rearrange("(k p) c -> p k c", p=128), writes=[w])
            for i in range(8):
                tok0 = th * 1024 + i * 128
                bank = P.ps()
                for k in range(16):
                    MM(P, bank[:, :nc_], hT[i][:, k, :], w[:, k, :nc_], k == 0, k == 15, [hT[i], w], [bank])
                sg = stg[si % 4]
                CP(P, 'act' if si % 2 == 0 else 'dve', sg[:, :nc_], bank[:, :nc_], [bank], [sg])
                P.dma('sp' if si % 2 == 0 else 'act', C.proj.ap[tok0:tok0 + 128, c0:c0 + nc_], sg[:, :nc_], reads=[sg])
                si += 1
    P.barrier()
    P.release(m0)


def phase_out_proj(P, C, l, xin, xout):
    m0 = P.mark()
    mT = [P.sb([128, 16, 128], F32R, 'mT') for _ in range(8)]
    wb = [P.sb([128, 16, 512], F32R, 'wb') for _ in range(2)]
    xb = [P.sb([128, 512], F32, 'xb') for _ in range(4)]
    wi = 0
    si = 0
    for th in range(2):
        for i in range(8):
            tok0 = th * 1024 + i * 128
            P.dma('pool', mT[i][:], C.mixedT.ap[:, tok0:tok0 + 128].rearrange("(k p) t -> p k t", p=128), writes=[mT[i]])
        for cb in range(4):
            c0 = cb * 512
            w = wb[wi % 2]
            wi += 1
            P.dma('pool', w[:], C.w_out.ap[l, :, c0:c0 + 512].rearrange("(k p) c -> p k c", p=128), writes=[w])
            for i in range(8):
                tok0 = th * 1024 + i * 128
                x_ = xb[si % 4]
                P.dma('sp', x_[:], xin[tok0:tok0 + 128, c0:c0 + 512], writes=[x_])
                bank = P.ps()
                for k in range(16):
                    MM(P, bank[:], mT[i][:, k, :], w[:, k, :], k == 0, k == 15, [mT[i], w], [bank])
                TT(P, 'dve', x_[:], x_[:], bank[:], ALU.add, [x_, bank], [x_])
                P.dma('act', xout[tok0:tok0 + 128, c0:c0 + 512], x_[:], reads=[x_])
                si += 1
    P.barrier()
    P.release(m0)


def phase_mlp(P, C, l, xin, xout):
    m0 = P.mark()
    gbc = P.sb([128, D], F32, 'gbc')
    P.dma('act', gbc[:], C.norm2_g.ap[l:l + 1, :].broadcast_to([128, D]), writes=[gbc])
    xacc = [P.sb([128, D], F32, 'xacc') for _ in range(4)]
    h = P.sb([128, D], F32, 'h')
    st = P.sb([128, 4], F32, 'st')
    h2T = P.sb([128, 16, 512], F32R, 'h2T')
    aT = [P.sb([128, 512], F32R, 'aT') for _ in range(16)]
    wb = [P.sb([128, 16, 512], F32R, 'wb') for _ in range(2)]
    rl = [P.sb([128, 512], F32, 'rl') for _ in range(2)]
    wi = 0
    ri = 0
    for tt4 in range(4):
        t0 = tt4 * 512
        for j in range(4):
            P.dma('sp', xacc[j][:], xin[t0 + j * 128:t0 + (j + 1) * 128, :], writes=[xacc[j]])
            norm_transpose_tile(P, C, xacc[j], h, st, gbc, h2T,
                                (lambda kq, j=j: h2T[:, kq * 4:(kq + 1) * 4, j * 128:(j + 1) * 128]), j)
        for q in range(4):
            for cbw in range(4):
                c0 = q * 2048 + cbw * 512
                w = wb[wi % 2]
                wi += 1
                P.dma('pool', w[:], C.mlp_w1.ap[l, :, c0:c0 + 512].rearrange("(k p) c -> p k c", p=128), writes=[w])
                for c in range(4):
                    bank = P.ps()
                    a = aT[cbw * 4 + c]
                    for k in range(16):
                        MM(P, bank[:], w[:, k, c * 128:(c + 1) * 128], h2T[:, k, :], k == 0, k == 15, [w, h2T], [bank])
                    r_ = rl[ri % 2]
                    ri += 1
                    ACT(P, r_[:], bank[:], AF.Relu, [bank], [r_])
                    TT(P, 'dve', a[:], r_[:], r_[:], ALU.mult, [r_], [a])
            for cb in range(4):
                c0 = cb * 512
                w = wb[wi % 2]
                wi += 1
                P.dma('pool', w[:], C.mlp_w2.ap[l, q * 2048:(q + 1) * 2048, c0:c0 + 512].rearrange("(k p) c -> p k c", p=128), writes=[w])
                for j in range(4):
                    bank = P.ps()
                    for k in range(16):
                        MM(P, bank[:], aT[k][:, j * 128:(j + 1) * 128], w[:, k, :], k == 0, k == 15, [aT[k], w], [bank])
                    TT(P, 'dve', xacc[j][:, c0:c0 + 512], xacc[j][:, c0:c0 + 512], bank[:], ALU.add, [xacc[j], bank], [xacc[j]])
        for j in range(4):
            P.dma('act', xout[t0 + j * 128:t0 + (j + 1) * 128, :], xacc[j][:], reads=[xacc[j]])
    P.barrier()
    P.release(m0)


WEIGHT_SPECS = [
    ('norm1_g', [L, D], F32), ('w_in', [L, D, N_IN], F32R),
    ('s5_lambda_re', [L, 32, 64], F32), ('s5_lambda_im', [L, 32, 64], F32), ('s5_log_dt', [L, 32], F32),
    ('s5_b_re', [L, 32, 64, 16], F32), ('s5_b_im', [L, 32, 64, 16], F32),
    ('s5_c_re', [L, 32, 16, 64], F32), ('s5_c_im', [L, 32, 16, 64], F32),
    ('s5_d', [L, 32, 16], F32), ('s5_w_glu', [L, 512, 512], F32R),
    ('nsa_pe_k', [L, 32, 128], F32), ('nsa_pe_v', [L, 32, 128], F32),
    ('nsa_w_cmp_k', [L, 4096, 128], F32), ('nsa_w_cmp_v', [L, 4096, 128], F32),
    ('diff_lq1', [L, 64], F32), ('diff_lk1', [L, 64], F32), ('diff_lq2', [L, 64], F32), ('diff_lk2', [L, 64], F32),
    ('w_out', [L, D, D], F32R), ('norm2_g', [L, D], F32),
    ('mlp_w1', [L, D, D_FF], F32R), ('mlp_w2', [L, D_FF, D], F32R),
]


def host_consts():
    c = {}
    c['c_ident'] = np.eye(128, dtype=np.float32)
    kk = np.arange(128)
    c['c_tri'] = (kk[:, None] <= kk[None, :]).astype(np.float32)
    c['c_trigt'] = (kk[:, None] > kk[None, :]).astype(np.float32)
    inv128 = (10000.0 ** (-np.arange(0, 128, 2, dtype=np.float32) / np.float32(128))).astype(np.float32)
    inv64 = (10000.0 ** (-np.arange(0, 64, 2, dtype=np.float32) / np.float32(64))).astype(np.float32)
    gam = 1.0 - 2.0 ** (-5.0 - np.arange(4, dtype=np.float64))
    sc = 128.0 ** -0.5
    dm = np.zeros((128, 4, 128), np.float64)
    for h in range(4):
        df = kk[None, :] - kk[:, None]
        dm[:, h, :] = np.where(df >= 0, gam[h] ** np.maximum(df, 0), 0.0) * sc
    c['c_ret_dm'] = dm.reshape(128, 512).astype(np.float32)
    c['c_ret_zeta'] = (gam[None, :] ** (127.0 - kk[:, None]) * sc).astype(np.float32)
    xi = gam[:, None] ** (kk[None, :] + 1.0)
    c['c_ret_xi'] = np.tile(xi.reshape(1, 512), (128, 1)).astype(np.float32)
    jp = np.arange(-126, 130)
    c['c_cmpbase'] = ((16 * jp[:, None] + 31) <= kk[None, :]).astype(np.float32)
    n_cmp = 127
    cmp_start = np.arange(n_cmp) * 16
    sel_start = np.arange(32) * 64
    ovl = ((cmp_start[None, :] < sel_start[:, None] + 64) & (cmp_start[None, :] + 32 > sel_start[:, None])).astype(np.float32)
    oz = np.zeros((128, 34), np.float32)
    oz[:127, 0] = 1.0
    oz[:127, 1:33] = ovl.T
    c['c_ovl'] = oz
    t = np.arange(S)
    cur = t // 64
    jj = np.arange(32)
    valid = (jj[None, :] <= cur[:, None])
    forced = (jj[None, :] == 0) | (jj[None, :] == cur[:, None]) | (jj[None, :] == cur[:, None] - 1)
    c['c_selvalid'] = valid.astype(np.float32)
    c['c_selcb'] = np.where(valid, np.where(forced, 1e4, 0.0), -1e30).astype(np.float32)
    ee = np.zeros((32, NT, 128), np.float32)
    for kb in range(NT):
        for k in range(128):
            ee[2 * kb + k // 64, kb, k] = 1.0
    c['c_eexp'] = ee.reshape(32, NT * 128)
    c['c_iota'] = np.tile(np.arange(S5L, dtype=np.float32)[None, :], (128, 1))
    c['c_inv128'] = np.tile(inv128[None, :], (128, 1)).astype(np.float32)
    c['c_inv64'] = np.tile(inv64[None, :], (128, 1)).astype(np.float32)
    return c


def setup_common(P, C, dbg, nlw=L):
    def kind_of(name, default="Internal"):
        return dbg.get(name, default)
    C.x = P.dram("x", [S, D], F32, kind="ExternalInput")
    C.pos = P.dram("pos", [S], I32, kind="ExternalInput")
    for name, shape, dtp in WEIGHT_SPECS:
        setattr(C, name, P.dram(name, [nlw] + list(shape[1:]), dtp, kind="ExternalInput"))
    for name, arr in host_consts().items():
        setattr(C, name, P.dram(name, list(arr.shape), F32, kind="ExternalInput"))
    C.out = P.dram("out", [S, D], F32, kind="ExternalOutput")
    C.proj = P.dram("proj", [S, N_IN], F32, kind=kind_of("proj"))
    C.mixedT = P.dram("mixedT", [D, S], F32R, kind=kind_of("mixedT"))
    C.xa = P.dram("xa", [S, D], F32, kind=kind_of("xa"))
    C.xb = P.dram("xb", [S, D], F32, kind=kind_of("xb"))
    P.init_psum()
    C.ident = P.sb([128, 128], F32, 'ident')
    P.dma('sp', C.ident[:], C.c_ident.ap[:, :], writes=[C.ident])
    C.epsT = P.sb([128, 1], F32, 'epsT')
    P.op('dve', lambda e: e.memset(C.epsT[:], EPS), [], [C.epsT])
    P.barrier()


def setup_rope(P, C):
    C.cos128 = P.sb([128, NT, 64], F32, 'cos128')
    C.sin128 = P.sb([128, NT, 64], F32, 'sin128')
    C.cos64 = P.sb([128, NT, 32], F32, 'cos64')
    C.sin64 = P.sb([128, NT, 32], F32, 'sin64')
    m0 = P.mark()
    posi = P.sb([128, NT], I32, 'posi')
    posf = P.sb([128, NT], F32, 'posf')
    P.dma('sp', posi[:], C.pos.ap.rearrange("(i p) -> p i", p=128), writes=[posi], allow_slow_non_contiguous=True)
    CP(P, 'dve', posf[:], posi[:], [posi], [posf])
    for (half, cinv, cosT, sinT) in ((64, C.c_inv128, C.cos128, C.sin128), (32, C.c_inv64, C.cos64, C.sin64)):
        inv = P.sb([128, half], F32, 'inv')
        P.dma('sp', inv[:], cinv.ap[:, :], writes=[inv])
        ang = P.sb([128, NT, half], F32, 'ang')
        tq = P.sb([128, NT, half], F32, 'tq')
        ti = P.sb([128, NT, half], I32, 'ti')
        TT(P, 'dve', ang[:], posf[:].unsqueeze(2).broadcast_to([128, NT, half]),
           inv[:].unsqueeze(1).broadcast_to([128, NT, half]), ALU.mult, [posf, inv], [ang])
        range_reduce_sincos(P, ang, tq, ti, sinT, cosT, [128, NT * half])
    P.barrier()
    P.release(m0)


def range_reduce_sincos(P, ang, tq, ti, sinT, cosT, shape2):
    def f(t):
        a = t[:]
        if len(a.shape) == 3:
            a = a.rearrange("p a b -> p (a b)")
        return a
    A, Q, I_ = f(ang), f(tq), f(ti)
    TS(P, 'dve', Q, A, 1.0 / (2 * PI), None, ALU.mult, None, [ang], [tq])
    CP(P, 'dve', I_, Q, [tq], [ti])
    CP(P, 'dve', Q, I_, [ti], [tq])
    STT(P, A, Q, -2 * PI, A, ALU.mult, ALU.add, [tq, ang], [ang])
    def wrap(X, xt, up=True, down=True):
        if up:
            TS(P, 'dve', I_.bitcast(F32), X, PI, -2 * PI, ALU.is_gt, ALU.mult, [xt], [ti])
            TT(P, 'dve', X, X, I_.bitcast(F32), ALU.add, [xt, ti], [xt])
        if down:
            TS(P, 'dve', I_.bitcast(F32), X, -PI, 2 * PI, ALU.is_lt, ALU.mult, [xt], [ti])
            TT(P, 'dve', X, X, I_.bitcast(F32), ALU.add, [xt, ti], [xt])
    wrap(A, ang)
    TS(P, 'dve', Q, A, PI / 2, None, ALU.add, None, [ang], [tq])
    wrap(Q, tq, down=False)
    TS(P, 'dve', A, A, PI, -PI, ALU.min, ALU.max, [ang], [ang])
    TS(P, 'dve', Q, Q, PI, -PI, ALU.min, ALU.max, [tq], [tq])
    ACT(P, f(sinT), A, AF.Sin, [ang], [sinT])
    ACT(P, f(cosT), Q, AF.Sin, [tq], [cosT])


def rope_tm(P, eng, dst, src, cosT, sinT, i, H, half, t1, t2, r, w):
    cb = cosT[:, i, :].unsqueeze(1).broadcast_to([128, H, half])
    sb_ = sinT[:, i, :].unsqueeze(1).broadcast_to([128, H, half])
    x1, x2 = src[:, :, :half], src[:, :, half:]
    d1, d2 = dst[:, :, :half], dst[:, :, half:]
    TT(P, eng, t1[:], x1, cb, ALU.mult, r + [cosT], [t1])
    TT(P, eng, t2[:], x2, sb_, ALU.mult, r + [sinT], [t2])
    TT(P, eng, d1, t1[:], t2[:], ALU.subtract, [t1, t2], w)
    TT(P, eng, t1[:], x2, cb, ALU.mult, r + [cosT], [t1])
    TT(P, eng, t2[:], x1, sb_, ALU.mult, r + [sinT], [t2])
    TT(P, eng, d2, t1[:], t2[:], ALU.add, [t1, t2], w)


def rms_scale_tm(P, C, src, H, dh, sq, st, scale, r):
    TT(P, 'pool', sq[:], src, src, ALU.mult, r, [sq])
    P.op('dve', lambda e: e.tensor_reduce(out=st[:, H:2 * H], in_=sq[:], axis=AX.X, op=ALU.add), [sq], [st])
    ACT(P, st[:, H:2 * H], st[:, H:2 * H], AF.Sqrt, [st, C.epsT], [st], scale=1.0 / dh, bias=C.epsT[:, 0:1])
    RECIP(P, st[:, 0:H], st[:, H:2 * H], [st], [st])
    if scale != 1.0:
        TS(P, 'dve', st[:, 0:H], st[:, 0:H], float(scale), None, ALU.mult, None, [st], [st])


def phase_diff(P, C, l):
    m0 = P.mark()
    lam_init = 0.8 - 0.6 * math.exp(-0.3 * (l + getattr(C, 'lbase', 0)))
    qT = P.sb([128, 4, S], BF16, 'qT')
    kT = P.sb([128, 4, S], BF16, 'kT')
    V1 = [P.sb([128, 4, 130], BF16, 'V1') for _ in range(NT)]
    tri = P.sb([128, 128], BF16, 'tri')
    trif = P.sb([128, 128], F32, 'trif')
    P.dma('sp', trif[:], C.c_tri.ap[:, :], writes=[trif])
    CP(P, 'dve', tri[:], trif[:], [trif], [tri])
    identb = P.sb([128, 128], BF16, 'identb')
    CP(P, 'dve', identb[:], C.ident[:], [C.ident], [identb])
    lam = P.sb([128, 8], F32, 'lam')
    lqk = P.sb([128, 4, 64], F32, 'lqk')
    for j, nm in enumerate(['diff_lq1', 'diff_lk1', 'diff_lq2', 'diff_lk2']):
        P.dma('sp', lqk[:, j, :], getattr(C, nm).ap[l:l + 1, :].broadcast_to([128, 64]), writes=[lqk])
    lp = P.sb([128, 2, 64], F32, 'lp')
    TT(P, 'dve', lp[:, 0, :], lqk[:, 0, :], lqk[:, 1, :], ALU.mult, [lqk], [lp])
    TT(P, 'dve', lp[:, 1, :], lqk[:, 2, :], lqk[:, 3, :], ALU.mult, [lqk], [lp])
    P.op('dve', lambda e: e.tensor_reduce(out=lam[:, 0:2], in_=lp[:], axis=AX.X, op=ALU.add), [lp], [lam])
    ACT(P, lam[:, 2:4], lam[:, 0:2], AF.Exp, [lam], [lam])
    TT(P, 'dve', lam[:, 4:5], lam[:, 3:4], lam[:, 2:3], ALU.subtract, [lam], [lam])
    TS(P, 'dve', lam[:, 5:6], lam[:, 4:5], -lam_init, None, ALU.add, None, [lam], [lam])
    neglam = lam[:, 5:6]
    m1 = P.mark()
    raws = [P.sb([128, 1536], F32, 'raw') for _ in range(2)]
    sqs = [P.sb([128, 8, 64], F32, 'sq') for _ in range(2)]
    sts = [P.sb([128, 16], F32, 'st') for _ in range(2)]
    qns = [P.sb([128, 8, 64], F32, 'qn') for _ in range(2)]
    t1s = [P.sb([128, 8, 32], F32, 't1') for _ in range(2)]
    t2s = [P.sb([128, 8, 32], F32, 't2') for _ in range(2)]
    qr = [P.sb([128, 8, 64], BF16, 'qr') for _ in range(2)]
    c0 = OFF['dq']
    for i in range(NT):
        raw = raws[i % 2]
        P.dma('sp', raw[:], C.proj.ap[i * 128:(i + 1) * 128, c0:c0 + 1536], writes=[raw])
        for which, dstT, scale in ((0, qT, 64 ** -0.5), (1, kT, 1.0)):
            src = raw[:, which * 512:(which + 1) * 512].rearrange("p (h d) -> p h d", h=8)
            sq, st, qn, t1, t2 = sqs[which], sts[which], qns[which], t1s[which], t2s[which]
            rms_scale_tm(P, C, src, 8, 64, sq, st, scale, [raw])
            TT(P, 'dve', qn[:], src, st[:, 0:8].unsqueeze(2).broadcast_to([128, 8, 64]), ALU.mult, [raw, st], [qn])
            q_ = qr[which]
            rope_tm(P, 'dve' if which == 0 else 'pool', q_[:], qn[:], C.cos64, C.sin64, i, 8, 32, t1, t2, [qn], [q_])
            bank = P.ps([6, 7])
            bb = bank[:].bitcast(BF16)
            for h in range(4):
                TR(P, bb[:, h * 128:(h + 1) * 128], q_[:, 2 * h:2 * h + 2, :].rearrange("p a b -> p (a b)"), identb[:], [q_, identb], [bank])
            CP(P, 'act', dstT[:, :, i * 128:(i + 1) * 128], bb[:, 0:512].rearrange("p (h t) -> p h t", h=4), [bank], [dstT])
        v1 = V1[i]
        CP(P, 'act', v1[:, :, 0:128], raw[:, 1024:1536].rearrange("p (h d) -> p h d", h=4), [raw], [v1])
        P.op('pool', lambda e, v1=v1: e.memset(v1[:, :, 128:129], 1.0), [], [v1])
    P.release(m1)
    PT = [P.sb([128, 512], BF16, 'PT') for _ in range(5)]
    o1 = [P.sb([128, 128], F32, 'o1') for _ in range(4)]
    dd = [P.sb([128, 128], F32, 'dd') for _ in range(2)]
    junk = P.sb([128, 128], F32, 'junk')
    es = [P.sb([128, 8], F32, 'es') for _ in range(2)]
    ostg = [P.sb([128, 128], F32R, 'ostg') for _ in range(2)]
    pti = 0
    ei = 0
    for h in range(4):
        for Q in range(4):
            for c in range(2):
                O = [P.psum[2 + j] for j in range(4)]
                nkb = 4 * Q + 4
                for kb in range(nkb):
                    jmin = max(0, kb - 4 * Q)
                    q0 = jmin * 128
                    sbk = P.ps([0, 1, 6])
                    MM(P, sbk[:, q0:512], kT[c * 64:(c + 1) * 64, h, kb * 128:(kb + 1) * 128],
                       qT[c * 64:(c + 1) * 64, h, Q * 512 + q0:(Q + 1) * 512], True, True, [kT, qT], [sbk])
                    pt = PT[pti % 5]
                    pti += 1
                    ACT(P, pt[:, q0:512], sbk[:, q0:512], AF.Exp, [sbk], [pt])
                    if kb >= 4 * Q:
                        TT(P, 'dve', pt[:, q0:q0 + 128], pt[:, q0:q0 + 128], tri[:], ALU.mult, [pt, tri], [pt])
                    for j in range(jmin, 4):
                        MM(P, O[j][:, 0:129], pt[:, j * 128:(j + 1) * 128], V1[kb][:, h, 0:129],
                           kb == 0, kb == 4 * Q + j, [pt, V1[kb]], [O[j]])
                for j in range(4):
                    e_ = es[ei % 2]
                    ei += 1
                    RECIP(P, e_[:, 0:1], O[j][:, 128:129], [O[j]], [e_])
                    if c == 0:
                        ACT(P, o1[j][:], O[j][:, 0:128], AF.Copy, [O[j], e_], [o1[j]], scale=e_[:, 0:1])
                    else:
                        d_ = dd[j % 2]
                        TT(P, 'dve', e_[:, 1:2], e_[:, 0:1], neglam, ALU.mult, [e_, lam], [e_])
                        STT(P, d_[:], O[j][:, 0:128], e_[:, 1:2], o1[j][:], ALU.mult, ALU.add, [O[j], e_, o1[j]], [d_])
                        ACT(P, junk[:], d_[:], AF.Square, [d_], [junk, e_], accum_out=e_[:, 2:3])
                        ACT(P, e_[:, 3:4], e_[:, 2:3], AF.Sqrt, [e_, C.epsT], [e_], scale=1.0 / 128, bias=C.epsT[:, 0:1])
                        RECIP(P, e_[:, 4:5], e_[:, 3:4], [e_], [e_])
                        TS(P, 'dve', d_[:], d_[:], e_[:, 4:5], 1.0 - lam_init, ALU.mult, ALU.mult, [d_, e_], [d_])
                        bank = P.ps([7])
                        TR(P, bank[:, 0:128], d_[:], C.ident[:], [d_, C.ident], [bank])
                        og = ostg[j % 2]
                        CP(P, 'act', og[:], bank[:, 0:128], [bank], [og])
                        tok0 = Q * 512 + j * 128
                        P.dma('pool', C.mixedT.ap[1536 + h * 128:1536 + (h + 1) * 128, tok0:tok0 + 128], og[:], reads=[og])
    P.barrier()
    P.release(m0)


def phase_ret(P, C, l, banks=None):
    bk = banks if banks is not None else [0, 2, 4, 6]
    gam = [1.0 - 2.0 ** (-5.0 - h) for h in range(4)]
    g128 = [g ** 128 for g in gam]
    identb = P.sb([128, 128], BF16, 'identb')
    CP(P, 'dve', identb[:], C.ident[:], [C.ident], [identb])
    dm = P.sb([128, 4, 128], F32, 'dm')
    P.dma('sp', dm[:].rearrange("p a b -> p (a b)"), C.c_ret_dm.ap[:, :], writes=[dm])
    zeta = P.sb([128, 4], F32, 'zeta')
    P.dma('sp', zeta[:], C.c_ret_zeta.ap[:, :], writes=[zeta])
    xi = P.sb([128, 4, 128], F32, 'xi')
    P.dma('sp', xi[:].rearrange("p a b -> p (a b)"), C.c_ret_xi.ap[:, :], writes=[xi])
    R = P.sb([128, 4, 128], F32, 'R')
    Rb = P.sb([128, 4, 128], BF16, 'Rb')
    raws = [P.sb([128, 2048], F32, 'raw') for _ in range(1)]
    t1 = P.sb([128, 4, 64], F32, 't1')
    t2 = P.sb([128, 4, 64], F32, 't2')
    t3 = P.sb([128, 4, 64], F32, 't3')
    t4 = P.sb([128, 4, 64], F32, 't4')
    qr = P.sb([128, 4, 128], BF16, 'qr')
    kr = P.sb([128, 4, 128], BF16, 'kr')
    kz = P.sb([128, 4, 128], BF16, 'kz')
    vb = P.sb([128, 4, 128], BF16, 'vb')
    qkT = P.sb([128, 8, 128], BF16, 'qkT')
    qxT = P.sb([128, 4, 128], BF16, 'qxT')
    inT = P.sb([128, 4, 128], BF16, 'inT')
    oc = P.sb([128, 4, 128], F32, 'oc')
    sq = P.sb([128, 4, 128], F32, 'sq')
    sg = P.sb([128, 4, 128], F32, 'sg')
    st = P.sb([128, 16], F32, 'st')
    ystg = [P.sb([128, 4, 128], F32R, 'ystg') for _ in range(1)]
    c0 = OFF['rq']
    yield 'main'
    for i in range(NT):
        raw = raws[0]
        P.dma('sp', raw[:], C.proj.ap[i * 128:(i + 1) * 128, c0:c0 + 2048], writes=[raw])
        qs = raw[:, 0:512].rearrange("p (h d) -> p h d", h=4)
        ks = raw[:, 512:1024].rearrange("p (h d) -> p h d", h=4)
        vs = raw[:, 1024:1536].rearrange("p (h d) -> p h d", h=4)
        gs = raw[:, 1536:2048]
        rope_tm(P, 'dve', qr[:], qs, C.cos128, C.sin128, i, 4, 64, t1, t2, [raw], [qr])
        rope_tm(P, 'pool', kr[:], ks, C.cos128, C.sin128, i, 4, 64, t3, t4, [raw], [kr])
        CP(P, 'act', vb[:], vs, [raw], [vb])
        ACT(P, sg[:].rearrange("p a b -> p (a b)"), gs, AF.Silu, [raw], [sg])
        TT(P, 'pool', kz[:], kr[:], zeta[:, :].unsqueeze(2).broadcast_to([128, 4, 128]), ALU.mult, [kr, zeta], [kz])
        bank = P.psum[bk[0]]
        bb = bank[:].bitcast(BF16)
        for h in range(4):
            TR(P, bb[:, h * 128:(h + 1) * 128], qr[:, h, :], identb[:], [qr, identb], [bank])
        for h in range(4):
            TR(P, bb[:, (4 + h) * 128:(5 + h) * 128], kr[:, h, :], identb[:], [kr, identb], [bank])
        CP(P, 'act', qkT[:].rearrange("p a b -> p (a b)"), bb[:, :], [bank], [qkT])
        if i > 0:
            TT(P, 'dve', qxT[:], qkT[:, 0:4, :], xi[:], ALU.mult, [qkT, xi], [qxT])
        ib = P.psum[bk[1]]
        for h in range(4):
            MM(P, ib[:, h * 128:(h + 1) * 128], qkT[:, 4 + h, :], qkT[:, h, :], True, True, [qkT], [ib])
        TT(P, 'dve', inT[:].rearrange("p a b -> p (a b)"), ib[:], dm[:].rearrange("p a b -> p (a b)"), ALU.mult, [ib, dm], [inT])
        ob = P.psum[bk[2]]
        for h in range(4):
            MM(P, ob[:, h * 128:(h + 1) * 128], inT[:, h, :], vb[:, h, :], True, i == 0, [inT, vb], [ob])
            if i > 0:
                MM(P, ob[:, h * 128:(h + 1) * 128], qxT[:, h, :], Rb[:, h, :], False, True, [qxT, Rb], [ob])
        kvb = P.psum[bk[3]]
        for h in range(4):
            MM(P, kvb[:, h * 128:(h + 1) * 128], kz[:, h, :], vb[:, h, :], True, True, [kz, vb], [kvb])
        for h in range(4):
            if i == 0:
                CP(P, 'dve', R[:, h, :], kvb[:, h * 128:(h + 1) * 128], [kvb], [R])
            else:
                STT(P, R[:, h, :], R[:, h, :], float(g128[h]), kvb[:, h * 128:(h + 1) * 128], ALU.mult, ALU.add, [R, kvb], [R])
        CP(P, 'act', Rb[:], R[:], [R], [Rb])
        o3 = ob[:].rearrange("p (h e) -> p h e", h=4)
        P.op('dve', lambda e, o3=o3: e.tensor_reduce(out=st[:, 0:4], in_=o3, axis=AX.X, op=ALU.add), [ob], [st])
        TS(P, 'dve', st[:, 0:4], st[:, 0:4], 1.0 / 128, None, ALU.mult, None, [st], [st])
        TT(P, 'dve', oc[:], o3, st[:, 0:4].unsqueeze(2).broadcast_to([128, 4, 128]), ALU.subtract, [ob, st], [oc])
        TT(P, 'pool', sq[:], oc[:], oc[:], ALU.mult, [oc], [sq])
        P.op('dve', lambda e: e.tensor_reduce(out=st[:, 4:8], in_=sq[:], axis=AX.X, op=ALU.add), [sq], [st])
        ACT(P, st[:, 8:12], st[:, 4:8], AF.Sqrt, [st, C.epsT], [st], scale=1.0 / 128, bias=C.epsT[:, 0:1])
        RECIP(P, st[:, 12:16], st[:, 8:12], [st], [st])
        TT(P, 'dve', oc[:], oc[:], st[:, 12:16].unsqueeze(2).broadcast_to([128, 4, 128]), ALU.mult, [oc, st], [oc])
        TT(P, 'pool', oc[:], oc[:], sg[:], ALU.mult, [oc, sg], [oc])
        yb = P.psum[bk[0]]
        for h in range(4):
            TR(P, yb[:, h * 128:(h + 1) * 128], oc[:, h, :], C.ident[:], [oc, C.ident], [yb])
        ys = ystg[0]
        CP(P, 'act', ys[:].rearrange("p a b -> p (a b)"), yb[:], [yb], [ys])
        P.dma('pool', C.mixedT.ap[512:1024, i * 128:(i + 1) * 128].rearrange("(h e) t -> e h t", h=4), ys[:], reads=[ys])
        yield


def phase_nsa(P, C, l):
    m0 = P.mark()
    scale = 128 ** -0.5
    identb = P.sb([128, 128], BF16, 'identb')
    CP(P, 'dve', identb[:], C.ident[:], [C.ident], [identb])
    ld = P.sb([128, 128], F32, 'ld')
    tri = P.sb([128, 128], BF16, 'tri')
    P.dma('sp', ld[:], C.c_tri.ap[:, :], writes=[ld])
    CP(P, 'dve', tri[:], ld[:], [ld], [tri])
    trigt = P.sb([128, 128], BF16, 'trigt')
    ld2 = P.sb([128, 128], F32, 'ld2')
    P.dma('sp', ld2[:], C.c_trigt.ap[:, :], writes=[ld2])
    CP(P, 'dve', trigt[:], ld2[:], [ld2], [trigt])
    ovl = P.sb([128, 34], BF16, 'ovl')
    ld3 = P.sb([128, 34], F32, 'ld3')
    P.dma('sp', ld3[:], C.c_ovl.ap[:, :], writes=[ld3])
    CP(P, 'dve', ovl[:], ld3[:], [ld3], [ovl])
    eexp = P.sb([32, NT, 128], BF16, 'eexp')
    ld4 = P.sb([32, NT * 128], F32, 'ld4')
    P.dma('sp', ld4[:], C.c_eexp.ap[:, :], writes=[ld4])
    CP(P, 'dve', eexp[:].rearrange("p a b -> p (a b)"), ld4[:], [ld4], [eexp])
    selvalid = P.sb([128, NT, 32], F32, 'selvalid')
    selcb = P.sb([128, NT, 32], F32, 'selcb')
    P.dma('sp', selvalid[:], C.c_selvalid.ap.rearrange("(i p) m -> p i m", p=128), writes=[selvalid])
    P.dma('sp', selcb[:], C.c_selcb.ap.rearrange("(i p) m -> p i m", p=128), writes=[selcb])
    qT = P.sb([128, NT, 4, 128], BF16, 'qT')
    kvT = P.sb([128, 4, S], BF16, 'kvT')
    vsb = P.sb([128, NT, 130], BF16, 'vsb')
    vwb = P.sb([128, NT, 130], BF16, 'vwb')
    gates = P.sb([128, NT, 12], F32, 'gates')
    P.op('pool', lambda e: e.memset(vsb[:, :, 128:130], 1.0), [], [vsb])
    P.op('pool', lambda e: e.memset(vwb[:, :, 128:130], 1.0), [], [vwb])
    m1 = P.mark()
    raws = [P.sb([128, 1292], F32, 'raw') for _ in range(2)]
    sq = P.sb([128, 4, 128], F32, 'sq')
    st = P.sb([128, 8], F32, 'st')
    qn = P.sb([128, 4, 128], F32, 'qn')
    t1 = P.sb([128, 4, 64], F32, 't1')
    t2 = P.sb([128, 4, 64], F32, 't2')
    sq3 = P.sb([128, 3, 128], F32, 'sq3')
    st3 = P.sb([128, 8], F32, 'st3')
    kn = P.sb([128, 3, 128], F32, 'kn')
    t3 = P.sb([128, 3, 64], F32, 't3')
    t4 = P.sb([128, 3, 64], F32, 't4')
    qk = [P.sb([128, 8, 128], BF16, 'qk') for _ in range(2)]
    c0 = OFF['nq']
    for i in range(NT):
        raw = raws[i % 2]
        P.dma('sp', raw[:], C.proj.ap[i * 128:(i + 1) * 128, c0:c0 + 1292], writes=[raw])
        q_ = qk[i % 2]
        qs = raw[:, 0:512].rearrange("p (h d) -> p h d", h=4)
        rms_scale_tm(P, C, qs, 4, 128, sq, st, scale, [raw])
        TT(P, 'dve', qn[:], qs, st[:, 0:4].unsqueeze(2).broadcast_to([128, 4, 128]), ALU.mult, [raw, st], [qn])
        rope_tm(P, 'dve', q_[:, 0:4, :], qn[:], C.cos128, C.sin128, i, 4, 64, t1, t2, [qn], [q_])
        kv6 = raw[:, 512:1280].rearrange("p (a b d) -> p a b d", a=3, b=2)
        k3 = kv6[:, :, 0, :]
        v3 = kv6[:, :, 1, :]
        rms_scale_tm(P, C, k3, 3, 128, sq3, st3, 1.0, [raw])
        P.op('dve', lambda e: e.memset(st3[:, 0:1], 1.0), [], [st3])
        TT(P, 'pool', kn[:], k3, st3[:, 0:3].unsqueeze(2).broadcast_to([128, 3, 128]), ALU.mult, [raw, st3], [kn])
        rope_tm(P, 'pool', q_[:, 4:7, :], kn[:], C.cos128, C.sin128, i, 3, 64, t3, t4, [kn], [q_])
        CP(P, 'act', q_[:, 7, :], v3[:, 0, :], [raw], [q_])
        CP(P, 'act', vsb[:, i, 0:128], v3[:, 1, :], [raw], [vsb])
        CP(P, 'act', vwb[:, i, 0:128], v3[:, 2, :], [raw], [vwb])
        ACT(P, gates[:, i, :], raw[:, 1280:1292], AF.Sigmoid, [raw], [gates])
        bank = P.ps([2, 3])
        bb = bank[:].bitcast(BF16)
        for a in range(8):
            TR(P, bb[:, a * 128:(a + 1) * 128], q_[:, a, :], identb[:], [q_, identb], [bank])
        CP(P, 'act', qT[:, i, :, :].rearrange("p h t -> p (h t)"), bb[:, 0:512], [bank], [qT])
        CP(P, 'dve', kvT[:, :, i * 128:(i + 1) * 128], bb[:, 512:1024].rearrange("p (a t) -> p a t", a=4), [bank], [kvT])
    P.release(m1)
    m2 = P.mark()
    wk = P.sb([128, 32, 128], BF16, 'wk')
    wv = P.sb([128, 32, 128], BF16, 'wv')
    P.dma('pool', wk[:], C.nsa_w_cmp_k.ap[l].rearrange("(a p) o -> p a o", p=128), writes=[wk])
    P.dma('pool', wv[:], C.nsa_w_cmp_v.ap[l].rearrange("(a p) o -> p a o", p=128), writes=[wv])
    pe2 = P.sb([128, 2, 128], F32, 'pe2')
    P.op('dve', lambda e: e.memset(pe2[:], 0.0), [], [pe2])
    P.dma('sp', pe2[0:32, 0, :], C.nsa_pe_k.ap[l], reads=[pe2], writes=[pe2])
    P.dma('sp', pe2[0:32, 1, :], C.nsa_pe_v.ap[l], reads=[pe2], writes=[pe2])
    peT = P.sb([128, 2, 32], BF16, 'peT')
    bank = P.ps([2, 3])
    for a in range(2):
        TR(P, bank[:, a * 128:(a + 1) * 128], pe2[:, a, :], C.ident[:], [pe2, C.ident], [bank])
    CP(P, 'dve', peT[:], bank[:, 0:256].rearrange("p (a b) -> p a b", a=2)[:, :, 0:32], [bank], [peT])
    onesb = P.sb([1, 128], BF16, 'onesb')
    P.op('dve', lambda e: e.memset(onesb[:], 1.0), [], [onesb])
    cvec = P.sb([1, 2, 128], BF16, 'cvec')
    kcmpT = P.sb([128, 128], BF16, 'kcmpT')
    vcmp = P.sb([128, 128], BF16, 'vcmp')
    kcn = P.sb([128, 128], BF16, 'kcn')
    junk = P.sb([128, 128], F32, 'junk')
    stc = P.sb([128, 4], F32, 'stc')
    for a, (wt, src_idx) in enumerate(((wk, 0), (wv, 3))):
        cb_ = P.ps([2, 3])
        for li in range(32):
            MM(P, cb_[0:1, 0:128], peT[:, a, li:li + 1], wt[:, li, :], li == 0, li == 31, [peT, wt], [cb_])
        CP(P, 'dve', cvec[:, a, :], cb_[0:1, 0:128], [cb_], [cvec])
        kb_ = P.ps([2, 3])
        for li in range(32):
            MM(P, kb_[0:127, 0:128], kvT[:, src_idx, li:li + 16 * 126 + 1:16], wt[:, li, :], li == 0, False, [kvT, wt], [kb_])
        MM(P, kb_[0:127, 0:128], onesb[0:1, 0:127], cvec[0:1, a, :], False, True, [onesb, cvec], [kb_])
        if a == 0:
            ACT(P, junk[0:127, :], kb_[0:127, 0:128], AF.Square, [kb_], [junk, stc], accum_out=stc[0:127, 0:1])
            ACT(P, stc[0:127, 1:2], stc[0:127, 0:1], AF.Sqrt, [stc, C.epsT], [stc], scale=1.0 / 128, bias=C.epsT[0:127, 0:1])
            RECIP(P, stc[0:127, 2:3], stc[0:127, 1:2], [stc], [stc])
            P.op('dve', lambda e: e.memset(kcn[:], 0.0), [], [kcn])
            ACT(P, kcn[0:127, :], kb_[0:127, 0:128], AF.Copy, [kb_, stc, kcn], [kcn], scale=stc[0:127, 2:3])
            tb = P.ps([2, 3])
            tbb = tb[:].bitcast(BF16)
            TR(P, tbb[:, 0:128], kcn[:], identb[:], [kcn, identb], [tb])
            CP(P, 'dve', kcmpT[:], tbb[:, 0:128], [tb], [kcmpT])
        else:
            P.op('dve', lambda e: e.memset(vcmp[:], 0.0), [], [vcmp])
            CP(P, 'act', vcmp[0:127, :], kb_[0:127, 0:128], [kb_, vcmp], [vcmp])
    P.dbg('dbg_kcmpT', kcmpT, kcmpT[:], [128, 128], BF16)
    P.dbg('dbg_vcmp', vcmp, vcmp[:], [128, 128], BF16)
    P.dbg('dbg_kcT', kvT, kvT[:, 0, :], [128, S], BF16)
    cmask = [P.sb([128, 128], F32, 'cmask') for _ in range(2)]
    PT = [P.sb([128, 4, 128], BF16, 'PT') for _ in range(5)]
    msk = [P.sb([128, 128], BF16, 'msk') for _ in range(2)]
    acc = [P.sb([128, 4, 128], F32, 'acc') for _ in range(2)]
    zi = P.sb([128, 4, 34], F32, 'zi')
    wrk = P.sb([128, 4, 32], F32, 'wrk')
    imp = P.sb([128, 32], F32, 'imp')
    top8 = P.sb([128, 8], F32, 'top8')
    selm = P.sb([128, 32], BF16, 'selm')
    selT = P.sb([32, 128], BF16, 'selT')
    cf = P.sb([128, 3, 4], F32, 'cf')
    rz = P.sb([128, 3, 4], F32, 'rz')
    ostg = [P.sb([128, 4, 128], F32R, 'ostg') for _ in range(2)]
    pti = 0
    for i in range(NT):
        qTi = qT[:, i, :, :].rearrange("p h t -> p (h t)")
        ac = acc[i % 2]
        cm = cmask[i % 2]
        P.dma('sp', cm[0:127, :], C.c_cmpbase.ap[126 - 8 * i:126 - 8 * i + 127, :], writes=[cm])
        sb_ = P.ps([0, 1])
        MM(P, sb_[0:127, :], kcmpT[:, 0:127], qTi, True, True, [kcmpT, qT], [sb_])
        pt = PT[pti % 5]
        pti += 1
        ACT(P, pt[0:127].rearrange("p h t -> p (h t)"), sb_[0:127, :], AF.Exp, [sb_], [pt])
        TT(P, 'dve', pt[0:127], pt[0:127], cm[0:127, :].unsqueeze(1).broadcast_to([127, 4, 128]), ALU.mult, [pt, cm], [pt])
        ob = P.ps([2, 3])
        zb = P.ps([2, 3])
        for h in range(4):
            MM(P, ob[:, h * 128:(h + 1) * 128], pt[0:127, h, :], vcmp[0:127, :], True, True, [pt, vcmp], [ob])
        for h in range(4):
            MM(P, zb[:, h * 34:h * 34 + 34], pt[0:127, h, :], ovl[0:127, :], True, True, [pt, ovl], [zb])
        CP(P, 'dve', zi[:].rearrange("p a b -> p (a b)"), zb[:, 0:136], [zb], [zi])
        TS(P, 'dve', rz[:, 0, :], zi[:, :, 0], 1e-30, None, ALU.max, None, [zi], [rz])
        RECIP(P, rz[:, 0, :], rz[:, 0, :], [rz], [rz])
        TT(P, 'dve', wrk[:], zi[:, :, 1:33], rz[:, 0, :].unsqueeze(2).broadcast_to([128, 4, 32]), ALU.mult, [zi, rz], [wrk])
        P.op('dve', lambda e: e.tensor_reduce(out=imp[:], in_=wrk[:].rearrange("p h m -> p m h"), axis=AX.X, op=ALU.add), [wrk], [imp])
        TT(P, 'dve', cf[:, 0, :], gates[:, i, 0:12:3], rz[:, 0, :], ALU.mult, [gates, rz], [cf])
        for h in range(4):
            ACT(P, ac[:, h, :], ob[:, h * 128:(h + 1) * 128], AF.Copy, [ob, cf], [ac], scale=cf[:, 0, h:h + 1])
        TT(P, 'dve', imp[:], imp[:], selvalid[:, i, :], ALU.mult, [imp, selvalid], [imp])
        TT(P, 'dve', imp[:], imp[:], selcb[:, i, :], ALU.add, [imp, selcb], [imp])
        if i == 13:
            P.dbg('dbg_imp', imp, imp[:], [128, 32])
        P.op('dve', lambda e: e.max(out=top8[:], in_=imp[:]), [imp], [top8])
        TS(P, 'dve', selm[:], imp[:], top8[:, 7:8], None, ALU.is_ge, None, [imp, top8], [selm])
        tb = P.ps([2, 3])
        tbb = tb[:].bitcast(BF16)
        TR(P, tbb[0:32, 0:128], selm[:], identb[:], [selm, identb], [tb])
        CP(P, 'act', selT[:], tbb[0:32, 0:128], [tb], [selT])
        if i == 13:
            P.dbg('dbg_selm', selm, selm[:], [128, 32], BF16)
            P.dbg('dbg_top8', top8, top8[:], [128, 8])
        if i == 13:
            P.dbg('dbg_zi', zi, zi[:], [128, 4, 34])
            P.dbg('dbg_cf', cf, cf[:, 0, :], [128, 4])
            P.dbg('dbg_ac0', ac, ac[:], [128, 4, 128])
            P.dbg('dbg_pt', pt, pt[0:127], [127, 4, 128], BF16)
            P.dbg('dbg_cm', cm, cm[0:127, :], [127, 128])
        for br in (2, 1):
            kbs = [kb for kb in (i - 2, i - 1, i) if kb >= 0] if br == 2 else list(range(i + 1))
            A_, B_ = (P.psum[4], P.psum[5]) if br == 2 else (P.psum[6], P.psum[7])
            vt = vwb if br == 2 else vsb
            for n_, kb in enumerate(kbs):
                sb_ = P.ps([0, 1])
                MM(P, sb_[:, :], kvT[:, br, kb * 128:(kb + 1) * 128], qTi, True, True, [kvT, qT], [sb_])
                pt = PT[pti % 5]
                pti += 1
                ACT(P, pt[:].rearrange("p h t -> p (h t)"), sb_[:, :], AF.Exp, [sb_], [pt])
                if br == 2:
                    if kb == i:
                        TT(P, 'pool', pt[:], pt[:], tri[:].unsqueeze(1).broadcast_to([128, 4, 128]), ALU.mult, [pt, tri], [pt])
                    elif kb == i - 2:
                        TT(P, 'pool', pt[:], pt[:], trigt[:].unsqueeze(1).broadcast_to([128, 4, 128]), ALU.mult, [pt, trigt], [pt])
                else:
                    mb = P.ps([2, 3])
                    MM(P, mb[:, 0:128], eexp[:, kb, :], selT[:, :], True, True, [eexp, selT], [mb])
                    if kb == i:
                        mk = msk[n_ % 2]
                        TT(P, 'dve', mk[:], mb[:, 0:128], tri[:], ALU.mult, [mb, tri], [mk])
                        TT(P, 'dve', pt[:], pt[:], mk[:].unsqueeze(1).broadcast_to([128, 4, 128]), ALU.mult, [pt, mk], [pt])
                    else:
                        TT(P, 'dve', pt[:], pt[:], mb[:, 0:128].unsqueeze(1).broadcast_to([128, 4, 128]), ALU.mult, [pt, mb], [pt])
                for h in range(4):
                    bk = A_ if h < 2 else B_
                    o0 = (h % 2) * 130
                    MM(P, bk[:, o0:o0 + 129], pt[:, h, :], vt[:, kb, 0:129], (n_ == 0 and h % 2 == 0), n_ == len(kbs) - 1,
                       [pt, vt], [bk], skip_group_check=True)
            for h in range(4):
                bk = A_ if h < 2 else B_
                o0 = (h % 2) * 130
                RECIP(P, rz[:, br, h:h + 1], bk[:, o0 + 128:o0 + 129], [bk], [rz])
            TT(P, 'dve', cf[:, br, :], gates[:, i, br:12:3], rz[:, br, :], ALU.mult, [gates, rz], [cf])
            for h in range(4):
                bk = A_ if h < 2 else B_
                o0 = (h % 2) * 130
                STT(P, ac[:, h, :], bk[:, o0:o0 + 128], cf[:, br, h:h + 1], ac[:, h, :], ALU.mult, ALU.add, [bk, cf, ac], [ac])
        yb = P.ps([2, 3])
        for h in range(4):
            TR(P, yb[:, h * 128:(h + 1) * 128], ac[:, h, :], C.ident[:], [ac, C.ident], [yb])
        og = ostg[i % 2]
        CP(P, 'act', og[:].rearrange("p a b -> p (a b)"), yb[:], [yb], [og])
        P.dma('pool', C.mixedT.ap[1024:1536, i * 128:(i + 1) * 128].rearrange("(h e) t -> e h t", h=4), og[:], reads=[og])
    P.barrier()
    P.release(m0)


def phase_s5(P, C, l, banks=None):
    Lc = S5L
    NCH = S // Lc
    cosE = P.sb([128, 16, Lc], F32, 'cosE')
    sinE = P.sb([128, 16, Lc], F32, 'sinE')
    Bc = P.sb([128, 16, 2, 128], F32R, 'Bc')
    Cx = P.sb([128, 16, 2, 128], F32R, 'Cx')
    Dg = P.sb([128, 4, 128], F32R, 'Dg')
    wg = P.sb([128, 4, 512], F32R, 'wg')
    prm = P.sb([128, 16, 16], F32, 'prm')
    P.dma('pool', wg[:], C.s5_w_glu.ap[l].rearrange("(m p) c -> p m c", p=128), writes=[wg])
    LR, LI, LD, DT, A_, TH, R_, CT, ST, FR, FI, CL, SL, DEN, T0, T1 = range(16)
    m1 = P.mark()
    pad = P.sb([128, 3, 128], F32, 'pad')
    P.op('dve', lambda e: e.memset(pad[:], 0.0), [], [pad])
    P.dma('sp', pad[0:16, 0, :], C.s5_lambda_re.ap[l].rearrange("(j g) p -> j (g p)", g=2), reads=[pad], writes=[pad])
    P.dma('sp', pad[0:16, 1, :], C.s5_lambda_im.ap[l].rearrange("(j g) p -> j (g p)", g=2), reads=[pad], writes=[pad])
    ldt = P.sb([16, 2], F32, 'ldt')
    P.dma('sp', ldt[:], C.s5_log_dt.ap[l].rearrange("(j g) -> j g", g=2), writes=[ldt])
    CP(P, 'dve', pad[0:16, 2, :].rearrange("j (g p) -> j g p", g=2), ldt[:].unsqueeze(2).broadcast_to([16, 2, 64]), [ldt, pad], [pad])
    bank = P.ps(banks)
    for k in range(3):
        TR(P, bank[:, k * 128:(k + 1) * 128], pad[:, k, :], C.ident[:], [pad, C.ident], [bank])
    CP(P, 'dve', prm[:, 0:3, :], bank[:, 0:384].rearrange("p (k c) -> p k c", k=3)[:, :, 0:16], [bank], [prm])

    def pv(k):
        return prm[:, k, :]
    ACT(P, pv(DT), pv(LD), AF.Exp, [prm], [prm])
    TT(P, 'dve', pv(A_), pv(LR), pv(DT), ALU.mult, [prm], [prm])
    TT(P, 'dve', pv(TH), pv(LI), pv(DT), ALU.mult, [prm], [prm])
    ACT(P, pv(R_), pv(A_), AF.Exp, [prm], [prm])
    angs = P.sb([128, 2, 16], F32, 'angs')
    tqs = P.sb([128, 2, 16], F32, 'tqs')
    tis = P.sb([128, 2, 16], I32, 'tis')
    sins = P.sb([128, 2, 16], F32, 'sins')
    coss = P.sb([128, 2, 16], F32, 'coss')
    CP(P, 'dve', angs[:, 0, :], pv(TH), [prm], [angs])
    TS(P, 'dve', angs[:, 1, :], pv(TH), float(Lc), None, ALU.mult, None, [prm], [angs])
    range_reduce_sincos(P, angs, tqs, tis, sins, coss, None)
    CP(P, 'dve', pv(ST), sins[:, 0, :], [sins, coss], [prm])
    CP(P, 'dve', pv(SL), sins[:, 1, :], [sins, coss], [prm])
    CP(P, 'dve', pv(CT), coss[:, 0, :], [sins, coss], [prm])
    CP(P, 'dve', pv(CL), coss[:, 1, :], [sins, coss], [prm])
    TT(P, 'dve', pv(T0), pv(R_), pv(CT), ALU.mult, [prm], [prm])
    TT(P, 'dve', pv(T1), pv(R_), pv(ST), ALU.mult, [prm], [prm])
    TS(P, 'dve', pv(T0), pv(T0), -1.0, None, ALU.add, None, [prm], [prm])
    TT(P, 'dve', pv(DEN), pv(LR), pv(LR), ALU.mult, [prm], [prm])
    TT(P, 'dve', pv(FR), pv(LI), pv(LI), ALU.mult, [prm], [prm])
    TT(P, 'dve', pv(DEN), pv(DEN), pv(FR), ALU.add, [prm], [prm])
    RECIP(P, pv(DEN), pv(DEN), [prm], [prm])
    TT(P, 'dve', pv(FR), pv(T0), pv(LR), ALU.mult, [prm], [prm])
    TT(P, 'dve', pv(FI), pv(T1), pv(LI), ALU.mult, [prm], [prm])
    TT(P, 'dve', pv(FR), pv(FR), pv(FI), ALU.add, [prm], [prm])
    TT(P, 'dve', pv(FR), pv(FR), pv(DEN), ALU.mult, [prm], [prm])
    TT(P, 'dve', pv(FI), pv(T1), pv(LR), ALU.mult, [prm], [prm])
    TT(P, 'dve', pv(T1), pv(T0), pv(LI), ALU.mult, [prm], [prm])
    TT(P, 'dve', pv(FI), pv(FI), pv(T1), ALU.subtract, [prm], [prm])
    TT(P, 'dve', pv(FI), pv(FI), pv(DEN), ALU.mult, [prm], [prm])
    iota = P.sb([128, Lc], F32, 'iota')
    P.dma('sp', iota[:], C.c_iota.ap[:, :], writes=[iota])
    ang = P.sb([128, 16, Lc], F32, 'ang')
    tq = P.sb([128, 16, Lc], F32, 'tq')
    ti = P.sb([128, 16, Lc], I32, 'ti')
    for j in range(16):
        TS(P, 'dve' if j % 2 == 0 else 'pool', ang[:, j, :], iota[:], prm[:, TH, j:j + 1], None, ALU.mult, None, [iota, prm], [ang])
    range_reduce_sincos(P, ang, tq, ti, sinE, cosE, None)
    P.release(m1)
    m1 = P.mark()
    braw = P.sb([128, 2, 16, 16], F32, 'braw')
    P.dma('sp', braw[:, 0, :, :], C.s5_b_re.ap[l].rearrange("(j g) p h -> (g p) j h", g=2), writes=[braw])
    P.dma('sp', braw[:, 1, :, :], C.s5_b_im.ap[l].rearrange("(j g) p h -> (g p) j h", g=2), writes=[braw])
    bb = P.sb([128, 2, 16, 16], F32, 'bb')
    tb1 = P.sb([128, 16, 16], F32, 'tb1')
    tb2 = P.sb([128, 16, 16], F32, 'tb2')
    frb = prm[:, FR, :].unsqueeze(2).broadcast_to([128, 16, 16])
    fib = prm[:, FI, :].unsqueeze(2).broadcast_to([128, 16, 16])
    TT(P, 'dve', tb1[:], braw[:, 0], frb, ALU.mult, [braw, prm], [tb1])
    TT(P, 'dve', tb2[:], braw[:, 1], fib, ALU.mult, [braw, prm], [tb2])
    TT(P, 'dve', bb[:, 0], tb1[:], tb2[:], ALU.subtract, [tb1, tb2], [bb])
    TT(P, 'dve', tb1[:], braw[:, 1], frb, ALU.mult, [braw, prm], [tb1])
    TT(P, 'dve', tb2[:], braw[:, 0], fib, ALU.mult, [braw, prm], [tb2])
    TT(P, 'dve', bb[:, 1], tb1[:], tb2[:], ALU.add, [tb1, tb2], [bb])
    X = P.sb([128, 16, 2, 128], F32, 'X')
    P.op('pool', lambda e: e.memset(X[:], 0.0), [], [X])
    for g2 in range(2):
        for jj in range(4):
            for c in range(2):
                col = 32 * jj + 16 * g2
                CP(P, 'dve', X[g2 * 64:(g2 + 1) * 64, jj:16:4, c, col:col + 16], bb[g2 * 64:(g2 + 1) * 64, c, jj:16:4, :], [bb, X], [X])
    for j in range(16):
        bank = P.ps(banks)
        for c in range(2):
            TR(P, bank[:, c * 128:(c + 1) * 128], X[:, j, c, :], C.ident[:], [X, C.ident], [bank])
        CP(P, 'act' if j % 2 == 0 else 'dve', Bc[:, j, :, :].rearrange("p c k -> p (c k)"), bank[:, 0:256], [bank], [Bc])
    craw = P.sb([128, 4, 2, 128], F32, 'craw')
    for c, src in enumerate((C.s5_c_re, C.s5_c_im)):
        for dup in range(2):
            P.dma('sp', craw[:, :, c, dup * 64:(dup + 1) * 64], src.ap[l].rearrange("(m gl) n p -> (gl n) m p", gl=8), writes=[craw])
    P.op('pool', lambda e: e.memset(Cx[:].bitcast(F32), 0.0), [], [Cx])
    ctr = P.sb([128, 128], F32, 'ctr')
    for m in range(4):
        for c in range(2):
            bank = P.ps(banks)
            TR(P, bank[:, 0:128], craw[:, m, c, :], C.ident[:], [craw, C.ident], [bank])
            if c == 0:
                CP(P, 'act', ctr[:], bank[:, 0:128], [bank], [ctr])
            else:
                ACT(P, ctr[:], bank[:, 0:128], AF.Copy, [bank], [ctr], scale=-1.0)
            for gl in range(8):
                g2 = gl % 2
                j = 4 * m + gl // 2
                CP(P, 'dve', Cx[g2 * 64:(g2 + 1) * 64, j, c, 16 * gl:16 * gl + 16], ctr[g2 * 64:(g2 + 1) * 64, 16 * gl:16 * gl + 16], [ctr, Cx], [Cx])
    dcol = P.sb([128, 4], F32, 'dcol')
    P.dma('sp', dcol[:], C.s5_d.ap[l].rearrange("(m gl) n -> (gl n) m", gl=8), writes=[dcol], allow_slow_non_contiguous=True)
    for m in range(4):
        TS(P, 'dve', Dg[:, m, :], C.ident[:], dcol[:, m:m + 1], None, ALU.mult, None, [C.ident, dcol], [Dg])
    P.release(m1)
    uraw = [P.sb([128, 512], F32, 'uraw') for _ in range(2)]
    uT = [P.sb([128, 4, Lc], F32R, 'uT') for _ in range(1)]
    sre = P.sb([128, 16, Lc], F32R, 'sre')
    sim_ = P.sb([128, 16, Lc], F32R, 'sim')
    wk_ = [[P.sb([128, Lc], F32, 'wk') for _ in range(8)] for _ in range(4)]
    bsb = [P.sb([128, 2 * Lc], F32, 'bsb') for _ in range(1)]
    zl = P.sb([128, 2, 16], F32, 'zl')
    zin = P.sb([128, 2, 16], F32, 'zin')
    ztmp = P.sb([128, 2, 16], F32, 'ztmp')
    gT = [P.sb([128, 4, Lc], F32R, 'gT') for _ in range(1)]
    ge = [P.sb([128, Lc], F32, 'ge') for _ in range(3)]
    oT = [P.sb([128, Lc], F32R, 'oT') for _ in range(1)]
    c0 = OFF['u']
    ui = 0
    yield 'main'
    for ch in range(NCH):
        t0 = ch * Lc
        u_T = uT[0]
        for tt in range(Lc // 128):
            ur = uraw[ui % 2]
            ui += 1
            P.dma('sp', ur[:], C.proj.ap[t0 + tt * 128:t0 + (tt + 1) * 128, c0:c0 + 512], writes=[ur])
            bank = P.ps(banks)
            for m in range(4):
                TR(P, bank[:, m * 128:(m + 1) * 128], ur[:, m * 128:(m + 1) * 128], C.ident[:], [ur, C.ident], [bank])
            CP(P, 'act', u_T[:, :, tt * 128:(tt + 1) * 128], bank[:].rearrange("p (m t) -> p m t", m=4), [bank], [u_T])
        if ch > 0:
            clb, slb = prm[:, CL, :], prm[:, SL, :]
            TT(P, 'dve', ztmp[:, 0, :], zl[:, 0, :], clb, ALU.mult, [zl, prm], [ztmp])
            TT(P, 'dve', ztmp[:, 1, :], zl[:, 1, :], slb, ALU.mult, [zl, prm], [ztmp])
            TT(P, 'dve', zin[:, 0, :], ztmp[:, 0, :], ztmp[:, 1, :], ALU.subtract, [ztmp], [zin])
            TT(P, 'dve', ztmp[:, 0, :], zl[:, 0, :], slb, ALU.mult, [zl, prm], [ztmp])
            TT(P, 'dve', ztmp[:, 1, :], zl[:, 1, :], clb, ALU.mult, [zl, prm], [ztmp])
            TT(P, 'dve', zin[:, 1, :], ztmp[:, 0, :], ztmp[:, 1, :], ALU.add, [ztmp], [zin])
        for j in range(16):
            w = wk_[j % 4]
            bank = P.ps(banks)
            MM(P, bank[:, 0:Lc], Bc[:, j, 0, :], u_T[:, j // 4, :], True, True, [Bc, u_T], [bank])
            MM(P, bank[:, Lc:2 * Lc], Bc[:, j, 1, :], u_T[:, j // 4, :], True, True, [Bc, u_T], [bank])
            cj, sj = cosE[:, j, :], sinE[:, j, :]
            if j % 2 == 0:
                e1 = 'dve'
                bre, bim = bank[:, 0:Lc], bank[:, Lc:2 * Lc]
                srcs = [bank]
            else:
                e1 = 'pool'
                bs = bsb[0]
                CP(P, 'act', bs[:], bank[:], [bank], [bs])
                bre, bim = bs[:, 0:Lc], bs[:, Lc:2 * Lc]
                srcs = [bs]
            TT(P, e1, w[0][:], bre, cj, ALU.mult, srcs + [cosE], [w[0]])
            TT(P, e1, w[1][:], bim, sj, ALU.mult, srcs + [sinE], [w[1]])
            TT(P, e1, w[2][:], w[0][:], w[1][:], ALU.add, [w[0], w[1]], [w[2]])
            TT(P, e1, w[3][:], bim, cj, ALU.mult, srcs + [cosE], [w[3]])
            TT(P, e1, w[4][:], bre, sj, ALU.mult, srcs + [sinE], [w[4]])
            TT(P, e1, w[5][:], w[3][:], w[4][:], ALU.subtract, [w[3], w[4]], [w[5]])
            rb = prm[:, R_, j:j + 1].broadcast_to([128, Lc])
            for c_, (src, dst) in enumerate(((w[2], w[6]), (w[5], w[7]))):
                init = 0.0 if ch == 0 else zin[:, c_, j:j + 1]
                P.op('dve', lambda e, src=src, dst=dst, init=init, rb=rb: e.tensor_tensor_scan(
                    out=dst[:], data0=rb, data1=src[:], initial=init, op0=ALU.mult, op1=ALU.add),
                    [src, prm, zin], [dst])
                CP(P, 'act', zl[:, c_, j:j + 1], dst[:, Lc - 1:Lc], [dst], [zl])
            zr, zi_ = w[6], w[7]
            TT(P, e1, w[0][:], zr[:], cj, ALU.mult, [zr, cosE], [w[0]])
            TT(P, e1, w[1][:], zi_[:], sj, ALU.mult, [zi_, sinE], [w[1]])
            TT(P, e1, w[3][:], zr[:], sj, ALU.mult, [zr, sinE], [w[3]])
            TT(P, e1, w[4][:], zi_[:], cj, ALU.mult, [zi_, cosE], [w[4]])
            TT(P, 'dve', sre[:, j, :], w[0][:], w[1][:], ALU.subtract, [w[0], w[1]], [sre])
            TT(P, 'dve', sim_[:, j, :], w[3][:], w[4][:], ALU.add, [w[3], w[4]], [sim_])
            yield
        g_T = gT[0]
        for m in range(4):
            bank = P.ps(banks)
            n_ = 0
            for j in range(4 * m, 4 * m + 4):
                for c_, st_ in enumerate((sre, sim_)):
                    MM(P, bank[:, 0:Lc], Cx[:, j, c_, :], st_[:, j, :], n_ == 0, False, [Cx, st_], [bank])
                    n_ += 1
            MM(P, bank[:, 0:Lc], Dg[:, m, :], u_T[:, m, :], False, True, [Dg, u_T], [bank])
            x_ = bank[:, 0:Lc]
            a, b, c_t = ge[0], ge[1], ge[2]
            ACT(P, a[:], x_, AF.Square, [bank], [a])
            TS(P, 'dve', a[:], a[:], 0.044715, 1.0, ALU.mult, ALU.add, [a], [a])
            TT(P, 'dve', b[:], a[:], x_, ALU.mult, [a, bank], [b])
            ACT(P, c_t[:], b[:], AF.Sigmoid, [b], [c_t], scale=1.5957691216057308)
            TT(P, 'dve', g_T[:, m, :], c_t[:], x_, ALU.mult, [c_t, bank], [g_T])
        for mo in range(4):
            bank = P.ps(banks)
            for m in range(4):
                MM(P, bank[:, 0:Lc], wg[:, m, mo * 128:(mo + 1) * 128], g_T[:, m, :], m == 0, m == 3, [wg, g_T], [bank])
            a = ge[mo % 3]
            ACT(P, a[:], bank[:, 0:Lc], AF.Sigmoid, [bank], [a])
            o_ = oT[0]
            TT(P, 'dve', o_[:], a[:], g_T[:, mo, :].bitcast(F32), ALU.mult, [a, g_T], [o_])
            P.dma('pool', C.mixedT.ap[mo * 128:(mo + 1) * 128, t0:t0 + Lc], o_[:], reads=[o_])
        yield


def run_alone(P, g):
    m0 = P.mark()
    for _ in g:
        pass
    P.release(m0)


def run_pair(P, gA, gB, ratio):
    m0 = P.mark()
    for g in (gA, gB):
        for v in g:
            if v == 'main':
                break
    live = {0: gA, 1: gB}
    while live:
        for k in (0, 1):
            if k not in live:
                continue
            for _ in range(ratio[k]):
                try:
                    next(live[k])
                except StopIteration:
                    del live[k]
                    break
    P.release(m0)


_CACHE = {}


def build_full(nl=L):
    nc = bass.Bass("TRN2", target_bir_lowering=False)
    P = Prog(nc)
    C = Ctx()
    setup_common(P, C, {})
    setup_rope(P, C)
    xin = C.x.ap
    for l in range(nl):
        phase_in_proj(P, C, l, xin)
        run_pair(P, phase_s5(P, C, l, [0, 1, 2, 3]), phase_ret(P, C, l, [4, 5, 6, 7]), (8, 1))
        phase_nsa(P, C, l)
        phase_diff(P, C, l)
        phase_out_proj(P, C, l, xin, C.xa.ap)
        xout = C.out.ap if l == nl - 1 else C.xb.ap
        phase_mlp(P, C, l, C.xa.ap, xout)
        xin = C.xb.ap
    P.barrier()
    P.finalize()
    return nc


def build_layer(lay):
    nc = bass.Bass("TRN2", target_bir_lowering=False)
    P = Prog(nc)
    C = Ctx()
    C.lbase = lay
    setup_common(P, C, {}, nlw=1)
    setup_rope(P, C)
    phase_in_proj(P, C, 0, C.x.ap)
    run_alone(P, phase_s5(P, C, 0))
    run_alone(P, phase_ret(P, C, 0))
    phase_nsa(P, C, 0)
    phase_diff(P, C, 0)
    phase_out_proj(P, C, 0, C.x.ap, C.xa.ap)
    phase_mlp(P, C, 0, C.xa.ap, C.out.ap)
    P.barrier()
    P.finalize()
    return nc


FUSED = True


def kernel(**inputs):
    x = np.ascontiguousarray(inputs['x'], dtype=np.float32)
    pos = np.ascontiguousarray(inputs['positions'], dtype=np.int32)
    B = x.shape[0]
    consts = host_consts()
    if FUSED:
        if 'nc' not in _CACHE:
            _CACHE['nc'] = build_full()
        nc = _CACHE['nc']
        shared = {name: np.ascontiguousarray(inputs[name], dtype=np.float32) for name, _, _ in WEIGHT_SPECS}
        shared.update(consts)
        in_maps = []
        for b in range(B):
            m = dict(shared)
            m['x'] = x[b]
            m['pos'] = pos[b]
            in_maps.append(m)
        res = run_bass_kernel_spmd(nc, in_maps, core_ids=list(range(B)))
        return np.stack([np.asarray(res.results[b]['out'], dtype=np.float32) for b in range(B)], axis=0)
    cur = [x[b] for b in range(B)]
    CPL = 4
    for lay in range(L):
        nc = build_layer(lay)
        shared = {name: np.ascontiguousarray(inputs[name][lay:lay + 1], dtype=np.float32) for name, _, _ in WEIGHT_SPECS}
        shared.update(consts)
        nxt = []
        for b0 in range(0, B, CPL):
            in_maps = []
            for b in range(b0, min(B, b0 + CPL)):
                m = dict(shared)
                m['x'] = cur[b]
                m['pos'] = pos[b]
                in_maps.append(m)
            res = run_bass_kernel_spmd(nc, in_maps, core_ids=list(range(len(in_maps))))
            nxt += [np.asarray(res.results[i]['out'], dtype=np.float32) for i in range(len(in_maps))]
        cur = nxt
    return np.stack(cur, axis=0)
```

```python
import math
import numpy as np
from contextlib import ExitStack
import concourse.bass as bass
import concourse.mybir as mybir
from concourse.bass_utils import run_bass_kernel_spmd

dt = mybir.dt
F32, BF16, F32R, I32 = dt.float32, dt.bfloat16, dt.float32r, dt.int32
AF = mybir.ActivationFunctionType
ALU = mybir.AluOpType
AX = mybir.AxisListType

S = 2048
D = 2048
L = 2
NT = S // 128
N_IN = 5388
D_FF = 8192
EPS = 1e-6
PI = math.pi
S5L = 256

ENGS = ['sp', 'act', 'dve', 'pool', 'pe']
BLK = {'sp': 'sync', 'act': 'scalar', 'dve': 'vector', 'pool': 'gpsimd', 'pe': 'tensor'}
DSIZE = {F32: 4, BF16: 2, I32: 4, F32R: 4}
N_DMA_SEM = 12
SB_BASE = 16384 + 512
SB_END = 224 * 1024

OFF = {}
_o = 0
for _n, _w in [('u', 512), ('rq', 512), ('rk', 512), ('rv', 512), ('rg', 512), ('nq', 512),
               ('nkc', 128), ('nvc', 128), ('nks', 128), ('nvs', 128), ('nkw', 128), ('nvw', 128),
               ('ngate', 12), ('dq', 512), ('dk', 512), ('dv', 512)]:
    OFF[_n] = _o
    _o += _w
assert _o == N_IN


class T:
    __slots__ = ('ap', 'w', 'rs', 'name', 'psum')

    def __init__(self, ap, name=''):
        self.ap = ap
        self.w = None
        self.rs = []
        self.name = name
        self.psum = False

    def __getitem__(self, k):
        return self.ap[k]


class Prog:
    def __init__(self, nc):
        self.nc = nc
        self.ops = {e: [] for e in ENGS}
        self.tiles = []
        self.pending = {e: set() for e in ENGS}
        self.dma_cnt = {}
        self.dma_rr = {e: 0 for e in ENGS}
        self.sb_off = SB_BASE
        self.uid = 0
        self.sb_max = 0
        self.psum = []
        self.ps_rr = 0

    def sb(self, shape, dtype=F32, name='t'):
        per = int(np.prod(shape[1:])) * DSIZE[dtype]
        per = (per + 63) // 64 * 64
        off = self.sb_off
        self.sb_off += per
        self.sb_max = max(self.sb_max, self.sb_off)
        assert self.sb_off <= SB_END, f"SBUF overflow {self.sb_off} at {name}"
        self.uid += 1
        h = self.nc.alloc_sbuf_tensor_at(f"{name}_{self.uid}", list(shape), dtype, offset=off)
        t = T(h.ap(), name)
        self.tiles.append(t)
        return t

    def mark(self):
        return self.sb_off

    def dbg(self, name, t, ap, shape, dtype=F32):
        if not getattr(self, 'debug', False):
            return
        d = self.dram(name, list(shape), dtype, kind="ExternalOutput")
        self.dma('sp', d.ap, ap, reads=[t])

    def release(self, m):
        self.barrier()
        self.sb_off = m

    def track(self, ap, name=''):
        t = T(ap, name)
        self.tiles.append(t)
        return t

    def dram(self, name, shape, dtype=F32, kind="Internal"):
        h = self.nc.dram_tensor(name, list(shape), dtype, kind=kind)
        return self.track(h.ap(), name)

    def init_psum(self):
        for i in range(8):
            h = self.nc.alloc_psum_tensor(f"psb{i}", [128, 512], F32)
            self.psum.append(self.track(h.ap(), f"ps{i}"))
            self.psum[-1].psum = True
        return self.psum

    def ps(self, group=None):
        grp = group if group is not None else list(range(8))
        self.ps_rr += 1
        return self.psum[grp[self.ps_rr % len(grp)]]

    def _deps(self, reads, writes):
        deps = set()
        for t in reads:
            if t.w is not None:
                deps.add(t.w)
            if t.psum:
                deps.update(t.rs)
        for t in writes:
            if t.w is not None:
                deps.add(t.w)
            deps.update(t.rs)
        return deps

    def op(self, eng, fn, reads=(), writes=()):
        idx = len(self.ops[eng])
        tok = ('c', eng, idx)
        deps = self._deps(reads, writes)
        deps |= self.pending[eng]
        self.pending[eng] = set()
        self.ops[eng].append(dict(fn=fn, deps=deps, kind='c', marked=False))
        for t in reads:
            t.rs = [r for r in t.rs if not (r[0] == 'c' and r[1] == eng)]
            t.rs.append(tok)
        for t in writes:
            t.w = tok
            t.rs = []
        return tok

    def dma(self, q, out_ap, in_ap, reads=(), writes=(), **kw):
        i = self.dma_rr[q]
        self.dma_rr[q] = (i + 1) % N_DMA_SEM
        key = (q, i)
        prev = self.dma_cnt.get(key, 0)
        cnt = prev + 16
        self.dma_cnt[key] = cnt
        tok = ('d', key, cnt)
        deps = self._deps(reads, writes)
        deps |= self.pending[q]
        self.pending[q] = set()
        if prev > 0:
            deps.add(('d', key, prev))
        self.ops[q].append(dict(fn=lambda e: e.dma_start(out=out_ap, in_=in_ap, **kw), deps=deps,
                                kind='d', key=key, marked=True))
        for t in reads:
            t.rs.append(tok)
        for t in writes:
            t.w = tok
            t.rs = []
        return tok

    def barrier(self):
        toks = set()
        for e in ENGS:
            for j in range(len(self.ops[e]) - 1, -1, -1):
                if self.ops[e][j]['kind'] == 'c':
                    toks.add(('c', e, j))
                    break
        for key, cnt in self.dma_cnt.items():
            toks.add(('d', key, cnt))
        for e in ENGS:
            self.pending[e] |= toks
        for t in self.tiles:
            t.w = None
            t.rs = []

    def finalize(self):
        nc = self.nc
        def skip(e, idx, d):
            return d[0] == 'c' and d[1] == e and (e == 'pe' or e == 'sp' or idx - d[2] > 12)
        self._skip = skip
        for e in ENGS:
            for idx, o in enumerate(self.ops[e]):
                for d in o['deps']:
                    if d[0] == 'c' and not skip(e, idx, d):
                        self.ops[d[1]][d[2]]['marked'] = True
            for d in self.pending[e]:
                if d[0] == 'c' and not skip(e, 10 ** 9, d):
                    self.ops[d[1]][d[2]]['marked'] = True
        for e in ENGS:
            c = 0
            for o in self.ops[e]:
                if o['kind'] == 'c' and o['marked']:
                    c += 1
                    o['inc'] = c
        self.nwait = 0
        with ExitStack() as st:
            esem = {e: st.enter_context(nc.semaphore(f"s_{e}")) for e in ENGS}
            dsem = {}
            for key in self.dma_cnt:
                dsem[key] = st.enter_context(nc.semaphore(f"d_{key[0]}_{key[1]}"))
            block = st.enter_context(nc.Block())

            def resolve(d):
                if d[0] == 'c':
                    return esem[d[1]], self.ops[d[1]][d[2]]['inc'], d[1]
                return dsem[d[1]], d[2], d[1]

            def replay(e, eng):
                seen = {}
                ops = self.ops[e]

                def do_waits(deps, idx):
                    need = {}
                    for d in deps:
                        if skip(e, idx, d):
                            continue
                        sem, val, key = resolve(d)
                        k = (d[0], key)
                        if val > need.get(k, (None, 0))[1]:
                            need[k] = (sem, val)
                    for k, (sem, val) in need.items():
                        if seen.get(k, 0) >= val:
                            continue
                        seen[k] = val
                        eng.wait_ge(sem, val)
                        self.nwait += 1

                for idx, o in enumerate(ops):
                    do_waits(o['deps'], idx)
                    ins = o['fn'](eng)
                    if o['kind'] == 'd':
                        ins.then_inc(dsem[o['key']], 16)
                    elif o['marked']:
                        ins.then_inc(esem[e], 1)
                do_waits(self.pending[e], 10 ** 9)

            for e in ENGS:
                getattr(block, BLK[e])(lambda eng, e=e: replay(e, eng))
        return nc


def ACT(P, out, in_, func, r, w, **kw):
    return P.op('act', lambda e: e.activation(out=out, in_=in_, func=func, **kw), r, w)


def TT(P, eng, out, in0, in1, op, r, w):
    return P.op(eng, lambda e: e.tensor_tensor(out=out, in0=in0, in1=in1, op=op), r, w)


def TS(P, eng, out, in0, s1, s2, op0, op1, r, w, **kw):
    if op1 is None:
        return P.op(eng, lambda e: e.tensor_scalar(out=out, in0=in0, scalar1=s1, scalar2=None, op0=op0, **kw), r, w)
    return P.op(eng, lambda e: e.tensor_scalar(out=out, in0=in0, scalar1=s1, scalar2=s2, op0=op0, op1=op1, **kw), r, w)


def STT(P, out, in0, scalar, in1, op0, op1, r, w):
    return P.op('dve', lambda e: e.scalar_tensor_tensor(out=out, in0=in0, scalar=scalar, in1=in1, op0=op0, op1=op1), r, w)


def MM(P, out, lhsT, rhs, start, stop, r, w, **kw):
    return P.op('pe', lambda e: e.matmul(out, lhsT=lhsT, rhs=rhs, start=start, stop=stop, **kw), r, w)


def TR(P, out, in_, ident, r, w):
    return P.op('pe', lambda e: e.transpose(out=out, in_=in_, identity=ident), r, w)


def CP(P, eng, out, in_, r, w):
    if eng == 'act':
        return P.op('act', lambda e: e.activation(out=out, in_=in_, func=AF.Copy), r, w)
    return P.op(eng, lambda e: e.tensor_copy(out=out, in_=in_), r, w)


def RECIP(P, out, in_, r, w):
    return P.op('dve', lambda e: e.reciprocal(out=out, in_=in_), r, w)


class Ctx:
    pass


def norm_transpose_tile(P, C, xt, h, st, gbc, hT_t, dst_fn, evac_rr):
    ACT(P, h[:], xt[:], AF.Square, [xt], [h, st], accum_out=st[:, 0:1])
    ACT(P, st[:, 1:2], st[:, 0:1], AF.Sqrt, [st, C.epsT], [st], scale=1.0 / D, bias=C.epsT[:, 0:1])
    RECIP(P, st[:, 2:3], st[:, 1:2], [st], [st])
    STT(P, h[:], xt[:], st[:, 2:3], gbc[:], ALU.mult, ALU.mult, [xt, st, gbc, h], [h])
    for kq in range(4):
        bank = P.ps()
        for j in range(4):
            k = kq * 4 + j
            TR(P, bank[:, j * 128:(j + 1) * 128], h[:, k * 128:(k + 1) * 128], C.ident[:], [h, C.ident], [bank])
        eng = 'act' if (evac_rr + kq) % 2 == 0 else 'dve'
        CP(P, eng, dst_fn(kq), bank[:].rearrange("p (a b) -> p a b", a=4), [bank], [hT_t])


def phase_in_proj(P, C, l, xin):
    m0 = P.mark()
    gbc = P.sb([128, D], F32, 'gbc')
    P.dma('act', gbc[:], C.norm1_g.ap[l:l + 1, :].broadcast_to([128, D]), writes=[gbc])
    xts = [P.sb([128, D], F32, 'xt') for _ in range(2)]
    hs = [P.sb([128, D], F32, 'h') for _ in range(2)]
    sts = [P.sb([128, 4], F32, 'st') for _ in range(2)]
    hT = [P.sb([128, 16, 128], F32R, 'hT') for _ in range(8)]
    wb = [P.sb([128, 16, 512], F32R, 'wb') for _ in range(2)]
    stg = [P.sb([128, 512], F32, 'stg') for _ in range(4)]
    nblk = (N_IN + 511) // 512
    wi = 0
    si = 0
    for th in range(2):
        for i in range(8):
            tok0 = th * 1024 + i * 128
            xt, h, st = xts[i % 2], hs[i % 2], sts[i % 2]
            P.dma('sp', xt[:], xin[tok0:tok0 + 128, :], writes=[xt])
            norm_transpose_tile(P, C, xt, h, st, gbc, hT[i], (lambda kq, t=hT[i]: t[:, kq * 4:(kq + 1) * 4, :]), i)
        for cb in range(nblk):
            c0 = cb * 512
            nc_ = min(512, N_IN - c0)
            w = wb[wi % 2]
            wi += 1
            P.dma('pool', w[:, :, :nc_], C.w_in.ap[l, :, c0:c0 + nc_].rearrange("(k p) c -> p k c", p=128), writes=[w])
            for i in range(8):
                tok0 = th * 1024 + i * 128
                bank = P.ps()
                for k in range(16):
                    MM(P, bank[:, :nc_], hT[i][:, k, :], w[:, k, :nc_], k == 0, k == 15, [hT[i], w], [bank])
                sg = stg[si % 4]
                CP(P, 'act' if si % 2 == 0 else 'dve', sg[:, :nc_], bank[:, :nc_], [bank], [sg])
                P.dma('sp' if si % 2 == 0 else 'act', C.proj.ap[tok0:tok0 + 128, c0:c0 + nc_], sg[:, :nc_], reads=[sg])
                si += 1
    P.barrier()
    P.release(m0)


def phase_out_proj(P, C, l, xin, xout):
    m0 = P.mark()
    mT = [P.sb([128, 16, 128], F32R, 'mT') for _ in range(8)]
    wb = [P.sb([128, 16, 512], F32R, 'wb') for _ in range(2)]
    xb = [P.sb([128, 512], F32, 'xb') for _ in range(4)]
    wi = 0
    si = 0
    for th in range(2):
        for i in range(8):
            tok0 = th * 1024 + i * 128
            P.dma('pool', mT[i][:], C.mixedT.ap[:, tok0:tok0 + 128].rearrange("(k p) t -> p k t", p=128), writes=[mT[i]])
        for cb in range(4):
            c0 = cb * 512
            w = wb[wi % 2]
            wi += 1
            P.dma('pool', w[:], C.w_out.ap[l, :, c0:c0 + 512].rearrange("(k p) c -> p k c", p=128), writes=[w])
            for i in range(8):
                tok0 = th * 1024 + i * 128
                x_ = xb[si % 4]
                P.dma('sp', x_[:], xin[tok0:tok0 + 128, c0:c0 + 512], writes=[x_])
                bank = P.ps()
                for k in range(16):
                    MM(P, bank[:], mT[i][:, k, :], w[:, k, :], k == 0, k == 15, [mT[i], w], [bank])
                TT(P, 'dve', x_[:], x_[:], bank[:], ALU.add, [x_, bank], [x_])
                P.dma('act', xout[tok0:tok0 + 128, c0:c0 + 512], x_[:], reads=[x_])
                si += 1
    P.barrier()
    P.release(m0)


def phase_mlp(P, C, l, xin, xout):
    m0 = P.mark()
    gbc = P.sb([128, D], F32, 'gbc')
    P.dma('act', gbc[:], C.norm2_g.ap[l:l + 1, :].broadcast_to([128, D]), writes=[gbc])
    xacc = [P.sb([128, D], F32, 'xacc') for _ in range(4)]
    h = P.sb([128, D], F32, 'h')
    st = P.sb([128, 4], F32, 'st')
    h2T = P.sb([128, 16, 512], F32R, 'h2T')
    aT = [P.sb([128, 512], F32R, 'aT') for _ in range(16)]
    wb = [P.sb([128, 16, 512], F32R, 'wb') for _ in range(2)]
    rl = [P.sb([128, 512], F32, 'rl') for _ in range(2)]
    wi = 0
    ri = 0
    for tt4 in range(4):
        t0 = tt4 * 512
        for j in range(4):
            P.dma('sp', xacc[j][:], xin[t0 + j * 128:t0 + (j + 1) * 128, :], writes=[xacc[j]])
            norm_transpose_tile(P, C, xacc[j], h, st, gbc, h2T,
                                (lambda kq, j=j: h2T[:, kq * 4:(kq + 1) * 4, j * 128:(j + 1) * 128]), j)
        for q in range(4):
            for cbw in range(4):
                c0 = q * 2048 + cbw * 512
                w = wb[wi % 2]
                wi += 1
                P.dma('pool', w[:], C.mlp_w1.ap[l, :, c0:c0 + 512].rearrange("(k p) c -> p k c", p=128), writes=[w])
                for c in range(4):
                    bank = P.ps()
                    a = aT[cbw * 4 + c]
                    for k in range(16):
                        MM(P, bank[:], w[:, k, c * 128:(c + 1) * 128], h2T[:, k, :], k == 0, k == 15, [w, h2T], [bank])
                    r_ = rl[ri % 2]
                    ri += 1
                    ACT(P, r_[:], bank[:], AF.Relu, [bank], [r_])
                    TT(P, 'dve', a[:], r_[:], r_[:], ALU.mult, [r_], [a])
            for cb in range(4):
                c0 = cb * 512
                w = wb[wi % 2]
                wi += 1
                P.dma('pool', w[:], C.mlp_w2.ap[l, q * 2048:(q + 1) * 2048, c0:c0 + 512].rearrange("(k p) c -> p k c", p=128), writes=[w])
                for j in range(4):
                    bank = P.ps()
                    for k in range(16):
                        MM(P, bank[:], aT[k][:, j * 128:(j + 1) * 128], w[:, k, :], k == 0, k == 15, [aT[k], w], [bank])
                    TT(P, 'dve', xacc[j][:, c0:c0 + 512], xacc[j][:, c0:c0 + 512], bank[:], ALU.add, [xacc[j], bank], [xacc[j]])
        for j in range(4):
            P.dma('act', xout[t0 + j * 128:t0 + (j + 1) * 128, :], xacc[j][:], reads=[xacc[j]])
    P.barrier()
    P.release(m0)


WEIGHT_SPECS = [
    ('norm1_g', [L, D], F32), ('w_in', [L, D, N_IN], F32R),
    ('s5_lambda_re', [L, 32, 64], F32), ('s5_lambda_im', [L, 32, 64], F32), ('s5_log_dt', [L, 32], F32),
    ('s5_b_re', [L, 32, 64, 16], F32), ('s5_b_im', [L, 32, 64, 16], F32),
    ('s5_c_re', [L, 32, 16, 64], F32), ('s5_c_im', [L, 32, 16, 64], F32),
    ('s5_d', [L, 32, 16], F32), ('s5_w_glu', [L, 512, 512], F32R),
    ('nsa_pe_k', [L, 32, 128], F32), ('nsa_pe_v', [L, 32, 128], F32),
    ('nsa_w_cmp_k', [L, 4096, 128], F32), ('nsa_w_cmp_v', [L, 4096, 128], F32),
    ('diff_lq1', [L, 64], F32), ('diff_lk1', [L, 64], F32), ('diff_lq2', [L, 64], F32), ('diff_lk2', [L, 64], F32),
    ('w_out', [L, D, D], F32R), ('norm2_g', [L, D], F32),
    ('mlp_w1', [L, D, D_FF], F32R), ('mlp_w2', [L, D_FF, D], F32R),
]


def host_consts():
    c = {}
    c['c_ident'] = np.eye(128, dtype=np.float32)
    kk = np.arange(128)
    c['c_tri'] = (kk[:, None] <= kk[None, :]).astype(np.float32)
    c['c_trigt'] = (kk[:, None] > kk[None, :]).astype(np.float32)
    inv128 = (10000.0 ** (-np.arange(0, 128, 2, dtype=np.float32) / np.float32(128))).astype(np.float32)
    inv64 = (10000.0 ** (-np.arange(0, 64, 2, dtype=np.float32) / np.float32(64))).astype(np.float32)
    gam = 1.0 - 2.0 ** (-5.0 - np.arange(4, dtype=np.float64))
    sc = 128.0 ** -0.5
    dm = np.zeros((128, 4, 128), np.float64)
    for h in range(4):
        df = kk[None, :] - kk[:, None]
        dm[:, h, :] = np.where(df >= 0, gam[h] ** np.maximum(df, 0), 0.0) * sc
    c['c_ret_dm'] = dm.reshape(128, 512).astype(np.float32)
    c['c_ret_zeta'] = (gam[None, :] ** (127.0 - kk[:, None]) * sc).astype(np.float32)
    xi = gam[:, None] ** (kk[None, :] + 1.0)
    c['c_ret_xi'] = np.tile(xi.reshape(1, 512), (128, 1)).astype(np.float32)
    jp = np.arange(-126, 130)
    c['c_cmpbase'] = ((16 * jp[:, None] + 31) <= kk[None, :]).astype(np.float32)
    n_cmp = 127
    cmp_start = np.arange(n_cmp) * 16
    sel_start = np.arange(32) * 64
    ovl = ((cmp_start[None, :] < sel_start[:, None] + 64) & (cmp_start[None, :] + 32 > sel_start[:, None])).astype(np.float32)
    oz = np.zeros((128, 34), np.float32)
    oz[:127, 0] = 1.0
    oz[:127, 1:33] = ovl.T
    c['c_ovl'] = oz
    t = np.arange(S)
    cur = t // 64
    jj = np.arange(32)
    valid = (jj[None, :] <= cur[:, None])
    forced = (jj[None, :] == 0) | (jj[None, :] == cur[:, None]) | (jj[None, :] == cur[:, None] - 1)
    c['c_selvalid'] = valid.astype(np.float32)
    c['c_selcb'] = np.where(valid, np.where(forced, 1e4, 0.0), -1e30).astype(np.float32)
    ee = np.zeros((32, NT, 128), np.float32)
    for kb in range(NT):
        for k in range(128):
            ee[2 * kb + k // 64, kb, k] = 1.0
    c['c_eexp'] = ee.reshape(32, NT * 128)
    c['c_iota'] = np.tile(np.arange(S5L, dtype=np.float32)[None, :], (128, 1))
    c['c_inv128'] = np.tile(inv128[None, :], (128, 1)).astype(np.float32)
    c['c_inv64'] = np.tile(inv64[None, :], (128, 1)).astype(np.float32)
    return c


def setup_common(P, C, dbg, nlw=L):
    def kind_of(name, default="Internal"):
        return dbg.get(name, default)
    C.x = P.dram("x", [S, D], F32, kind="ExternalInput")
    C.pos = P.dram("pos", [S], I32, kind="ExternalInput")
    for name, shape, dtp in WEIGHT_SPECS:
        setattr(C, name, P.dram(name, [nlw] + list(shape[1:]), dtp, kind="ExternalInput"))
    for name, arr in host_consts().items():
        setattr(C, name, P.dram(name, list(arr.shape), F32, kind="ExternalInput"))
    C.out = P.dram("out", [S, D], F32, kind="ExternalOutput")
    C.proj = P.dram("proj", [S, N_IN], F32, kind=kind_of("proj"))
    C.mixedT = P.dram("mixedT", [D, S], F32R, kind=kind_of("mixedT"))
    C.xa = P.dram("xa", [S, D], F32, kind=kind_of("xa"))
    C.xb = P.dram("xb", [S, D], F32, kind=kind_of("xb"))
    P.init_psum()
    C.ident = P.sb([128, 128], F32, 'ident')
    P.dma('sp', C.ident[:], C.c_ident.ap[:, :], writes=[C.ident])
    C.epsT = P.sb([128, 1], F32, 'epsT')
    P.op('dve', lambda e: e.memset(C.epsT[:], EPS), [], [C.epsT])
    P.barrier()


def setup_rope(P, C):
    C.cos128 = P.sb([128, NT, 64], F32, 'cos128')
    C.sin128 = P.sb([128, NT, 64], F32, 'sin128')
    C.cos64 = P.sb([128, NT, 32], F32, 'cos64')
    C.sin64 = P.sb([128, NT, 32], F32, 'sin64')
    m0 = P.mark()
    posi = P.sb([128, NT], I32, 'posi')
    posf = P.sb([128, NT], F32, 'posf')
    P.dma('sp', posi[:], C.pos.ap.rearrange("(i p) -> p i", p=128), writes=[posi], allow_slow_non_contiguous=True)
    CP(P, 'dve', posf[:], posi[:], [posi], [posf])
    for (half, cinv, cosT, sinT) in ((64, C.c_inv128, C.cos128, C.sin128), (32, C.c_inv64, C.cos64, C.sin64)):
        inv = P.sb([128, half], F32, 'inv')
        P.dma('sp', inv[:], cinv.ap[:, :], writes=[inv])
        ang = P.sb([128, NT, half], F32, 'ang')
        tq = P.sb([128, NT, half], F32, 'tq')
        ti = P.sb([128, NT, half], I32, 'ti')
        TT(P, 'dve', ang[:], posf[:].unsqueeze(2).broadcast_to([128, NT, half]),
           inv[:].unsqueeze(1).broadcast_to([128, NT, half]), ALU.mult, [posf, inv], [ang])
        range_reduce_sincos(P, ang, tq, ti, sinT, cosT, [128, NT * half])
    P.barrier()
    P.release(m0)


def range_reduce_sincos(P, ang, tq, ti, sinT, cosT, shape2):
    def f(t):
        a = t[:]
        if len(a.shape) == 3:
            a = a.rearrange("p a b -> p (a b)")
        return a
    A, Q, I_ = f(ang), f(tq), f(ti)
    TS(P, 'dve', Q, A, 1.0 / (2 * PI), None, ALU.mult, None, [ang], [tq])
    CP(P, 'dve', I_, Q, [tq], [ti])
    CP(P, 'dve', Q, I_, [ti], [tq])
    STT(P, A, Q, -2 * PI, A, ALU.mult, ALU.add, [tq, ang], [ang])
    def wrap(X, xt, up=True, down=True):
        if up:
            TS(P, 'dve', I_.bitcast(F32), X, PI, -2 * PI, ALU.is_gt, ALU.mult, [xt], [ti])
            TT(P, 'dve', X, X, I_.bitcast(F32), ALU.add, [xt, ti], [xt])
        if down:
            TS(P, 'dve', I_.bitcast(F32), X, -PI, 2 * PI, ALU.is_lt, ALU.mult, [xt], [ti])
            TT(P, 'dve', X, X, I_.bitcast(F32), ALU.add, [xt, ti], [xt])
    wrap(A, ang)
    TS(P, 'dve', Q, A, PI / 2, None, ALU.add, None, [ang], [tq])
    wrap(Q, tq, down=False)
    TS(P, 'dve', A, A, PI, -PI, ALU.min, ALU.max, [ang], [ang])
    TS(P, 'dve', Q, Q, PI, -PI, ALU.min, ALU.max, [tq], [tq])
    ACT(P, f(sinT), A, AF.Sin, [ang], [sinT])
    ACT(P, f(cosT), Q, AF.Sin, [tq], [cosT])


def rope_tm(P, eng, dst, src, cosT, sinT, i, H, half, t1, t2, r, w):
    cb = cosT[:, i, :].unsqueeze(1).broadcast_to([128, H, half])
    sb_ = sinT[:, i, :].unsqueeze(1).broadcast_to([128, H, half])
    x1, x2 = src[:, :, :half], src[:, :, half:]
    d1, d2 = dst[:, :, :half], dst[:, :, half:]
    TT(P, eng, t1[:], x1, cb, ALU.mult, r + [cosT], [t1])
    TT(P, eng, t2[:], x2, sb_, ALU.mult, r + [sinT], [t2])
    TT(P, eng, d1, t1[:], t2[:], ALU.subtract, [t1, t2], w)
    TT(P, eng, t1[:], x2, cb, ALU.mult, r + [cosT], [t1])
    TT(P, eng, t2[:], x1, sb_, ALU.mult, r + [sinT], [t2])
    TT(P, eng, d2, t1[:], t2[:], ALU.add, [t1, t2], w)


def rms_scale_tm(P, C, src, H, dh, sq, st, scale, r):
    TT(P, 'pool', sq[:], src, src, ALU.mult, r, [sq])
    P.op('dve', lambda e: e.tensor_reduce(out=st[:, H:2 * H], in_=sq[:], axis=AX.X, op=ALU.add), [sq], [st])
    ACT(P, st[:, H:2 * H], st[:, H:2 * H], AF.Sqrt, [st, C.epsT], [st], scale=1.0 / dh, bias=C.epsT[:, 0:1])
    RECIP(P, st[:, 0:H], st[:, H:2 * H], [st], [st])
    if scale != 1.0:
        TS(P, 'dve', st[:, 0:H], st[:, 0:H], float(scale), None, ALU.mult, None, [st], [st])


def phase_diff(P, C, l):
    m0 = P.mark()
    lam_init = 0.8 - 0.6 * math.exp(-0.3 * (l + getattr(C, 'lbase', 0)))
    qT = P.sb([128, 4, S], BF16, 'qT')
    kT = P.sb([128, 4, S], BF16, 'kT')
    V1 = [P.sb([128, 4, 130], BF16, 'V1') for _ in range(NT)]
    tri = P.sb([128, 128], BF16, 'tri')
    trif = P.sb([128, 128], F32, 'trif')
    P.dma('sp', trif[:], C.c_tri.ap[:, :], writes=[trif])
    CP(P, 'dve', tri[:], trif[:], [trif], [tri])
    identb = P.sb([128, 128], BF16, 'identb')
    CP(P, 'dve', identb[:], C.ident[:], [C.ident], [identb])
    lam = P.sb([128, 8], F32, 'lam')
    lqk = P.sb([128, 4, 64], F32, 'lqk')
    for j, nm in enumerate(['diff_lq1', 'diff_lk1', 'diff_lq2', 'diff_lk2']):
        P.dma('sp', lqk[:, j, :], getattr(C, nm).ap[l:l + 1, :].broadcast_to([128, 64]), writes=[lqk])
    lp = P.sb([128, 2, 64], F32, 'lp')
    TT(P, 'dve', lp[:, 0, :], lqk[:, 0, :], lqk[:, 1, :], ALU.mult, [lqk], [lp])
    TT(P, 'dve', lp[:, 1, :], lqk[:, 2, :], lqk[:, 3, :], ALU.mult, [lqk], [lp])
    P.op('dve', lambda e: e.tensor_reduce(out=lam[:, 0:2], in_=lp[:], axis=AX.X, op=ALU.add), [lp], [lam])
    ACT(P, lam[:, 2:4], lam[:, 0:2], AF.Exp, [lam], [lam])
    TT(P, 'dve', lam[:, 4:5], lam[:, 3:4], lam[:, 2:3], ALU.subtract, [lam], [lam])
    TS(P, 'dve', lam[:, 5:6], lam[:, 4:5], -lam_init, None, ALU.add, None, [lam], [lam])
    neglam = lam[:, 5:6]
    m1 = P.mark()
    raws = [P.sb([128, 1536], F32, 'raw') for _ in range(2)]
    sqs = [P.sb([128, 8, 64], F32, 'sq') for _ in range(2)]
    sts = [P.sb([128, 16], F32, 'st') for _ in range(2)]
    qns = [P.sb([128, 8, 64], F32, 'qn') for _ in range(2)]
    t1s = [P.sb([128, 8, 32], F32, 't1') for _ in range(2)]
    t2s = [P.sb([128, 8, 32], F32, 't2') for _ in range(2)]
    qr = [P.sb([128, 8, 64], BF16, 'qr') for _ in range(2)]
    c0 = OFF['dq']
    for i in range(NT):
        raw = raws[i % 2]
        P.dma('sp', raw[:], C.proj.ap[i * 128:(i + 1) * 128, c0:c0 + 1536], writes=[raw])
        for which, dstT, scale in ((0, qT, 64 ** -0.5), (1, kT, 1.0)):
            src = raw[:, which * 512:(which + 1) * 512].rearrange("p (h d) -> p h d", h=8)
            sq, st, qn, t1, t2 = sqs[which], sts[which], qns[which], t1s[which], t2s[which]
            rms_scale_tm(P, C, src, 8, 64, sq, st, scale, [raw])
            TT(P, 'dve', qn[:], src, st[:, 0:8].unsqueeze(2).broadcast_to([128, 8, 64]), ALU.mult, [raw, st], [qn])
            q_ = qr[which]
            rope_tm(P, 'dve' if which == 0 else 'pool', q_[:], qn[:], C.cos64, C.sin64, i, 8, 32, t1, t2, [qn], [q_])
            bank = P.ps([6, 7])
            bb = bank[:].bitcast(BF16)
            for h in range(4):
                TR(P, bb[:, h * 128:(h + 1) * 128], q_[:, 2 * h:2 * h + 2, :].rearrange("p a b -> p (a b)"), identb[:], [q_, identb], [bank])
            CP(P, 'act', dstT[:, :, i * 128:(i + 1) * 128], bb[:, 0:512].rearrange("p (h t) -> p h t", h=4), [bank], [dstT])
        v1 = V1[i]
        CP(P, 'act', v1[:, :, 0:128], raw[:, 1024:1536].rearrange("p (h d) -> p h d", h=4), [raw], [v1])
        P.op('pool', lambda e, v1=v1: e.memset(v1[:, :, 128:129], 1.0), [], [v1])
    P.release(m1)
    PT = [P.sb([128, 512], BF16, 'PT') for _ in range(3)]
    o1 = [P.sb([128, 128], F32, 'o1') for _ in range(4)]
    dd = [P.sb([128, 128], F32, 'dd') for _ in range(2)]
    junk = P.sb([128, 128], F32, 'junk')
    es = [P.sb([128, 8], F32, 'es') for _ in range(2)]
    ostg = [P.sb([128, 128], F32R, 'ostg') for _ in range(2)]
    pti = 0
    ei = 0
    for h in range(4):
        for Q in range(4):
            for c in range(2):
                O = [P.psum[2 + j] for j in range(4)]
                nkb = 4 * Q + 4
                for kb in range(nkb):
                    jmin = max(0, kb - 4 * Q)
                    q0 = jmin * 128
                    sbk = P.ps([0, 1])
                    MM(P, sbk[:, q0:512], kT[c * 64:(c + 1) * 64, h, kb * 128:(kb + 1) * 128],
                       qT[c * 64:(c + 1) * 64, h, Q * 512 + q0:(Q + 1) * 512], True, True, [kT, qT], [sbk])
                    pt = PT[pti % 3]
                    pti += 1
                    ACT(P, pt[:, q0:512], sbk[:, q0:512], AF.Exp, [sbk], [pt])
                    if kb >= 4 * Q:
                        TT(P, 'dve', pt[:, q0:q0 + 128], pt[:, q0:q0 + 128], tri[:], ALU.mult, [pt, tri], [pt])
                    for j in range(jmin, 4):
                        MM(P, O[j][:, 0:129], pt[:, j * 128:(j + 1) * 128], V1[kb][:, h, 0:129],
                           kb == 0, kb == 4 * Q + j, [pt, V1[kb]], [O[j]])
                for j in range(4):
                    e_ = es[ei % 2]
                    ei += 1
                    RECIP(P, e_[:, 0:1], O[j][:, 128:129], [O[j]], [e_])
                    if c == 0:
                        ACT(P, o1[j][:], O[j][:, 0:128], AF.Copy, [O[j], e_], [o1[j]], scale=e_[:, 0:1])
                    else:
                        d_ = dd[j % 2]
                        TT(P, 'dve', e_[:, 1:2], e_[:, 0:1], neglam, ALU.mult, [e_, lam], [e_])
                        STT(P, d_[:], O[j][:, 0:128], e_[:, 1:2], o1[j][:], ALU.mult, ALU.add, [O[j], e_, o1[j]], [d_])
                        ACT(P, junk[:], d_[:], AF.Square, [d_], [junk, e_], accum_out=e_[:, 2:3])
                        ACT(P, e_[:, 3:4], e_[:, 2:3], AF.Sqrt, [e_, C.epsT], [e_], scale=1.0 / 128, bias=C.epsT[:, 0:1])
                        RECIP(P, e_[:, 4:5], e_[:, 3:4], [e_], [e_])
                        TS(P, 'dve', d_[:], d_[:], e_[:, 4:5], 1.0 - lam_init, ALU.mult, ALU.mult, [d_, e_], [d_])
                        bank = P.ps([6, 7])
                        TR(P, bank[:, 0:128], d_[:], C.ident[:], [d_, C.ident], [bank])
                        og = ostg[j % 2]
                        CP(P, 'act', og[:], bank[:, 0:128], [bank], [og])
                        tok0 = Q * 512 + j * 128
                        P.dma('pool', C.mixedT.ap[1536 + h * 128:1536 + (h + 1) * 128, tok0:tok0 + 128], og[:], reads=[og])
    P.barrier()
    P.release(m0)


def phase_ret(P, C, l, banks=None):
    bk = banks if banks is not None else [0, 2, 4, 6]
    gam = [1.0 - 2.0 ** (-5.0 - h) for h in range(4)]
    g128 = [g ** 128 for g in gam]
    identb = P.sb([128, 128], BF16, 'identb')
    CP(P, 'dve', identb[:], C.ident[:], [C.ident], [identb])
    dm = P.sb([128, 4, 128], F32, 'dm')
    P.dma('sp', dm[:].rearrange("p a b -> p (a b)"), C.c_ret_dm.ap[:, :], writes=[dm])
    zeta = P.sb([128, 4], F32, 'zeta')
    P.dma('sp', zeta[:], C.c_ret_zeta.ap[:, :], writes=[zeta])
    xi = P.sb([128, 4, 128], F32, 'xi')
    P.dma('sp', xi[:].rearrange("p a b -> p (a b)"), C.c_ret_xi.ap[:, :], writes=[xi])
    R = P.sb([128, 4, 128], F32, 'R')
    Rb = P.sb([128, 4, 128], BF16, 'Rb')
    raws = [P.sb([128, 2048], F32, 'raw') for _ in range(1)]
    t1 = P.sb([128, 4, 64], F32, 't1')
    t2 = P.sb([128, 4, 64], F32, 't2')
    t3 = P.sb([128, 4, 64], F32, 't3')
    t4 = P.sb([128, 4, 64], F32, 't4')
    qr = P.sb([128, 4, 128], BF16, 'qr')
    kr = P.sb([128, 4, 128], BF16, 'kr')
    kz = P.sb([128, 4, 128], BF16, 'kz')
    vb = P.sb([128, 4, 128], BF16, 'vb')
    qkT = P.sb([128, 8, 128], BF16, 'qkT')
    qxT = P.sb([128, 4, 128], BF16, 'qxT')
    inT = P.sb([128, 4, 128], BF16, 'inT')
    oc = P.sb([128, 4, 128], F32, 'oc')
    sq = P.sb([128, 4, 128], F32, 'sq')
    sg = P.sb([128, 4, 128], F32, 'sg')
    st = P.sb([128, 16], F32, 'st')
    ystg = [P.sb([128, 4, 128], F32R, 'ystg') for _ in range(1)]
    c0 = OFF['rq']
    yield 'main'
    for i in range(NT):
        raw = raws[0]
        P.dma('sp', raw[:], C.proj.ap[i * 128:(i + 1) * 128, c0:c0 + 2048], writes=[raw])
        qs = raw[:, 0:512].rearrange("p (h d) -> p h d", h=4)
        ks = raw[:, 512:1024].rearrange("p (h d) -> p h d", h=4)
        vs = raw[:, 1024:1536].rearrange("p (h d) -> p h d", h=4)
        gs = raw[:, 1536:2048]
        rope_tm(P, 'dve', qr[:], qs, C.cos128, C.sin128, i, 4, 64, t1, t2, [raw], [qr])
        rope_tm(P, 'pool', kr[:], ks, C.cos128, C.sin128, i, 4, 64, t3, t4, [raw], [kr])
        CP(P, 'act', vb[:], vs, [raw], [vb])
        ACT(P, sg[:].rearrange("p a b -> p (a b)"), gs, AF.Silu, [raw], [sg])
        TT(P, 'pool', kz[:], kr[:], zeta[:, :].unsqueeze(2).broadcast_to([128, 4, 128]), ALU.mult, [kr, zeta], [kz])
        bank = P.psum[bk[0]]
        bb = bank[:].bitcast(BF16)
        for h in range(4):
            TR(P, bb[:, h * 128:(h + 1) * 128], qr[:, h, :], identb[:], [qr, identb], [bank])
        for h in range(4):
            TR(P, bb[:, (4 + h) * 128:(5 + h) * 128], kr[:, h, :], identb[:], [kr, identb], [bank])
        CP(P, 'act', qkT[:].rearrange("p a b -> p (a b)"), bb[:, :], [bank], [qkT])
        if i > 0:
            TT(P, 'dve', qxT[:], qkT[:, 0:4, :], xi[:], ALU.mult, [qkT, xi], [qxT])
        ib = P.psum[bk[1]]
        for h in range(4):
            MM(P, ib[:, h * 128:(h + 1) * 128], qkT[:, 4 + h, :], qkT[:, h, :], True, True, [qkT], [ib])
        TT(P, 'dve', inT[:].rearrange("p a b -> p (a b)"), ib[:], dm[:].rearrange("p a b -> p (a b)"), ALU.mult, [ib, dm], [inT])
        ob = P.psum[bk[2]]
        for h in range(4):
            MM(P, ob[:, h * 128:(h + 1) * 128], inT[:, h, :], vb[:, h, :], True, i == 0, [inT, vb], [ob])
            if i > 0:
                MM(P, ob[:, h * 128:(h + 1) * 128], qxT[:, h, :], Rb[:, h, :], False, True, [qxT, Rb], [ob])
        kvb = P.psum[bk[3]]
        for h in range(4):
            MM(P, kvb[:, h * 128:(h + 1) * 128], kz[:, h, :], vb[:, h, :], True, True, [kz, vb], [kvb])
        for h in range(4):
            if i == 0:
                CP(P, 'dve', R[:, h, :], kvb[:, h * 128:(h + 1) * 128], [kvb], [R])
            else:
                STT(P, R[:, h, :], R[:, h, :], float(g128[h]), kvb[:, h * 128:(h + 1) * 128], ALU.mult, ALU.add, [R, kvb], [R])
        CP(P, 'act', Rb[:], R[:], [R], [Rb])
        o3 = ob[:].rearrange("p (h e) -> p h e", h=4)
        P.op('dve', lambda e, o3=o3: e.tensor_reduce(out=st[:, 0:4], in_=o3, axis=AX.X, op=ALU.add), [ob], [st])
        TS(P, 'dve', st[:, 0:4], st[:, 0:4], 1.0 / 128, None, ALU.mult, None, [st], [st])
        TT(P, 'dve', oc[:], o3, st[:, 0:4].unsqueeze(2).broadcast_to([128, 4, 128]), ALU.subtract, [ob, st], [oc])
        TT(P, 'pool', sq[:], oc[:], oc[:], ALU.mult, [oc], [sq])
        P.op('dve', lambda e: e.tensor_reduce(out=st[:, 4:8], in_=sq[:], axis=AX.X, op=ALU.add), [sq], [st])
        ACT(P, st[:, 8:12], st[:, 4:8], AF.Sqrt, [st, C.epsT], [st], scale=1.0 / 128, bias=C.epsT[:, 0:1])
        RECIP(P, st[:, 12:16], st[:, 8:12], [st], [st])
        TT(P, 'dve', oc[:], oc[:], st[:, 12:16].unsqueeze(2).broadcast_to([128, 4, 128]), ALU.mult, [oc, st], [oc])
        TT(P, 'pool', oc[:], oc[:], sg[:], ALU.mult, [oc, sg], [oc])
        yb = P.psum[bk[0]]
        for h in range(4):
            TR(P, yb[:, h * 128:(h + 1) * 128], oc[:, h, :], C.ident[:], [oc, C.ident], [yb])
        ys = ystg[0]
        CP(P, 'act', ys[:].rearrange("p a b -> p (a b)"), yb[:], [yb], [ys])
        P.dma('pool', C.mixedT.ap[512:1024, i * 128:(i + 1) * 128].rearrange("(h e) t -> e h t", h=4), ys[:], reads=[ys])
        yield


def phase_nsa(P, C, l):
    m0 = P.mark()
    scale = 128 ** -0.5
    identb = P.sb([128, 128], BF16, 'identb')
    CP(P, 'dve', identb[:], C.ident[:], [C.ident], [identb])
    ld = P.sb([128, 128], F32, 'ld')
    tri = P.sb([128, 128], BF16, 'tri')
    P.dma('sp', ld[:], C.c_tri.ap[:, :], writes=[ld])
    CP(P, 'dve', tri[:], ld[:], [ld], [tri])
    trigt = P.sb([128, 128], BF16, 'trigt')
    ld2 = P.sb([128, 128], F32, 'ld2')
    P.dma('sp', ld2[:], C.c_trigt.ap[:, :], writes=[ld2])
    CP(P, 'dve', trigt[:], ld2[:], [ld2], [trigt])
    ovl = P.sb([128, 34], BF16, 'ovl')
    ld3 = P.sb([128, 34], F32, 'ld3')
    P.dma('sp', ld3[:], C.c_ovl.ap[:, :], writes=[ld3])
    CP(P, 'dve', ovl[:], ld3[:], [ld3], [ovl])
    eexp = P.sb([32, NT, 128], BF16, 'eexp')
    ld4 = P.sb([32, NT * 128], F32, 'ld4')
    P.dma('sp', ld4[:], C.c_eexp.ap[:, :], writes=[ld4])
    CP(P, 'dve', eexp[:].rearrange("p a b -> p (a b)"), ld4[:], [ld4], [eexp])
    selvalid = P.sb([128, NT, 32], F32, 'selvalid')
    selcb = P.sb([128, NT, 32], F32, 'selcb')
    P.dma('sp', selvalid[:], C.c_selvalid.ap.rearrange("(i p) m -> p i m", p=128), writes=[selvalid])
    P.dma('sp', selcb[:], C.c_selcb.ap.rearrange("(i p) m -> p i m", p=128), writes=[selcb])
    qT = P.sb([128, NT, 4, 128], BF16, 'qT')
    kvT = P.sb([128, 4, S], BF16, 'kvT')
    vsb = P.sb([128, NT, 130], BF16, 'vsb')
    vwb = P.sb([128, NT, 130], BF16, 'vwb')
    gates = P.sb([128, NT, 12], F32, 'gates')
    P.op('pool', lambda e: e.memset(vsb[:, :, 128:130], 1.0), [], [vsb])
    P.op('pool', lambda e: e.memset(vwb[:, :, 128:130], 1.0), [], [vwb])
    m1 = P.mark()
    raws = [P.sb([128, 1292], F32, 'raw') for _ in range(2)]
    sq = P.sb([128, 4, 128], F32, 'sq')
    st = P.sb([128, 8], F32, 'st')
    qn = P.sb([128, 4, 128], F32, 'qn')
    t1 = P.sb([128, 4, 64], F32, 't1')
    t2 = P.sb([128, 4, 64], F32, 't2')
    sq3 = P.sb([128, 3, 128], F32, 'sq3')
    st3 = P.sb([128, 8], F32, 'st3')
    kn = P.sb([128, 3, 128], F32, 'kn')
    t3 = P.sb([128, 3, 64], F32, 't3')
    t4 = P.sb([128, 3, 64], F32, 't4')
    qk = [P.sb([128, 8, 128], BF16, 'qk') for _ in range(2)]
    c0 = OFF['nq']
    for i in range(NT):
        raw = raws[i % 2]
        P.dma('sp', raw[:], C.proj.ap[i * 128:(i + 1) * 128, c0:c0 + 1292], writes=[raw])
        q_ = qk[i % 2]
        qs = raw[:, 0:512].rearrange("p (h d) -> p h d", h=4)
        rms_scale_tm(P, C, qs, 4, 128, sq, st, scale, [raw])
        TT(P, 'dve', qn[:], qs, st[:, 0:4].unsqueeze(2).broadcast_to([128, 4, 128]), ALU.mult, [raw, st], [qn])
        rope_tm(P, 'dve', q_[:, 0:4, :], qn[:], C.cos128, C.sin128, i, 4, 64, t1, t2, [qn], [q_])
        kv6 = raw[:, 512:1280].rearrange("p (a b d) -> p a b d", a=3, b=2)
        k3 = kv6[:, :, 0, :]
        v3 = kv6[:, :, 1, :]
        rms_scale_tm(P, C, k3, 3, 128, sq3, st3, 1.0, [raw])
        P.op('dve', lambda e: e.memset(st3[:, 0:1], 1.0), [], [st3])
        TT(P, 'pool', kn[:], k3, st3[:, 0:3].unsqueeze(2).broadcast_to([128, 3, 128]), ALU.mult, [raw, st3], [kn])
        rope_tm(P, 'pool', q_[:, 4:7, :], kn[:], C.cos128, C.sin128, i, 3, 64, t3, t4, [kn], [q_])
        CP(P, 'act', q_[:, 7, :], v3[:, 0, :], [raw], [q_])
        CP(P, 'act', vsb[:, i, 0:128], v3[:, 1, :], [raw], [vsb])
        CP(P, 'act', vwb[:, i, 0:128], v3[:, 2, :], [raw], [vwb])
        ACT(P, gates[:, i, :], raw[:, 1280:1292], AF.Sigmoid, [raw], [gates])
        bank = P.ps([2, 3])
        bb = bank[:].bitcast(BF16)
        for a in range(8):
            TR(P, bb[:, a * 128:(a + 1) * 128], q_[:, a, :], identb[:], [q_, identb], [bank])
        CP(P, 'act', qT[:, i, :, :].rearrange("p h t -> p (h t)"), bb[:, 0:512], [bank], [qT])
        CP(P, 'dve', kvT[:, :, i * 128:(i + 1) * 128], bb[:, 512:1024].rearrange("p (a t) -> p a t", a=4), [bank], [kvT])
    P.release(m1)
    m2 = P.mark()
    wk = P.sb([128, 32, 128], BF16, 'wk')
    wv = P.sb([128, 32, 128], BF16, 'wv')
    P.dma('pool', wk[:], C.nsa_w_cmp_k.ap[l].rearrange("(a p) o -> p a o", p=128), writes=[wk])
    P.dma('pool', wv[:], C.nsa_w_cmp_v.ap[l].rearrange("(a p) o -> p a o", p=128), writes=[wv])
    pe2 = P.sb([128, 2, 128], F32, 'pe2')
    P.op('dve', lambda e: e.memset(pe2[:], 0.0), [], [pe2])
    P.dma('sp', pe2[0:32, 0, :], C.nsa_pe_k.ap[l], reads=[pe2], writes=[pe2])
    P.dma('sp', pe2[0:32, 1, :], C.nsa_pe_v.ap[l], reads=[pe2], writes=[pe2])
    peT = P.sb([128, 2, 32], BF16, 'peT')
    bank = P.ps([2, 3])
    for a in range(2):
        TR(P, bank[:, a * 128:(a + 1) * 128], pe2[:, a, :], C.ident[:], [pe2, C.ident], [bank])
    CP(P, 'dve', peT[:], bank[:, 0:256].rearrange("p (a b) -> p a b", a=2)[:, :, 0:32], [bank], [peT])
    onesb = P.sb([1, 128], BF16, 'onesb')
    P.op('dve', lambda e: e.memset(onesb[:], 1.0), [], [onesb])
    cvec = P.sb([1, 2, 128], BF16, 'cvec')
    kcmpT = P.sb([128, 128], BF16, 'kcmpT')
    vcmp = P.sb([128, 128], BF16, 'vcmp')
    kcn = P.sb([128, 128], BF16, 'kcn')
    junk = P.sb([128, 128], F32, 'junk')
    stc = P.sb([128, 4], F32, 'stc')
    for a, (wt, src_idx) in enumerate(((wk, 0), (wv, 3))):
        cb_ = P.ps([2, 3])
        for li in range(32):
            MM(P, cb_[0:1, 0:128], peT[:, a, li:li + 1], wt[:, li, :], li == 0, li == 31, [peT, wt], [cb_])
        CP(P, 'dve', cvec[:, a, :], cb_[0:1, 0:128], [cb_], [cvec])
        kb_ = P.ps([2, 3])
        for li in range(32):
            MM(P, kb_[0:127, 0:128], kvT[:, src_idx, li:li + 16 * 126 + 1:16], wt[:, li, :], li == 0, False, [kvT, wt], [kb_])
        MM(P, kb_[0:127, 0:128], onesb[0:1, 0:127], cvec[0:1, a, :], False, True, [onesb, cvec], [kb_])
        if a == 0:
            ACT(P, junk[0:127, :], kb_[0:127, 0:128], AF.Square, [kb_], [junk, stc], accum_out=stc[0:127, 0:1])
            ACT(P, stc[0:127, 1:2], stc[0:127, 0:1], AF.Sqrt, [stc, C.epsT], [stc], scale=1.0 / 128, bias=C.epsT[0:127, 0:1])
            RECIP(P, stc[0:127, 2:3], stc[0:127, 1:2], [stc], [stc])
            P.op('dve', lambda e: e.memset(kcn[:], 0.0), [], [kcn])
            ACT(P, kcn[0:127, :], kb_[0:127, 0:128], AF.Copy, [kb_, stc, kcn], [kcn], scale=stc[0:127, 2:3])
            tb = P.ps([2, 3])
            tbb = tb[:].bitcast(BF16)
            TR(P, tbb[:, 0:128], kcn[:], identb[:], [kcn, identb], [tb])
            CP(P, 'dve', kcmpT[:], tbb[:, 0:128], [tb], [kcmpT])
        else:
            P.op('dve', lambda e: e.memset(vcmp[:], 0.0), [], [vcmp])
            CP(P, 'act', vcmp[0:127, :], kb_[0:127, 0:128], [kb_, vcmp], [vcmp])
    P.dbg('dbg_kcmpT', kcmpT, kcmpT[:], [128, 128], BF16)
    P.dbg('dbg_vcmp', vcmp, vcmp[:], [128, 128], BF16)
    P.dbg('dbg_kcT', kvT, kvT[:, 0, :], [128, S], BF16)
    cmask = [P.sb([128, 128], F32, 'cmask') for _ in range(2)]
    PT = [P.sb([128, 4, 128], BF16, 'PT') for _ in range(3)]
    msk = [P.sb([128, 128], BF16, 'msk') for _ in range(2)]
    acc = [P.sb([128, 4, 128], F32, 'acc') for _ in range(2)]
    zi = P.sb([128, 4, 34], F32, 'zi')
    wrk = P.sb([128, 4, 32], F32, 'wrk')
    imp = P.sb([128, 32], F32, 'imp')
    top8 = P.sb([128, 8], F32, 'top8')
    selm = P.sb([128, 32], BF16, 'selm')
    selT = P.sb([32, 128], BF16, 'selT')
    cf = P.sb([128, 3, 4], F32, 'cf')
    rz = P.sb([128, 3, 4], F32, 'rz')
    ostg = [P.sb([128, 4, 128], F32R, 'ostg') for _ in range(2)]
    pti = 0
    for i in range(NT):
        qTi = qT[:, i, :, :].rearrange("p h t -> p (h t)")
        ac = acc[i % 2]
        cm = cmask[i % 2]
        P.dma('sp', cm[0:127, :], C.c_cmpbase.ap[126 - 8 * i:126 - 8 * i + 127, :], writes=[cm])
        sb_ = P.ps([0, 1])
        MM(P, sb_[0:127, :], kcmpT[:, 0:127], qTi, True, True, [kcmpT, qT], [sb_])
        pt = PT[pti % 3]
        pti += 1
        ACT(P, pt[0:127].rearrange("p h t -> p (h t)"), sb_[0:127, :], AF.Exp, [sb_], [pt])
        TT(P, 'dve', pt[0:127], pt[0:127], cm[0:127, :].unsqueeze(1).broadcast_to([127, 4, 128]), ALU.mult, [pt, cm], [pt])
        ob = P.ps([2, 3])
        zb = P.ps([2, 3])
        for h in range(4):
            MM(P, ob[:, h * 128:(h + 1) * 128], pt[0:127, h, :], vcmp[0:127, :], True, True, [pt, vcmp], [ob])
        for h in range(4):
            MM(P, zb[:, h * 34:h * 34 + 34], pt[0:127, h, :], ovl[0:127, :], True, True, [pt, ovl], [zb])
        CP(P, 'dve', zi[:].rearrange("p a b -> p (a b)"), zb[:, 0:136], [zb], [zi])
        TS(P, 'dve', rz[:, 0, :], zi[:, :, 0], 1e-30, None, ALU.max, None, [zi], [rz])
        RECIP(P, rz[:, 0, :], rz[:, 0, :], [rz], [rz])
        TT(P, 'dve', wrk[:], zi[:, :, 1:33], rz[:, 0, :].unsqueeze(2).broadcast_to([128, 4, 32]), ALU.mult, [zi, rz], [wrk])
        P.op('dve', lambda e: e.tensor_reduce(out=imp[:], in_=wrk[:].rearrange("p h m -> p m h"), axis=AX.X, op=ALU.add), [wrk], [imp])
        TT(P, 'dve', cf[:, 0, :], gates[:, i, 0:12:3], rz[:, 0, :], ALU.mult, [gates, rz], [cf])
        for h in range(4):
            ACT(P, ac[:, h, :], ob[:, h * 128:(h + 1) * 128], AF.Copy, [ob, cf], [ac], scale=cf[:, 0, h:h + 1])
        TT(P, 'dve', imp[:], imp[:], selvalid[:, i, :], ALU.mult, [imp, selvalid], [imp])
        TT(P, 'dve', imp[:], imp[:], selcb[:, i, :], ALU.add, [imp, selcb], [imp])
        if i == 13:
            P.dbg('dbg_imp', imp, imp[:], [128, 32])
        P.op('dve', lambda e: e.max(out=top8[:], in_=imp[:]), [imp], [top8])
        TS(P, 'dve', selm[:], imp[:], top8[:, 7:8], None, ALU.is_ge, None, [imp, top8], [selm])
        tb = P.ps([2, 3])
        tbb = tb[:].bitcast(BF16)
        TR(P, tbb[0:32, 0:128], selm[:], identb[:], [selm, identb], [tb])
        CP(P, 'act', selT[:], tbb[0:32, 0:128], [tb], [selT])
        if i == 13:
            P.dbg('dbg_selm', selm, selm[:], [128, 32], BF16)
            P.dbg('dbg_top8', top8, top8[:], [128, 8])
        if i == 13:
            P.dbg('dbg_zi', zi, zi[:], [128, 4, 34])
            P.dbg('dbg_cf', cf, cf[:, 0, :], [128, 4])
            P.dbg('dbg_ac0', ac, ac[:], [128, 4, 128])
            P.dbg('dbg_pt', pt, pt[0:127], [127, 4, 128], BF16)
            P.dbg('dbg_cm', cm, cm[0:127, :], [127, 128])
        for br in (2, 1):
            kbs = [kb for kb in (i - 2, i - 1, i) if kb >= 0] if br == 2 else list(range(i + 1))
            A_, B_ = (P.psum[4], P.psum[5]) if br == 2 else (P.psum[6], P.psum[7])
            vt = vwb if br == 2 else vsb
            for n_, kb in enumerate(kbs):
                sb_ = P.ps([0, 1])
                MM(P, sb_[:, :], kvT[:, br, kb * 128:(kb + 1) * 128], qTi, True, True, [kvT, qT], [sb_])
                pt = PT[pti % 3]
                pti += 1
                ACT(P, pt[:].rearrange("p h t -> p (h t)"), sb_[:, :], AF.Exp, [sb_], [pt])
                if br == 2:
                    if kb == i:
                        TT(P, 'pool', pt[:], pt[:], tri[:].unsqueeze(1).broadcast_to([128, 4, 128]), ALU.mult, [pt, tri], [pt])
                    elif kb == i - 2:
                        TT(P, 'pool', pt[:], pt[:], trigt[:].unsqueeze(1).broadcast_to([128, 4, 128]), ALU.mult, [pt, trigt], [pt])
                else:
                    mb = P.ps([2, 3])
                    MM(P, mb[:, 0:128], eexp[:, kb, :], selT[:, :], True, True, [eexp, selT], [mb])
                    if kb == i:
                        mk = msk[n_ % 2]
                        TT(P, 'dve', mk[:], mb[:, 0:128], tri[:], ALU.mult, [mb, tri], [mk])
                        TT(P, 'dve', pt[:], pt[:], mk[:].unsqueeze(1).broadcast_to([128, 4, 128]), ALU.mult, [pt, mk], [pt])
                    else:
                        TT(P, 'dve', pt[:], pt[:], mb[:, 0:128].unsqueeze(1).broadcast_to([128, 4, 128]), ALU.mult, [pt, mb], [pt])
                for h in range(4):
                    bk = A_ if h < 2 else B_
                    o0 = (h % 2) * 130
                    MM(P, bk[:, o0:o0 + 129], pt[:, h, :], vt[:, kb, 0:129], (n_ == 0 and h % 2 == 0), n_ == len(kbs) - 1,
                       [pt, vt], [bk], skip_group_check=True)
            for h in range(4):
                bk = A_ if h < 2 else B_
                o0 = (h % 2) * 130
                RECIP(P, rz[:, br, h:h + 1], bk[:, o0 + 128:o0 + 129], [bk], [rz])
            TT(P, 'dve', cf[:, br, :], gates[:, i, br:12:3], rz[:, br, :], ALU.mult, [gates, rz], [cf])
            for h in range(4):
                bk = A_ if h < 2 else B_
                o0 = (h % 2) * 130
                STT(P, ac[:, h, :], bk[:, o0:o0 + 128], cf[:, br, h:h + 1], ac[:, h, :], ALU.mult, ALU.add, [bk, cf, ac], [ac])
        yb = P.ps([2, 3])
        for h in range(4):
            TR(P, yb[:, h * 128:(h + 1) * 128], ac[:, h, :], C.ident[:], [ac, C.ident], [yb])
        og = ostg[i % 2]
        CP(P, 'act', og[:].rearrange("p a b -> p (a b)"), yb[:], [yb], [og])
        P.dma('pool', C.mixedT.ap[1024:1536, i * 128:(i + 1) * 128].rearrange("(h e) t -> e h t", h=4), og[:], reads=[og])
    P.barrier()
    P.release(m0)


def phase_s5(P, C, l, banks=None):
    Lc = S5L
    NCH = S // Lc
    cosE = P.sb([128, 16, Lc], F32, 'cosE')
    sinE = P.sb([128, 16, Lc], F32, 'sinE')
    Bc = P.sb([128, 16, 2, 128], F32R, 'Bc')
    Cx = P.sb([128, 16, 2, 128], F32R, 'Cx')
    Dg = P.sb([128, 4, 128], F32R, 'Dg')
    wg = P.sb([128, 4, 512], F32R, 'wg')
    prm = P.sb([128, 16, 16], F32, 'prm')
    P.dma('pool', wg[:], C.s5_w_glu.ap[l].rearrange("(m p) c -> p m c", p=128), writes=[wg])
    LR, LI, LD, DT, A_, TH, R_, CT, ST, FR, FI, CL, SL, DEN, T0, T1 = range(16)
    m1 = P.mark()
    pad = P.sb([128, 3, 128], F32, 'pad')
    P.op('dve', lambda e: e.memset(pad[:], 0.0), [], [pad])
    P.dma('sp', pad[0:16, 0, :], C.s5_lambda_re.ap[l].rearrange("(j g) p -> j (g p)", g=2), reads=[pad], writes=[pad])
    P.dma('sp', pad[0:16, 1, :], C.s5_lambda_im.ap[l].rearrange("(j g) p -> j (g p)", g=2), reads=[pad], writes=[pad])
    ldt = P.sb([16, 2], F32, 'ldt')
    P.dma('sp', ldt[:], C.s5_log_dt.ap[l].rearrange("(j g) -> j g", g=2), writes=[ldt])
    CP(P, 'dve', pad[0:16, 2, :].rearrange("j (g p) -> j g p", g=2), ldt[:].unsqueeze(2).broadcast_to([16, 2, 64]), [ldt, pad], [pad])
    bank = P.ps(banks)
    for k in range(3):
        TR(P, bank[:, k * 128:(k + 1) * 128], pad[:, k, :], C.ident[:], [pad, C.ident], [bank])
    CP(P, 'dve', prm[:, 0:3, :], bank[:, 0:384].rearrange("p (k c) -> p k c", k=3)[:, :, 0:16], [bank], [prm])

    def pv(k):
        return prm[:, k, :]
    ACT(P, pv(DT), pv(LD), AF.Exp, [prm], [prm])
    TT(P, 'dve', pv(A_), pv(LR), pv(DT), ALU.mult, [prm], [prm])
    TT(P, 'dve', pv(TH), pv(LI), pv(DT), ALU.mult, [prm], [prm])
    ACT(P, pv(R_), pv(A_), AF.Exp, [prm], [prm])
    angs = P.sb([128, 2, 16], F32, 'angs')
    tqs = P.sb([128, 2, 16], F32, 'tqs')
    tis = P.sb([128, 2, 16], I32, 'tis')
    sins = P.sb([128, 2, 16], F32, 'sins')
    coss = P.sb([128, 2, 16], F32, 'coss')
    CP(P, 'dve', angs[:, 0, :], pv(TH), [prm], [angs])
    TS(P, 'dve', angs[:, 1, :], pv(TH), float(Lc), None, ALU.mult, None, [prm], [angs])
    range_reduce_sincos(P, angs, tqs, tis, sins, coss, None)
    CP(P, 'dve', pv(ST), sins[:, 0, :], [sins, coss], [prm])
    CP(P, 'dve', pv(SL), sins[:, 1, :], [sins, coss], [prm])
    CP(P, 'dve', pv(CT), coss[:, 0, :], [sins, coss], [prm])
    CP(P, 'dve', pv(CL), coss[:, 1, :], [sins, coss], [prm])
    TT(P, 'dve', pv(T0), pv(R_), pv(CT), ALU.mult, [prm], [prm])
    TT(P, 'dve', pv(T1), pv(R_), pv(ST), ALU.mult, [prm], [prm])
    TS(P, 'dve', pv(T0), pv(T0), -1.0, None, ALU.add, None, [prm], [prm])
    TT(P, 'dve', pv(DEN), pv(LR), pv(LR), ALU.mult, [prm], [prm])
    TT(P, 'dve', pv(FR), pv(LI), pv(LI), ALU.mult, [prm], [prm])
    TT(P, 'dve', pv(DEN), pv(DEN), pv(FR), ALU.add, [prm], [prm])
    RECIP(P, pv(DEN), pv(DEN), [prm], [prm])
    TT(P, 'dve', pv(FR), pv(T0), pv(LR), ALU.mult, [prm], [prm])
    TT(P, 'dve', pv(FI), pv(T1), pv(LI), ALU.mult, [prm], [prm])
    TT(P, 'dve', pv(FR), pv(FR), pv(FI), ALU.add, [prm], [prm])
    TT(P, 'dve', pv(FR), pv(FR), pv(DEN), ALU.mult, [prm], [prm])
    TT(P, 'dve', pv(FI), pv(T1), pv(LR), ALU.mult, [prm], [prm])
    TT(P, 'dve', pv(T1), pv(T0), pv(LI), ALU.mult, [prm], [prm])
    TT(P, 'dve', pv(FI), pv(FI), pv(T1), ALU.subtract, [prm], [prm])
    TT(P, 'dve', pv(FI), pv(FI), pv(DEN), ALU.mult, [prm], [prm])
    iota = P.sb([128, Lc], F32, 'iota')
    P.dma('sp', iota[:], C.c_iota.ap[:, :], writes=[iota])
    ang = P.sb([128, 16, Lc], F32, 'ang')
    tq = P.sb([128, 16, Lc], F32, 'tq')
    ti = P.sb([128, 16, Lc], I32, 'ti')
    for j in range(16):
        TS(P, 'dve' if j % 2 == 0 else 'pool', ang[:, j, :], iota[:], prm[:, TH, j:j + 1], None, ALU.mult, None, [iota, prm], [ang])
    range_reduce_sincos(P, ang, tq, ti, sinE, cosE, None)
    P.release(m1)
    m1 = P.mark()
    braw = P.sb([128, 2, 16, 16], F32, 'braw')
    P.dma('sp', braw[:, 0, :, :], C.s5_b_re.ap[l].rearrange("(j g) p h -> (g p) j h", g=2), writes=[braw])
    P.dma('sp', braw[:, 1, :, :], C.s5_b_im.ap[l].rearrange("(j g) p h -> (g p) j h", g=2), writes=[braw])
    bb = P.sb([128, 2, 16, 16], F32, 'bb')
    tb1 = P.sb([128, 16, 16], F32, 'tb1')
    tb2 = P.sb([128, 16, 16], F32, 'tb2')
    frb = prm[:, FR, :].unsqueeze(2).broadcast_to([128, 16, 16])
    fib = prm[:, FI, :].unsqueeze(2).broadcast_to([128, 16, 16])
    TT(P, 'dve', tb1[:], braw[:, 0], frb, ALU.mult, [braw, prm], [tb1])
    TT(P, 'dve', tb2[:], braw[:, 1], fib, ALU.mult, [braw, prm], [tb2])
    TT(P, 'dve', bb[:, 0], tb1[:], tb2[:], ALU.subtract, [tb1, tb2], [bb])
    TT(P, 'dve', tb1[:], braw[:, 1], frb, ALU.mult, [braw, prm], [tb1])
    TT(P, 'dve', tb2[:], braw[:, 0], fib, ALU.mult, [braw, prm], [tb2])
    TT(P, 'dve', bb[:, 1], tb1[:], tb2[:], ALU.add, [tb1, tb2], [bb])
    X = P.sb([128, 16, 2, 128], F32, 'X')
    P.op('pool', lambda e: e.memset(X[:], 0.0), [], [X])
    for g2 in range(2):
        for jj in range(4):
            for c in range(2):
                col = 32 * jj + 16 * g2
                CP(P, 'dve', X[g2 * 64:(g2 + 1) * 64, jj:16:4, c, col:col + 16], bb[g2 * 64:(g2 + 1) * 64, c, jj:16:4, :], [bb, X], [X])
    for j in range(16):
        bank = P.ps(banks)
        for c in range(2):
            TR(P, bank[:, c * 128:(c + 1) * 128], X[:, j, c, :], C.ident[:], [X, C.ident], [bank])
        CP(P, 'act' if j % 2 == 0 else 'dve', Bc[:, j, :, :].rearrange("p c k -> p (c k)"), bank[:, 0:256], [bank], [Bc])
    craw = P.sb([128, 4, 2, 128], F32, 'craw')
    for c, src in enumerate((C.s5_c_re, C.s5_c_im)):
        for dup in range(2):
            P.dma('sp', craw[:, :, c, dup * 64:(dup + 1) * 64], src.ap[l].rearrange("(m gl) n p -> (gl n) m p", gl=8), writes=[craw])
    P.op('pool', lambda e: e.memset(Cx[:].bitcast(F32), 0.0), [], [Cx])
    ctr = P.sb([128, 128], F32, 'ctr')
    for m in range(4):
        for c in range(2):
            bank = P.ps(banks)
            TR(P, bank[:, 0:128], craw[:, m, c, :], C.ident[:], [craw, C.ident], [bank])
            if c == 0:
                CP(P, 'act', ctr[:], bank[:, 0:128], [bank], [ctr])
            else:
                ACT(P, ctr[:], bank[:, 0:128], AF.Copy, [bank], [ctr], scale=-1.0)
            for gl in range(8):
                g2 = gl % 2
                j = 4 * m + gl // 2
                CP(P, 'dve', Cx[g2 * 64:(g2 + 1) * 64, j, c, 16 * gl:16 * gl + 16], ctr[g2 * 64:(g2 + 1) * 64, 16 * gl:16 * gl + 16], [ctr, Cx], [Cx])
    dcol = P.sb([128, 4], F32, 'dcol')
    P.dma('sp', dcol[:], C.s5_d.ap[l].rearrange("(m gl) n -> (gl n) m", gl=8), writes=[dcol], allow_slow_non_contiguous=True)
    for m in range(4):
        TS(P, 'dve', Dg[:, m, :], C.ident[:], dcol[:, m:m + 1], None, ALU.mult, None, [C.ident, dcol], [Dg])
    P.release(m1)
    uraw = [P.sb([128, 512], F32, 'uraw') for _ in range(2)]
    uT = [P.sb([128, 4, Lc], F32R, 'uT') for _ in range(1)]
    sre = P.sb([128, 16, Lc], F32R, 'sre')
    sim_ = P.sb([128, 16, Lc], F32R, 'sim')
    wk_ = [[P.sb([128, Lc], F32, 'wk') for _ in range(8)] for _ in range(4)]
    bsb = [P.sb([128, 2 * Lc], F32, 'bsb') for _ in range(1)]
    zl = P.sb([128, 2, 16], F32, 'zl')
    zin = P.sb([128, 2, 16], F32, 'zin')
    ztmp = P.sb([128, 2, 16], F32, 'ztmp')
    gT = [P.sb([128, 4, Lc], F32R, 'gT') for _ in range(1)]
    ge = [P.sb([128, Lc], F32, 'ge') for _ in range(3)]
    oT = [P.sb([128, Lc], F32R, 'oT') for _ in range(1)]
    c0 = OFF['u']
    ui = 0
    yield 'main'
    for ch in range(NCH):
        t0 = ch * Lc
        u_T = uT[0]
        for tt in range(Lc // 128):
            ur = uraw[ui % 2]
            ui += 1
            P.dma('sp', ur[:], C.proj.ap[t0 + tt * 128:t0 + (tt + 1) * 128, c0:c0 + 512], writes=[ur])
            bank = P.ps(banks)
            for m in range(4):
                TR(P, bank[:, m * 128:(m + 1) * 128], ur[:, m * 128:(m + 1) * 128], C.ident[:], [ur, C.ident], [bank])
            CP(P, 'act', u_T[:, :, tt * 128:(tt + 1) * 128], bank[:].rearrange("p (m t) -> p m t", m=4), [bank], [u_T])
        if ch > 0:
            clb, slb = prm[:, CL, :], prm[:, SL, :]
            TT(P, 'dve', ztmp[:, 0, :], zl[:, 0, :], clb, ALU.mult, [zl, prm], [ztmp])
            TT(P, 'dve', ztmp[:, 1, :], zl[:, 1, :], slb, ALU.mult, [zl, prm], [ztmp])
            TT(P, 'dve', zin[:, 0, :], ztmp[:, 0, :], ztmp[:, 1, :], ALU.subtract, [ztmp], [zin])
            TT(P, 'dve', ztmp[:, 0, :], zl[:, 0, :], slb, ALU.mult, [zl, prm], [ztmp])
            TT(P, 'dve', ztmp[:, 1, :], zl[:, 1, :], clb, ALU.mult, [zl, prm], [ztmp])
            TT(P, 'dve', zin[:, 1, :], ztmp[:, 0, :], ztmp[:, 1, :], ALU.add, [ztmp], [zin])
        def st_mm(j):
            bank = P.ps(banks)
            MM(P, bank[:, 0:Lc], Bc[:, j, 0, :], u_T[:, j // 4, :], True, True, [Bc, u_T], [bank])
            MM(P, bank[:, Lc:2 * Lc], Bc[:, j, 1, :], u_T[:, j // 4, :], True, True, [Bc, u_T], [bank])
            return bank

        def st_pre(j, e1, bre, bim, srcs):
            w = wk_[j % 4]
            cj, sj = cosE[:, j, :], sinE[:, j, :]
            TT(P, e1, w[0][:], bre, cj, ALU.mult, srcs + [cosE], [w[0]])
            TT(P, e1, w[1][:], bim, sj, ALU.mult, srcs + [sinE], [w[1]])
            TT(P, e1, w[2][:], w[0][:], w[1][:], ALU.add, [w[0], w[1]], [w[2]])
            TT(P, e1, w[3][:], bim, cj, ALU.mult, srcs + [cosE], [w[3]])
            TT(P, e1, w[4][:], bre, sj, ALU.mult, srcs + [sinE], [w[4]])
            TT(P, e1, w[5][:], w[3][:], w[4][:], ALU.subtract, [w[3], w[4]], [w[5]])

        def st_scan(j):
            w = wk_[j % 4]
            rb = prm[:, R_, j:j + 1].broadcast_to([128, Lc])
            for c_, (src, dst) in enumerate(((w[2], w[6]), (w[5], w[7]))):
                init = 0.0 if ch == 0 else zin[:, c_, j:j + 1]
                P.op('dve', lambda e, src=src, dst=dst, init=init, rb=rb: e.tensor_tensor_scan(
                    out=dst[:], data0=rb, data1=src[:], initial=init, op0=ALU.mult, op1=ALU.add),
                    [src, prm, zin], [dst])
                CP(P, 'act', zl[:, c_, j:j + 1], dst[:, Lc - 1:Lc], [dst], [zl])

        def st_post(j, e1):
            w = wk_[j % 4]
            cj, sj = cosE[:, j, :], sinE[:, j, :]
            zr, zi_ = w[6], w[7]
            TT(P, e1, w[0][:], zr[:], cj, ALU.mult, [zr, cosE], [w[0]])
            TT(P, e1, w[1][:], zi_[:], sj, ALU.mult, [zi_, sinE], [w[1]])
            TT(P, e1, w[3][:], zr[:], sj, ALU.mult, [zr, sinE], [w[3]])
            TT(P, e1, w[4][:], zi_[:], cj, ALU.mult, [zi_, cosE], [w[4]])

        def st_fin(j):
            w = wk_[j % 4]
            TT(P, 'dve', sre[:, j, :], w[0][:], w[1][:], ALU.subtract, [w[0], w[1]], [sre])
            TT(P, 'dve', sim_[:, j, :], w[3][:], w[4][:], ALU.add, [w[3], w[4]], [sim_])

        deferred = None
        for p_ in range(8):
            jo, je = 2 * p_ + 1, 2 * p_
            bko = st_mm(jo)
            bs = bsb[0]
            CP(P, 'act', bs[:], bko[:], [bko], [bs])
            st_pre(jo, 'pool', bs[:, 0:Lc], bs[:, Lc:2 * Lc], [bs])
            bke = st_mm(je)
            st_pre(je, 'dve', bke[:, 0:Lc], bke[:, Lc:2 * Lc], [bke])
            st_scan(je)
            st_post(je, 'dve')
            st_fin(je)
            if deferred is not None:
                st_fin(deferred)
            st_scan(jo)
            st_post(jo, 'pool')
            deferred = jo
            yield
        st_fin(deferred)
        g_T = gT[0]
        for m in range(4):
            bank = P.ps(banks)
            n_ = 0
            for j in range(4 * m, 4 * m + 4):
                for c_, st_ in enumerate((sre, sim_)):
                    MM(P, bank[:, 0:Lc], Cx[:, j, c_, :], st_[:, j, :], n_ == 0, False, [Cx, st_], [bank])
                    n_ += 1
            MM(P, bank[:, 0:Lc], Dg[:, m, :], u_T[:, m, :], False, True, [Dg, u_T], [bank])
            x_ = bank[:, 0:Lc]
            a, b, c_t = ge[0], ge[1], ge[2]
            ACT(P, a[:], x_, AF.Square, [bank], [a])
            TS(P, 'dve', a[:], a[:], 0.044715, 1.0, ALU.mult, ALU.add, [a], [a])
            TT(P, 'dve', b[:], a[:], x_, ALU.mult, [a, bank], [b])
            ACT(P, c_t[:], b[:], AF.Sigmoid, [b], [c_t], scale=1.5957691216057308)
            TT(P, 'dve', g_T[:, m, :], c_t[:], x_, ALU.mult, [c_t, bank], [g_T])
        for mo in range(4):
            bank = P.ps(banks)
            for m in range(4):
                MM(P, bank[:, 0:Lc], wg[:, m, mo * 128:(mo + 1) * 128], g_T[:, m, :], m == 0, m == 3, [wg, g_T], [bank])
            a = ge[mo % 3]
            ACT(P, a[:], bank[:, 0:Lc], AF.Sigmoid, [bank], [a])
            o_ = oT[0]
            TT(P, 'dve', o_[:], a[:], g_T[:, mo, :].bitcast(F32), ALU.mult, [a, g_T], [o_])
            P.dma('pool', C.mixedT.ap[mo * 128:(mo + 1) * 128, t0:t0 + Lc], o_[:], reads=[o_])
        yield


def run_alone(P, g):
    m0 = P.mark()
    for _ in g:
        pass
    P.release(m0)


def run_pair(P, gA, gB, ratio):
    m0 = P.mark()
    for g in (gA, gB):
        for v in g:
            if v == 'main':
                break
    live = {0: gA, 1: gB}
    while live:
        for k in (0, 1):
            if k not in live:
                continue
            for _ in range(ratio[k]):
                try:
                    next(live[k])
                except StopIteration:
                    del live[k]
                    break
    P.release(m0)


_CACHE = {}


def build_full(nl=L):
    nc = bass.Bass("TRN2", target_bir_lowering=False)
    P = Prog(nc)
    C = Ctx()
    setup_common(P, C, {})
    setup_rope(P, C)
    xin = C.x.ap
    for l in range(nl):
        phase_in_proj(P, C, l, xin)
        run_pair(P, phase_s5(P, C, l, [0, 1, 2, 3]), phase_ret(P, C, l, [4, 5, 6, 7]), (4, 1))
        phase_nsa(P, C, l)
        phase_diff(P, C, l)
        phase_out_proj(P, C, l, xin, C.xa.ap)
        xout = C.out.ap if l == nl - 1 else C.xb.ap
        phase_mlp(P, C, l, C.xa.ap, xout)
        xin = C.xb.ap
    P.barrier()
    P.finalize()
    return nc


def build_layer(lay):
    nc = bass.Bass("TRN2", target_bir_lowering=False)
    P = Prog(nc)
    C = Ctx()
    C.lbase = lay
    setup_common(P, C, {}, nlw=1)
    setup_rope(P, C)
    phase_in_proj(P, C, 0, C.x.ap)
    run_alone(P, phase_s5(P, C, 0))
    run_alone(P, phase_ret(P, C, 0))
    phase_nsa(P, C, 0)
    phase_diff(P, C, 0)
    phase_out_proj(P, C, 0, C.x.ap, C.xa.ap)
    phase_mlp(P, C, 0, C.xa.ap, C.out.ap)
    P.barrier()
    P.finalize()
    return nc


FUSED = True


def kernel(**inputs):
    x = np.ascontiguousarray(inputs['x'], dtype=np.float32)
    pos = np.ascontiguousarray(inputs['positions'], dtype=np.int32)
    B = x.shape[0]
    consts = host_consts()
    if FUSED:
        if 'nc' not in _CACHE:
            _CACHE['nc'] = build_full()
        nc = _CACHE['nc']
        shared = {name: np.ascontiguousarray(inputs[name], dtype=np.float32) for name, _, _ in WEIGHT_SPECS}
        shared.update(consts)
        in_maps = []
        for b in range(B):
            m = dict(shared)
            m['x'] = x[b]
            m['pos'] = pos[b]
            in_maps.append(m)
        res = run_bass_kernel_spmd(nc, in_maps, core_ids=list(range(B)))
        return np.stack([np.asarray(res.results[b]['out'], dtype=np.float32) for b in range(B)], axis=0)
    cur = [x[b] for b in range(B)]
    CPL = 4
    for lay in range(L):
        nc = build_layer(lay)
        shared = {name: np.ascontiguousarray(inputs[name][lay:lay + 1], dtype=np.float32) for name, _, _ in WEIGHT_SPECS}
        shared.update(consts)
        nxt = []
        for b0 in range(0, B, CPL):
            in_maps = []
            for b in range(b0, min(B, b0 + CPL)):
                m = dict(shared)
                m['x'] = cur[b]
                m['pos'] = pos[b]
                in_maps.append(m)
            res = run_bass_kernel_spmd(nc, in_maps, core_ids=list(range(len(in_maps))))
            nxt += [np.asarray(res.results[i]['out'], dtype=np.float32) for i in range(len(in_maps))]
        cur = nxt
    return np.stack(cur, axis=0)
```

```python
import math
import numpy as np
from contextlib import ExitStack
import concourse.bass as bass
import concourse.mybir as mybir
from concourse.bass_utils import run_bass_kernel_spmd

dt = mybir.dt
F32, BF16, F32R, I32 = dt.float32, dt.bfloat16, dt.float32r, dt.int32
AF = mybir.ActivationFunctionType
ALU = mybir.AluOpType
AX = mybir.AxisListType

S = 2048
D = 2048
L = 2
NT = S // 128
N_IN = 5388
D_FF = 8192
EPS = 1e-6
PI = math.pi
S5L = 256

ENGS = ['sp', 'act', 'dve', 'pool', 'pe']
BLK = {'sp': 'sync', 'act': 'scalar', 'dve': 'vector', 'pool': 'gpsimd', 'pe': 'tensor'}
DSIZE = {F32: 4, BF16: 2, I32: 4, F32R: 4}
N_DMA_SEM = 12
SB_BASE = 16384 + 512
SB_END = 224 * 1024

OFF = {}
_o = 0
for _n, _w in [('u', 512), ('rq', 512), ('rk', 512), ('rv', 512), ('rg', 512), ('nq', 512),
               ('nkc', 128), ('nvc', 128), ('nks', 128), ('nvs', 128), ('nkw', 128), ('nvw', 128),
               ('ngate', 12), ('dq', 512), ('dk', 512), ('dv', 512)]:
    OFF[_n] = _o
    _o += _w
assert _o == N_IN


class T:
    __slots__ = ('ap', 'w', 'rs', 'name', 'psum')

    def __init__(self, ap, name=''):
        self.ap = ap
        self.w = None
        self.rs = []
        self.name = name
        self.psum = False

    def __getitem__(self, k):
        return self.ap[k]


class Prog:
    def __init__(self, nc):
        self.nc = nc
        self.ops = {e: [] for e in ENGS}
        self.tiles = []
        self.pending = {e: set() for e in ENGS}
        self.dma_cnt = {}
        self.dma_rr = {e: 0 for e in ENGS}
        self.sb_off = SB_BASE
        self.uid = 0
        self.sb_max = 0
        self.psum = []
        self.ps_rr = 0

    def sb(self, shape, dtype=F32, name='t'):
        per = int(np.prod(shape[1:])) * DSIZE[dtype]
        per = (per + 63) // 64 * 64
        off = self.sb_off
        self.sb_off += per
        self.sb_max = max(self.sb_max, self.sb_off)
        assert self.sb_off <= SB_END, f"SBUF overflow {self.sb_off} at {name}"
        self.uid += 1
        h = self.nc.alloc_sbuf_tensor_at(f"{name}_{self.uid}", list(shape), dtype, offset=off)
        t = T(h.ap(), name)
        self.tiles.append(t)
        return t

    def mark(self):
        return self.sb_off

    def dbg(self, name, t, ap, shape, dtype=F32):
        if not getattr(self, 'debug', False):
            return
        d = self.dram(name, list(shape), dtype, kind="ExternalOutput")
        self.dma('sp', d.ap, ap, reads=[t])

    def release(self, m):
        self.barrier()
        self.sb_off = m

    def track(self, ap, name=''):
        t = T(ap, name)
        self.tiles.append(t)
        return t

    def dram(self, name, shape, dtype=F32, kind="Internal"):
        h = self.nc.dram_tensor(name, list(shape), dtype, kind=kind)
        return self.track(h.ap(), name)

    def init_psum(self):
        for i in range(8):
            h = self.nc.alloc_psum_tensor(f"psb{i}", [128, 512], F32)
            self.psum.append(self.track(h.ap(), f"ps{i}"))
            self.psum[-1].psum = True
        return self.psum

    def ps(self, group=None):
        grp = group if group is not None else list(range(8))
        self.ps_rr += 1
        return self.psum[grp[self.ps_rr % len(grp)]]

    def _deps(self, reads, writes):
        deps = set()
        for t in reads:
            if t.w is not None:
                deps.add(t.w)
            if t.psum:
                deps.update(t.rs)
        for t in writes:
            if t.w is not None:
                deps.add(t.w)
            deps.update(t.rs)
        return deps

    def op(self, eng, fn, reads=(), writes=()):
        idx = len(self.ops[eng])
        tok = ('c', eng, idx)
        deps = self._deps(reads, writes)
        deps |= self.pending[eng]
        self.pending[eng] = set()
        self.ops[eng].append(dict(fn=fn, deps=deps, kind='c', marked=False))
        for t in reads:
            t.rs = [r for r in t.rs if not (r[0] == 'c' and r[1] == eng)]
            t.rs.append(tok)
        for t in writes:
            t.w = tok
            t.rs = []
        return tok

    def dma(self, q, out_ap, in_ap, reads=(), writes=(), **kw):
        i = self.dma_rr[q]
        self.dma_rr[q] = (i + 1) % N_DMA_SEM
        key = (q, i)
        prev = self.dma_cnt.get(key, 0)
        cnt = prev + 16
        self.dma_cnt[key] = cnt
        tok = ('d', key, cnt)
        deps = self._deps(reads, writes)
        deps |= self.pending[q]
        self.pending[q] = set()
        if prev > 0:
            deps.add(('d', key, prev))
        self.ops[q].append(dict(fn=lambda e: e.dma_start(out=out_ap, in_=in_ap, **kw), deps=deps,
                                kind='d', key=key, marked=True))
        for t in reads:
            t.rs.append(tok)
        for t in writes:
            t.w = tok
            t.rs = []
        return tok

    def barrier(self):
        toks = set()
        for e in ENGS:
            for j in range(len(self.ops[e]) - 1, -1, -1):
                if self.ops[e][j]['kind'] == 'c':
                    toks.add(('c', e, j))
                    break
        for key, cnt in self.dma_cnt.items():
            toks.add(('d', key, cnt))
        for e in ENGS:
            self.pending[e] |= toks
        for t in self.tiles:
            t.w = None
            t.rs = []

    def finalize(self):
        nc = self.nc
        def skip(e, idx, d):
            return d[0] == 'c' and d[1] == e and (e == 'pe' or e == 'sp' or idx - d[2] > 12)
        self._skip = skip
        for e in ENGS:
            for idx, o in enumerate(self.ops[e]):
                for d in o['deps']:
                    if d[0] == 'c' and not skip(e, idx, d):
                        self.ops[d[1]][d[2]]['marked'] = True
            for d in self.pending[e]:
                if d[0] == 'c' and not skip(e, 10 ** 9, d):
                    self.ops[d[1]][d[2]]['marked'] = True
        for e in ENGS:
            c = 0
            for o in self.ops[e]:
                if o['kind'] == 'c' and o['marked']:
                    c += 1
                    o['inc'] = c
        self.nwait = 0
        with ExitStack() as st:
            esem = {e: st.enter_context(nc.semaphore(f"s_{e}")) for e in ENGS}
            dsem = {}
            for key in self.dma_cnt:
                dsem[key] = st.enter_context(nc.semaphore(f"d_{key[0]}_{key[1]}"))
            block = st.enter_context(nc.Block())

            def resolve(d):
                if d[0] == 'c':
                    return esem[d[1]], self.ops[d[1]][d[2]]['inc'], d[1]
                return dsem[d[1]], d[2], d[1]

            def replay(e, eng):
                seen = {}
                ops = self.ops[e]

                def do_waits(deps, idx):
                    need = {}
                    for d in deps:
                        if skip(e, idx, d):
                            continue
                        sem, val, key = resolve(d)
                        k = (d[0], key)
                        if val > need.get(k, (None, 0))[1]:
                            need[k] = (sem, val)
                    for k, (sem, val) in need.items():
                        if seen.get(k, 0) >= val:
                            continue
                        seen[k] = val
                        eng.wait_ge(sem, val)
                        self.nwait += 1

                for idx, o in enumerate(ops):
                    do_waits(o['deps'], idx)
                    ins = o['fn'](eng)
                    if o['kind'] == 'd':
                        ins.then_inc(dsem[o['key']], 16)
                    elif o['marked']:
                        ins.then_inc(esem[e], 1)
                do_waits(self.pending[e], 10 ** 9)

            for e in ENGS:
                getattr(block, BLK[e])(lambda eng, e=e: replay(e, eng))
        return nc


def ACT(P, out, in_, func, r, w, **kw):
    return P.op('act', lambda e: e.activation(out=out, in_=in_, func=func, **kw), r, w)


def TT(P, eng, out, in0, in1, op, r, w):
    return P.op(eng, lambda e: e.tensor_tensor(out=out, in0=in0, in1=in1, op=op), r, w)


def TS(P, eng, out, in0, s1, s2, op0, op1, r, w, **kw):
    if op1 is None:
        return P.op(eng, lambda e: e.tensor_scalar(out=out, in0=in0, scalar1=s1, scalar2=None, op0=op0, **kw), r, w)
    return P.op(eng, lambda e: e.tensor_scalar(out=out, in0=in0, scalar1=s1, scalar2=s2, op0=op0, op1=op1, **kw), r, w)


def STT(P, out, in0, scalar, in1, op0, op1, r, w):
    return P.op('dve', lambda e: e.scalar_tensor_tensor(out=out, in0=in0, scalar=scalar, in1=in1, op0=op0, op1=op1), r, w)


def MM(P, out, lhsT, rhs, start, stop, r, w, **kw):
    return P.op('pe', lambda e: e.matmul(out, lhsT=lhsT, rhs=rhs, start=start, stop=stop, **kw), r, w)


def TR(P, out, in_, ident, r, w):
    return P.op('pe', lambda e: e.transpose(out=out, in_=in_, identity=ident), r, w)


def CP(P, eng, out, in_, r, w):
    if eng == 'act':
        return P.op('act', lambda e: e.activation(out=out, in_=in_, func=AF.Copy), r, w)
    return P.op(eng, lambda e: e.tensor_copy(out=out, in_=in_), r, w)


def RECIP(P, out, in_, r, w):
    return P.op('dve', lambda e: e.reciprocal(out=out, in_=in_), r, w)


class Ctx:
    pass


def norm_transpose_tile(P, C, xt, h, st, gbc, hT_t, dst_fn, evac_rr):
    ACT(P, h[:], xt[:], AF.Square, [xt], [h, st], accum_out=st[:, 0:1])
    ACT(P, st[:, 1:2], st[:, 0:1], AF.Sqrt, [st, C.epsT], [st], scale=1.0 / D, bias=C.epsT[:, 0:1])
    RECIP(P, st[:, 2:3], st[:, 1:2], [st], [st])
    STT(P, h[:], xt[:], st[:, 2:3], gbc[:], ALU.mult, ALU.mult, [xt, st, gbc, h], [h])
    for kq in range(4):
        bank = P.ps()
        for j in range(4):
            k = kq * 4 + j
            TR(P, bank[:, j * 128:(j + 1) * 128], h[:, k * 128:(k + 1) * 128], C.ident[:], [h, C.ident], [bank])
        eng = 'act' if (evac_rr + kq) % 2 == 0 else 'dve'
        CP(P, eng, dst_fn(kq), bank[:].rearrange("p (a b) -> p a b", a=4), [bank], [hT_t])


def phase_in_proj(P, C, l, xin):
    m0 = P.mark()
    gbc = P.sb([128, D], F32, 'gbc')
    P.dma('act', gbc[:], C.norm1_g.ap[l:l + 1, :].broadcast_to([128, D]), writes=[gbc])
    xts = [P.sb([128, D], F32, 'xt') for _ in range(2)]
    hs = [P.sb([128, D], F32, 'h') for _ in range(2)]
    sts = [P.sb([128, 4], F32, 'st') for _ in range(2)]
    hT = [P.sb([128, 16, 128], F32R, 'hT') for _ in range(8)]
    wb = [P.sb([128, 16, 512], F32R, 'wb') for _ in range(2)]
    stg = [P.sb([128, 512], F32, 'stg') for _ in range(4)]
    nblk = (N_IN + 511) // 512
    wi = 0
    si = 0
    for th in range(2):
        for i in range(8):
            tok0 = th * 1024 + i * 128
            xt, h, st = xts[i % 2], hs[i % 2], sts[i % 2]
            P.dma('sp', xt[:], xin[tok0:tok0 + 128, :], writes=[xt])
            norm_transpose_tile(P, C, xt, h, st, gbc, hT[i], (lambda kq, t=hT[i]: t[:, kq * 4:(kq + 1) * 4, :]), i)
        for cb in range(nblk):
            c0 = cb * 512
            nc_ = min(512, N_IN - c0)
            w = wb[wi % 2]
            wi += 1
            P.dma('pool', w[:, :, :nc_], C.w_in.ap[l, :, c0:c0 + nc_].rearrange("(k p) c -> p k c", p=128), writes=[w])
            for i in range(8):
                tok0 = th * 1024 + i * 128
                bank = P.ps()
                for k in range(16):
                    MM(P, bank[:, :nc_], hT[i][:, k, :], w[:, k, :nc_], k == 0, k == 15, [hT[i], w], [bank])
                sg = stg[si % 4]
                CP(P, 'act' if si % 2 == 0 else 'dve', sg[:, :nc_], bank[:, :nc_], [bank], [sg])
                P.dma('sp' if si % 2 == 0 else 'act', C.proj.ap[tok0:tok0 + 128, c0:c0 + nc_], sg[:, :nc_], reads=[sg])
                si += 1
    P.barrier()
    P.release(m0)


def phase_out_proj(P, C, l, xin, xout):
    m0 = P.mark()
    mT = [P.sb([128, 16, 128], F32R, 'mT') for _ in range(8)]
    wb = [P.sb([128, 16, 512], F32R, 'wb') for _ in range(2)]
    xb = [P.sb([128, 512], F32, 'xb') for _ in range(4)]
    wi = 0
    si = 0
    for th in range(2):
        for i in range(8):
            tok0 = th * 1024 + i * 128
            P.dma('pool', mT[i][:], C.mixedT.ap[:, tok0:tok0 + 128].rearrange("(k p) t -> p k t", p=128), writes=[mT[i]])
        for cb in range(4):
            c0 = cb * 512
            w = wb[wi % 2]
            wi += 1
            P.dma('pool', w[:], C.w_out.ap[l, :, c0:c0 + 512].rearrange("(k p) c -> p k c", p=128), writes=[w])
            for i in range(8):
                tok0 = th * 1024 + i * 128
                x_ = xb[si % 4]
                P.dma('sp', x_[:], xin[tok0:tok0 + 128, c0:c0 + 512], writes=[x_])
                bank = P.ps()
                for k in range(16):
                    MM(P, bank[:], mT[i][:, k, :], w[:, k, :], k == 0, k == 15, [mT[i], w], [bank])
                TT(P, 'dve', x_[:], x_[:], bank[:], ALU.add, [x_, bank], [x_])
                P.dma('act', xout[tok0:tok0 + 128, c0:c0 + 512], x_[:], reads=[x_])
                si += 1
    P.barrier()
    P.release(m0)


def phase_mlp(P, C, l, xin, xout):
    m0 = P.mark()
    gbc = P.sb([128, D], F32, 'gbc')
    P.dma('act', gbc[:], C.norm2_g.ap[l:l + 1, :].broadcast_to([128, D]), writes=[gbc])
    xacc = [P.sb([128, D], F32, 'xacc') for _ in range(4)]
    h = P.sb([128, D], F32, 'h')
    st = P.sb([128, 4], F32, 'st')
    h2T = P.sb([128, 16, 512], F32R, 'h2T')
    aT = [P.sb([128, 512], F32R, 'aT') for _ in range(16)]
    wb = [P.sb([128, 16, 512], F32R, 'wb') for _ in range(2)]
    rl = [P.sb([128, 512], F32, 'rl') for _ in range(2)]
    wi = 0
    ri = 0
    for tt4 in range(4):
        t0 = tt4 * 512
        for j in range(4):
            P.dma('sp', xacc[j][:], xin[t0 + j * 128:t0 + (j + 1) * 128, :], writes=[xacc[j]])
            norm_transpose_tile(P, C, xacc[j], h, st, gbc, h2T,
                                (lambda kq, j=j: h2T[:, kq * 4:(kq + 1) * 4, j * 128:(j + 1) * 128]), j)
        for q in range(4):
            for cbw in range(4):
                c0 = q * 2048 + cbw * 512
                w = wb[wi % 2]
                wi += 1
                P.dma('pool', w[:], C.mlp_w1.ap[l, :, c0:c0 + 512].rearrange("(k p) c -> p k c", p=128), writes=[w])
                for c in range(4):
                    bank = P.ps()
                    a = aT[cbw * 4 + c]
                    for k in range(16):
                        MM(P, bank[:], w[:, k, c * 128:(c + 1) * 128], h2T[:, k, :], k == 0, k == 15, [w, h2T], [bank])
                    r_ = rl[ri % 2]
                    ri += 1
                    ACT(P, r_[:], bank[:], AF.Relu, [bank], [r_])
                    TT(P, 'dve', a[:], r_[:], r_[:], ALU.mult, [r_], [a])
            for cb in range(4):
                c0 = cb * 512
                w = wb[wi % 2]
                wi += 1
                P.dma('pool', w[:], C.mlp_w2.ap[l, q * 2048:(q + 1) * 2048, c0:c0 + 512].rearrange("(k p) c -> p k c", p=128), writes=[w])
                for j in range(4):
                    bank = P.ps()
                    for k in range(16):
                        MM(P, bank[:], aT[k][:, j * 128:(j + 1) * 128], w[:, k, :], k == 0, k == 15, [aT[k], w], [bank])
                    TT(P, 'dve', xacc[j][:, c0:c0 + 512], xacc[j][:, c0:c0 + 512], bank[:], ALU.add, [xacc[j], bank], [xacc[j]])
        for j in range(4):
            P.dma('act', xout[t0 + j * 128:t0 + (j + 1) * 128, :], xacc[j][:], reads=[xacc[j]])
    P.barrier()
    P.release(m0)


WEIGHT_SPECS = [
    ('norm1_g', [L, D], F32), ('w_in', [L, D, N_IN], F32R),
    ('s5_lambda_re', [L, 32, 64], F32), ('s5_lambda_im', [L, 32, 64], F32), ('s5_log_dt', [L, 32], F32),
    ('s5_b_re', [L, 32, 64, 16], F32), ('s5_b_im', [L, 32, 64, 16], F32),
    ('s5_c_re', [L, 32, 16, 64], F32), ('s5_c_im', [L, 32, 16, 64], F32),
    ('s5_d', [L, 32, 16], F32), ('s5_w_glu', [L, 512, 512], F32R),
    ('nsa_pe_k', [L, 32, 128], F32), ('nsa_pe_v', [L, 32, 128], F32),
    ('nsa_w_cmp_k', [L, 4096, 128], F32), ('nsa_w_cmp_v', [L, 4096, 128], F32),
    ('diff_lq1', [L, 64], F32), ('diff_lk1', [L, 64], F32), ('diff_lq2', [L, 64], F32), ('diff_lk2', [L, 64], F32),
    ('w_out', [L, D, D], F32R), ('norm2_g', [L, D], F32),
    ('mlp_w1', [L, D, D_FF], F32R), ('mlp_w2', [L, D_FF, D], F32R),
]


def host_consts():
    c = {}
    c['c_ident'] = np.eye(128, dtype=np.float32)
    kk = np.arange(128)
    c['c_tri'] = (kk[:, None] <= kk[None, :]).astype(np.float32)
    c['c_trigt'] = (kk[:, None] > kk[None, :]).astype(np.float32)
    inv128 = (10000.0 ** (-np.arange(0, 128, 2, dtype=np.float32) / np.float32(128))).astype(np.float32)
    inv64 = (10000.0 ** (-np.arange(0, 64, 2, dtype=np.float32) / np.float32(64))).astype(np.float32)
    gam = 1.0 - 2.0 ** (-5.0 - np.arange(4, dtype=np.float64))
    sc = 128.0 ** -0.5
    dm = np.zeros((128, 4, 128), np.float64)
    for h in range(4):
        df = kk[None, :] - kk[:, None]
        dm[:, h, :] = np.where(df >= 0, gam[h] ** np.maximum(df, 0), 0.0) * sc
    c['c_ret_dm'] = dm.reshape(128, 512).astype(np.float32)
    c['c_ret_zeta'] = (gam[None, :] ** (127.0 - kk[:, None]) * sc).astype(np.float32)
    xi = gam[:, None] ** (kk[None, :] + 1.0)
    c['c_ret_xi'] = np.tile(xi.reshape(1, 512), (128, 1)).astype(np.float32)
    jp = np.arange(-126, 130)
    c['c_cmpbase'] = ((16 * jp[:, None] + 31) <= kk[None, :]).astype(np.float32)
    n_cmp = 127
    cmp_start = np.arange(n_cmp) * 16
    sel_start = np.arange(32) * 64
    ovl = ((cmp_start[None, :] < sel_start[:, None] + 64) & (cmp_start[None, :] + 32 > sel_start[:, None])).astype(np.float32)
    oz = np.zeros((128, 34), np.float32)
    oz[:127, 0] = 1.0
    oz[:127, 1:33] = ovl.T
    c['c_ovl'] = oz
    t = np.arange(S)
    cur = t // 64
    jj = np.arange(32)
    valid = (jj[None, :] <= cur[:, None])
    forced = (jj[None, :] == 0) | (jj[None, :] == cur[:, None]) | (jj[None, :] == cur[:, None] - 1)
    c['c_selvalid'] = valid.astype(np.float32)
    c['c_selcb'] = np.where(valid, np.where(forced, 1e4, 0.0), -1e30).astype(np.float32)
    ee = np.zeros((32, NT, 128), np.float32)
    for kb in range(NT):
        for k in range(128):
            ee[2 * kb + k // 64, kb, k] = 1.0
    c['c_eexp'] = ee.reshape(32, NT * 128)
    c['c_iota'] = np.tile(np.arange(S5L, dtype=np.float32)[None, :], (128, 1))
    c['c_inv128'] = np.tile(inv128[None, :], (128, 1)).astype(np.float32)
    c['c_inv64'] = np.tile(inv64[None, :], (128, 1)).astype(np.float32)
    return c


def setup_common(P, C, dbg, nlw=L):
    def kind_of(name, default="Internal"):
        return dbg.get(name, default)
    C.x = P.dram("x", [S, D], F32, kind="ExternalInput")
    C.pos = P.dram("pos", [S], I32, kind="ExternalInput")
    for name, shape, dtp in WEIGHT_SPECS:
        setattr(C, name, P.dram(name, [nlw] + list(shape[1:]), dtp, kind="ExternalInput"))
    for name, arr in host_consts().items():
        setattr(C, name, P.dram(name, list(arr.shape), F32, kind="ExternalInput"))
    C.out = P.dram("out", [S, D], F32, kind="ExternalOutput")
    C.proj = P.dram("proj", [S, N_IN], F32, kind=kind_of("proj"))
    C.mixedT = P.dram("mixedT", [D, S], F32R, kind=kind_of("mixedT"))
    C.xa = P.dram("xa", [S, D], F32, kind=kind_of("xa"))
    C.xb = P.dram("xb", [S, D], F32, kind=kind_of("xb"))
    P.init_psum()
    C.ident = P.sb([128, 128], F32, 'ident')
    P.dma('sp', C.ident[:], C.c_ident.ap[:, :], writes=[C.ident])
    C.epsT = P.sb([128, 1], F32, 'epsT')
    P.op('dve', lambda e: e.memset(C.epsT[:], EPS), [], [C.epsT])
    P.barrier()


def setup_rope(P, C):
    C.cos128 = P.sb([128, NT, 64], F32, 'cos128')
    C.sin128 = P.sb([128, NT, 64], F32, 'sin128')
    C.cos64 = P.sb([128, NT, 32], F32, 'cos64')
    C.sin64 = P.sb([128, NT, 32], F32, 'sin64')
    m0 = P.mark()
    posi = P.sb([128, NT], I32, 'posi')
    posf = P.sb([128, NT], F32, 'posf')
    P.dma('sp', posi[:], C.pos.ap.rearrange("(i p) -> p i", p=128), writes=[posi], allow_slow_non_contiguous=True)
    CP(P, 'dve', posf[:], posi[:], [posi], [posf])
    for (half, cinv, cosT, sinT) in ((64, C.c_inv128, C.cos128, C.sin128), (32, C.c_inv64, C.cos64, C.sin64)):
        inv = P.sb([128, half], F32, 'inv')
        P.dma('sp', inv[:], cinv.ap[:, :], writes=[inv])
        ang = P.sb([128, NT, half], F32, 'ang')
        tq = P.sb([128, NT, half], F32, 'tq')
        ti = P.sb([128, NT, half], I32, 'ti')
        TT(P, 'dve', ang[:], posf[:].unsqueeze(2).broadcast_to([128, NT, half]),
           inv[:].unsqueeze(1).broadcast_to([128, NT, half]), ALU.mult, [posf, inv], [ang])
        range_reduce_sincos(P, ang, tq, ti, sinT, cosT, [128, NT * half])
    P.barrier()
    P.release(m0)


def range_reduce_sincos(P, ang, tq, ti, sinT, cosT, shape2):
    def f(t):
        a = t[:]
        if len(a.shape) == 3:
            a = a.rearrange("p a b -> p (a b)")
        return a
    A, Q, I_ = f(ang), f(tq), f(ti)
    TS(P, 'dve', Q, A, 1.0 / (2 * PI), None, ALU.mult, None, [ang], [tq])
    CP(P, 'dve', I_, Q, [tq], [ti])
    CP(P, 'dve', Q, I_, [ti], [tq])
    STT(P, A, Q, -2 * PI, A, ALU.mult, ALU.add, [tq, ang], [ang])
    def wrap(X, xt, up=True, down=True):
        if up:
            TS(P, 'dve', I_.bitcast(F32), X, PI, -2 * PI, ALU.is_gt, ALU.mult, [xt], [ti])
            TT(P, 'dve', X, X, I_.bitcast(F32), ALU.add, [xt, ti], [xt])
        if down:
            TS(P, 'dve', I_.bitcast(F32), X, -PI, 2 * PI, ALU.is_lt, ALU.mult, [xt], [ti])
            TT(P, 'dve', X, X, I_.bitcast(F32), ALU.add, [xt, ti], [xt])
    wrap(A, ang)
    TS(P, 'dve', Q, A, PI / 2, None, ALU.add, None, [ang], [tq])
    wrap(Q, tq, down=False)
    TS(P, 'dve', A, A, PI, -PI, ALU.min, ALU.max, [ang], [ang])
    TS(P, 'dve', Q, Q, PI, -PI, ALU.min, ALU.max, [tq], [tq])
    ACT(P, f(sinT), A, AF.Sin, [ang], [sinT])
    ACT(P, f(cosT), Q, AF.Sin, [tq], [cosT])


def rope_tm(P, eng, dst, src, cosT, sinT, i, H, half, t1, t2, r, w):
    cb = cosT[:, i, :].unsqueeze(1).broadcast_to([128, H, half])
    sb_ = sinT[:, i, :].unsqueeze(1).broadcast_to([128, H, half])
    x1, x2 = src[:, :, :half], src[:, :, half:]
    d1, d2 = dst[:, :, :half], dst[:, :, half:]
    TT(P, eng, t1[:], x1, cb, ALU.mult, r + [cosT], [t1])
    TT(P, eng, t2[:], x2, sb_, ALU.mult, r + [sinT], [t2])
    TT(P, eng, d1, t1[:], t2[:], ALU.subtract, [t1, t2], w)
    TT(P, eng, t1[:], x2, cb, ALU.mult, r + [cosT], [t1])
    TT(P, eng, t2[:], x1, sb_, ALU.mult, r + [sinT], [t2])
    TT(P, eng, d2, t1[:], t2[:], ALU.add, [t1, t2], w)


def rms_scale_tm(P, C, src, H, dh, sq, st, scale, r):
    TT(P, 'pool', sq[:], src, src, ALU.mult, r, [sq])
    P.op('dve', lambda e: e.tensor_reduce(out=st[:, H:2 * H], in_=sq[:], axis=AX.X, op=ALU.add), [sq], [st])
    ACT(P, st[:, H:2 * H], st[:, H:2 * H], AF.Sqrt, [st, C.epsT], [st], scale=1.0 / dh, bias=C.epsT[:, 0:1])
    RECIP(P, st[:, 0:H], st[:, H:2 * H], [st], [st])
    if scale != 1.0:
        TS(P, 'dve', st[:, 0:H], st[:, 0:H], float(scale), None, ALU.mult, None, [st], [st])


def phase_diff(P, C, l):
    m0 = P.mark()
    lam_init = 0.8 - 0.6 * math.exp(-0.3 * (l + getattr(C, 'lbase', 0)))
    qT = P.sb([128, 4, S], BF16, 'qT')
    kT = P.sb([128, 4, S], BF16, 'kT')
    V1 = [P.sb([128, 4, 130], BF16, 'V1') for _ in range(NT)]
    tri = P.sb([128, 128], BF16, 'tri')
    trif = P.sb([128, 128], F32, 'trif')
    P.dma('sp', trif[:], C.c_tri.ap[:, :], writes=[trif])
    CP(P, 'dve', tri[:], trif[:], [trif], [tri])
    identb = P.sb([128, 128], BF16, 'identb')
    CP(P, 'dve', identb[:], C.ident[:], [C.ident], [identb])
    lam = P.sb([128, 8], F32, 'lam')
    lqk = P.sb([128, 4, 64], F32, 'lqk')
    for j, nm in enumerate(['diff_lq1', 'diff_lk1', 'diff_lq2', 'diff_lk2']):
        P.dma('sp', lqk[:, j, :], getattr(C, nm).ap[l:l + 1, :].broadcast_to([128, 64]), writes=[lqk])
    lp = P.sb([128, 2, 64], F32, 'lp')
    TT(P, 'dve', lp[:, 0, :], lqk[:, 0, :], lqk[:, 1, :], ALU.mult, [lqk], [lp])
    TT(P, 'dve', lp[:, 1, :], lqk[:, 2, :], lqk[:, 3, :], ALU.mult, [lqk], [lp])
    P.op('dve', lambda e: e.tensor_reduce(out=lam[:, 0:2], in_=lp[:], axis=AX.X, op=ALU.add), [lp], [lam])
    ACT(P, lam[:, 2:4], lam[:, 0:2], AF.Exp, [lam], [lam])
    TT(P, 'dve', lam[:, 4:5], lam[:, 3:4], lam[:, 2:3], ALU.subtract, [lam], [lam])
    TS(P, 'dve', lam[:, 5:6], lam[:, 4:5], -lam_init, None, ALU.add, None, [lam], [lam])
    neglam = lam[:, 5:6]
    m1 = P.mark()
    raws = [P.sb([128, 1536], F32, 'raw') for _ in range(2)]
    sqs = [P.sb([128, 8, 64], F32, 'sq') for _ in range(2)]
    sts = [P.sb([128, 16], F32, 'st') for _ in range(2)]
    qns = [P.sb([128, 8, 64], F32, 'qn') for _ in range(2)]
    t1s = [P.sb([128, 8, 32], F32, 't1') for _ in range(2)]
    t2s = [P.sb([128, 8, 32], F32, 't2') for _ in range(2)]
    qr = [P.sb([128, 8, 64], BF16, 'qr') for _ in range(2)]
    c0 = OFF['dq']
    for i in range(NT):
        raw = raws[i % 2]
        P.dma('sp', raw[:], C.proj.ap[i * 128:(i + 1) * 128, c0:c0 + 1536], writes=[raw])
        for which, dstT, scale in ((0, qT, 64 ** -0.5), (1, kT, 1.0)):
            src = raw[:, which * 512:(which + 1) * 512].rearrange("p (h d) -> p h d", h=8)
            sq, st, qn, t1, t2 = sqs[which], sts[which], qns[which], t1s[which], t2s[which]
            rms_scale_tm(P, C, src, 8, 64, sq, st, scale, [raw])
            TT(P, 'dve', qn[:], src, st[:, 0:8].unsqueeze(2).broadcast_to([128, 8, 64]), ALU.mult, [raw, st], [qn])
            q_ = qr[which]
            rope_tm(P, 'dve' if which == 0 else 'pool', q_[:], qn[:], C.cos64, C.sin64, i, 8, 32, t1, t2, [qn], [q_])
            bank = P.ps([6, 7])
            bb = bank[:].bitcast(BF16)
            for h in range(4):
                TR(P, bb[:, h * 128:(h + 1) * 128], q_[:, 2 * h:2 * h + 2, :].rearrange("p a b -> p (a b)"), identb[:], [q_, identb], [bank])
            CP(P, 'act', dstT[:, :, i * 128:(i + 1) * 128], bb[:, 0:512].rearrange("p (h t) -> p h t", h=4), [bank], [dstT])
        v1 = V1[i]
        CP(P, 'act', v1[:, :, 0:128], raw[:, 1024:1536].rearrange("p (h d) -> p h d", h=4), [raw], [v1])
        P.op('pool', lambda e, v1=v1: e.memset(v1[:, :, 128:129], 1.0), [], [v1])
    P.release(m1)
    PT = [P.sb([128, 512], BF16, 'PT') for _ in range(3)]
    o1 = [P.sb([128, 128], F32, 'o1') for _ in range(4)]
    dd = [P.sb([128, 128], F32, 'dd') for _ in range(2)]
    junk = P.sb([128, 128], F32, 'junk')
    es = [P.sb([128, 8], F32, 'es') for _ in range(2)]
    ostg = [P.sb([128, 128], F32R, 'ostg') for _ in range(2)]
    pti = 0
    ei = 0
    for h in range(4):
        for Q in range(4):
            for c in range(2):
                O = [P.psum[2 + j] for j in range(4)]
                nkb = 4 * Q + 4
                for kb in range(nkb):
                    jmin = max(0, kb - 4 * Q)
                    q0 = jmin * 128
                    sbk = P.ps([0, 1])
                    MM(P, sbk[:, q0:512], kT[c * 64:(c + 1) * 64, h, kb * 128:(kb + 1) * 128],
                       qT[c * 64:(c + 1) * 64, h, Q * 512 + q0:(Q + 1) * 512], True, True, [kT, qT], [sbk])
                    pt = PT[pti % 3]
                    pti += 1
                    ACT(P, pt[:, q0:512], sbk[:, q0:512], AF.Exp, [sbk], [pt])
                    if kb >= 4 * Q:
                        TT(P, 'dve', pt[:, q0:q0 + 128], pt[:, q0:q0 + 128], tri[:], ALU.mult, [pt, tri], [pt])
                    for j in range(jmin, 4):
                        MM(P, O[j][:, 0:129], pt[:, j * 128:(j + 1) * 128], V1[kb][:, h, 0:129],
                           kb == 0, kb == 4 * Q + j, [pt, V1[kb]], [O[j]])
                for j in range(4):
                    e_ = es[ei % 2]
                    ei += 1
                    RECIP(P, e_[:, 0:1], O[j][:, 128:129], [O[j]], [e_])
                    if c == 0:
                        ACT(P, o1[j][:], O[j][:, 0:128], AF.Copy, [O[j], e_], [o1[j]], scale=e_[:, 0:1])
                    else:
                        d_ = dd[j % 2]
                        TT(P, 'dve', e_[:, 1:2], e_[:, 0:1], neglam, ALU.mult, [e_, lam], [e_])
                        STT(P, d_[:], O[j][:, 0:128], e_[:, 1:2], o1[j][:], ALU.mult, ALU.add, [O[j], e_, o1[j]], [d_])
                        ACT(P, junk[:], d_[:], AF.Square, [d_], [junk, e_], accum_out=e_[:, 2:3])
                        ACT(P, e_[:, 3:4], e_[:, 2:3], AF.Sqrt, [e_, C.epsT], [e_], scale=1.0 / 128, bias=C.epsT[:, 0:1])
                        RECIP(P, e_[:, 4:5], e_[:, 3:4], [e_], [e_])
                        TS(P, 'dve', d_[:], d_[:], e_[:, 4:5], 1.0 - lam_init, ALU.mult, ALU.mult, [d_, e_], [d_])
                        bank = P.ps([6, 7])
                        TR(P, bank[:, 0:128], d_[:], C.ident[:], [d_, C.ident], [bank])
                        og = ostg[j % 2]
                        CP(P, 'act', og[:], bank[:, 0:128], [bank], [og])
                        tok0 = Q * 512 + j * 128
                        P.dma('pool', C.mixedT.ap[1536 + h * 128:1536 + (h + 1) * 128, tok0:tok0 + 128], og[:], reads=[og])
    P.barrier()
    P.release(m0)


def phase_ret(P, C, l, banks=None):
    bk = banks if banks is not None else [0, 2, 4, 6]
    gam = [1.0 - 2.0 ** (-5.0 - h) for h in range(4)]
    g128 = [g ** 128 for g in gam]
    identb = P.sb([128, 128], BF16, 'identb')
    CP(P, 'dve', identb[:], C.ident[:], [C.ident], [identb])
    dm = P.sb([128, 4, 128], F32, 'dm')
    P.dma('sp', dm[:].rearrange("p a b -> p (a b)"), C.c_ret_dm.ap[:, :], writes=[dm])
    zeta = P.sb([128, 4], F32, 'zeta')
    P.dma('sp', zeta[:], C.c_ret_zeta.ap[:, :], writes=[zeta])
    xi = P.sb([128, 4, 128], F32, 'xi')
    P.dma('sp', xi[:].rearrange("p a b -> p (a b)"), C.c_ret_xi.ap[:, :], writes=[xi])
    R = P.sb([128, 4, 128], F32, 'R')
    Rb = P.sb([128, 4, 128], BF16, 'Rb')
    raws = [P.sb([128, 2048], F32, 'raw') for _ in range(1)]
    t1 = P.sb([128, 4, 64], F32, 't1')
    t2 = P.sb([128, 4, 64], F32, 't2')
    t3 = P.sb([128, 4, 64], F32, 't3')
    t4 = P.sb([128, 4, 64], F32, 't4')
    qr = P.sb([128, 4, 128], BF16, 'qr')
    kr = P.sb([128, 4, 128], BF16, 'kr')
    kz = P.sb([128, 4, 128], BF16, 'kz')
    vb = P.sb([128, 4, 128], BF16, 'vb')
    qkT = P.sb([128, 8, 128], BF16, 'qkT')
    qxT = P.sb([128, 4, 128], BF16, 'qxT')
    inT = P.sb([128, 4, 128], BF16, 'inT')
    oc = P.sb([128, 4, 128], F32, 'oc')
    sq = P.sb([128, 4, 128], F32, 'sq')
    sg = P.sb([128, 4, 128], F32, 'sg')
    st = P.sb([128, 16], F32, 'st')
    ystg = [P.sb([128, 4, 128], F32R, 'ystg') for _ in range(1)]
    c0 = OFF['rq']
    yield 'main'
    for i in range(NT):
        raw = raws[0]
        P.dma('sp', raw[:], C.proj.ap[i * 128:(i + 1) * 128, c0:c0 + 2048], writes=[raw])
        qs = raw[:, 0:512].rearrange("p (h d) -> p h d", h=4)
        ks = raw[:, 512:1024].rearrange("p (h d) -> p h d", h=4)
        vs = raw[:, 1024:1536].rearrange("p (h d) -> p h d", h=4)
        gs = raw[:, 1536:2048]
        rope_tm(P, 'dve', qr[:], qs, C.cos128, C.sin128, i, 4, 64, t1, t2, [raw], [qr])
        rope_tm(P, 'pool', kr[:], ks, C.cos128, C.sin128, i, 4, 64, t3, t4, [raw], [kr])
        CP(P, 'act', vb[:], vs, [raw], [vb])
        ACT(P, sg[:].rearrange("p a b -> p (a b)"), gs, AF.Silu, [raw], [sg])
        TT(P, 'pool', kz[:], kr[:], zeta[:, :].unsqueeze(2).broadcast_to([128, 4, 128]), ALU.mult, [kr, zeta], [kz])
        bank = P.psum[bk[0]]
        bb = bank[:].bitcast(BF16)
        for h in range(4):
            TR(P, bb[:, h * 128:(h + 1) * 128], qr[:, h, :], identb[:], [qr, identb], [bank])
        for h in range(4):
            TR(P, bb[:, (4 + h) * 128:(5 + h) * 128], kr[:, h, :], identb[:], [kr, identb], [bank])
        CP(P, 'act', qkT[:].rearrange("p a b -> p (a b)"), bb[:, :], [bank], [qkT])
        if i > 0:
            TT(P, 'dve', qxT[:], qkT[:, 0:4, :], xi[:], ALU.mult, [qkT, xi], [qxT])
        ib = P.psum[bk[1]]
        for h in range(4):
            MM(P, ib[:, h * 128:(h + 1) * 128], qkT[:, 4 + h, :], qkT[:, h, :], True, True, [qkT], [ib])
        TT(P, 'dve', inT[:].rearrange("p a b -> p (a b)"), ib[:], dm[:].rearrange("p a b -> p (a b)"), ALU.mult, [ib, dm], [inT])
        ob = P.psum[bk[2]]
        for h in range(4):
            MM(P, ob[:, h * 128:(h + 1) * 128], inT[:, h, :], vb[:, h, :], True, i == 0, [inT, vb], [ob])
            if i > 0:
                MM(P, ob[:, h * 128:(h + 1) * 128], qxT[:, h, :], Rb[:, h, :], False, True, [qxT, Rb], [ob])
        kvb = P.psum[bk[3]]
        for h in range(4):
            MM(P, kvb[:, h * 128:(h + 1) * 128], kz[:, h, :], vb[:, h, :], True, True, [kz, vb], [kvb])
        for h in range(4):
            if i == 0:
                CP(P, 'dve', R[:, h, :], kvb[:, h * 128:(h + 1) * 128], [kvb], [R])
            else:
                STT(P, R[:, h, :], R[:, h, :], float(g128[h]), kvb[:, h * 128:(h + 1) * 128], ALU.mult, ALU.add, [R, kvb], [R])
        CP(P, 'act', Rb[:], R[:], [R], [Rb])
        o3 = ob[:].rearrange("p (h e) -> p h e", h=4)
        P.op('dve', lambda e, o3=o3: e.tensor_reduce(out=st[:, 0:4], in_=o3, axis=AX.X, op=ALU.add), [ob], [st])
        TS(P, 'dve', st[:, 0:4], st[:, 0:4], 1.0 / 128, None, ALU.mult, None, [st], [st])
        TT(P, 'dve', oc[:], o3, st[:, 0:4].unsqueeze(2).broadcast_to([128, 4, 128]), ALU.subtract, [ob, st], [oc])
        TT(P, 'pool', sq[:], oc[:], oc[:], ALU.mult, [oc], [sq])
        P.op('dve', lambda e: e.tensor_reduce(out=st[:, 4:8], in_=sq[:], axis=AX.X, op=ALU.add), [sq], [st])
        ACT(P, st[:, 8:12], st[:, 4:8], AF.Sqrt, [st, C.epsT], [st], scale=1.0 / 128, bias=C.epsT[:, 0:1])
        RECIP(P, st[:, 12:16], st[:, 8:12], [st], [st])
        TT(P, 'dve', oc[:], oc[:], st[:, 12:16].unsqueeze(2).broadcast_to([128, 4, 128]), ALU.mult, [oc, st], [oc])
        TT(P, 'pool', oc[:], oc[:], sg[:], ALU.mult, [oc, sg], [oc])
        yb = P.psum[bk[0]]
        for h in range(4):
            TR(P, yb[:, h * 128:(h + 1) * 128], oc[:, h, :], C.ident[:], [oc, C.ident], [yb])
        ys = ystg[0]
        CP(P, 'act', ys[:].rearrange("p a b -> p (a b)"), yb[:], [yb], [ys])
        P.dma('pool', C.mixedT.ap[512:1024, i * 128:(i + 1) * 128].rearrange("(h e) t -> e h t", h=4), ys[:], reads=[ys])
        yield


def phase_nsa(P, C, l):
    m0 = P.mark()
    scale = 128 ** -0.5
    identb = P.sb([128, 128], BF16, 'identb')
    CP(P, 'dve', identb[:], C.ident[:], [C.ident], [identb])
    ld = P.sb([128, 128], F32, 'ld')
    tri = P.sb([128, 128], BF16, 'tri')
    P.dma('sp', ld[:], C.c_tri.ap[:, :], writes=[ld])
    CP(P, 'dve', tri[:], ld[:], [ld], [tri])
    trigt = P.sb([128, 128], BF16, 'trigt')
    ld2 = P.sb([128, 128], F32, 'ld2')
    P.dma('sp', ld2[:], C.c_trigt.ap[:, :], writes=[ld2])
    CP(P, 'dve', trigt[:], ld2[:], [ld2], [trigt])
    ovl = P.sb([128, 34], BF16, 'ovl')
    ld3 = P.sb([128, 34], F32, 'ld3')
    P.dma('sp', ld3[:], C.c_ovl.ap[:, :], writes=[ld3])
    CP(P, 'dve', ovl[:], ld3[:], [ld3], [ovl])
    eexp = P.sb([32, NT, 128], BF16, 'eexp')
    ld4 = P.sb([32, NT * 128], F32, 'ld4')
    P.dma('sp', ld4[:], C.c_eexp.ap[:, :], writes=[ld4])
    CP(P, 'dve', eexp[:].rearrange("p a b -> p (a b)"), ld4[:], [ld4], [eexp])
    selvalid = P.sb([128, NT, 32], F32, 'selvalid')
    selcb = P.sb([128, NT, 32], F32, 'selcb')
    P.dma('sp', selvalid[:], C.c_selvalid.ap.rearrange("(i p) m -> p i m", p=128), writes=[selvalid])
    P.dma('sp', selcb[:], C.c_selcb.ap.rearrange("(i p) m -> p i m", p=128), writes=[selcb])
    qT = P.sb([128, NT, 4, 128], BF16, 'qT')
    kvT = P.sb([128, 4, S], BF16, 'kvT')
    vsb = P.sb([128, NT, 130], BF16, 'vsb')
    vwb = P.sb([128, NT, 130], BF16, 'vwb')
    gates = P.sb([128, NT, 12], F32, 'gates')
    P.op('pool', lambda e: e.memset(vsb[:, :, 128:130], 1.0), [], [vsb])
    P.op('pool', lambda e: e.memset(vwb[:, :, 128:130], 1.0), [], [vwb])
    m1 = P.mark()
    raws = [P.sb([128, 1292], F32, 'raw') for _ in range(2)]
    sq = P.sb([128, 4, 128], F32, 'sq')
    st = P.sb([128, 8], F32, 'st')
    qn = P.sb([128, 4, 128], F32, 'qn')
    t1 = P.sb([128, 4, 64], F32, 't1')
    t2 = P.sb([128, 4, 64], F32, 't2')
    sq3 = P.sb([128, 3, 128], F32, 'sq3')
    st3 = P.sb([128, 8], F32, 'st3')
    kn = P.sb([128, 3, 128], F32, 'kn')
    t3 = P.sb([128, 3, 64], F32, 't3')
    t4 = P.sb([128, 3, 64], F32, 't4')
    qk = [P.sb([128, 8, 128], BF16, 'qk') for _ in range(2)]
    c0 = OFF['nq']
    for i in range(NT):
        raw = raws[i % 2]
        P.dma('sp', raw[:], C.proj.ap[i * 128:(i + 1) * 128, c0:c0 + 1292], writes=[raw])
        q_ = qk[i % 2]
        qs = raw[:, 0:512].rearrange("p (h d) -> p h d", h=4)
        rms_scale_tm(P, C, qs, 4, 128, sq, st, scale, [raw])
        TT(P, 'dve', qn[:], qs, st[:, 0:4].unsqueeze(2).broadcast_to([128, 4, 128]), ALU.mult, [raw, st], [qn])
        rope_tm(P, 'dve', q_[:, 0:4, :], qn[:], C.cos128, C.sin128, i, 4, 64, t1, t2, [qn], [q_])
        kv6 = raw[:, 512:1280].rearrange("p (a b d) -> p a b d", a=3, b=2)
        k3 = kv6[:, :, 0, :]
        v3 = kv6[:, :, 1, :]
        rms_scale_tm(P, C, k3, 3, 128, sq3, st3, 1.0, [raw])
        P.op('dve', lambda e: e.memset(st3[:, 0:1], 1.0), [], [st3])
        TT(P, 'pool', kn[:], k3, st3[:, 0:3].unsqueeze(2).broadcast_to([128, 3, 128]), ALU.mult, [raw, st3], [kn])
        rope_tm(P, 'pool', q_[:, 4:7, :], kn[:], C.cos128, C.sin128, i, 3, 64, t3, t4, [kn], [q_])
        CP(P, 'act', q_[:, 7, :], v3[:, 0, :], [raw], [q_])
        CP(P, 'act', vsb[:, i, 0:128], v3[:, 1, :], [raw], [vsb])
        CP(P, 'act', vwb[:, i, 0:128], v3[:, 2, :], [raw], [vwb])
        ACT(P, gates[:, i, :], raw[:, 1280:1292], AF.Sigmoid, [raw], [gates])
        bank = P.ps([2, 3])
        bb = bank[:].bitcast(BF16)
        for a in range(8):
            TR(P, bb[:, a * 128:(a + 1) * 128], q_[:, a, :], identb[:], [q_, identb], [bank])
        CP(P, 'act', qT[:, i, :, :].rearrange("p h t -> p (h t)"), bb[:, 0:512], [bank], [qT])
        CP(P, 'dve', kvT[:, :, i * 128:(i + 1) * 128], bb[:, 512:1024].rearrange("p (a t) -> p a t", a=4), [bank], [kvT])
    P.release(m1)
    m2 = P.mark()
    wk = P.sb([128, 32, 128], BF16, 'wk')
    wv = P.sb([128, 32, 128], BF16, 'wv')
    P.dma('pool', wk[:], C.nsa_w_cmp_k.ap[l].rearrange("(a p) o -> p a o", p=128), writes=[wk])
    P.dma('pool', wv[:], C.nsa_w_cmp_v.ap[l].rearrange("(a p) o -> p a o", p=128), writes=[wv])
    pe2 = P.sb([128, 2, 128], F32, 'pe2')
    P.op('dve', lambda e: e.memset(pe2[:], 0.0), [], [pe2])
    P.dma('sp', pe2[0:32, 0, :], C.nsa_pe_k.ap[l], reads=[pe2], writes=[pe2])
    P.dma('sp', pe2[0:32, 1, :], C.nsa_pe_v.ap[l], reads=[pe2], writes=[pe2])
    peT = P.sb([128, 2, 32], BF16, 'peT')
    bank = P.ps([2, 3])
    for a in range(2):
        TR(P, bank[:, a * 128:(a + 1) * 128], pe2[:, a, :], C.ident[:], [pe2, C.ident], [bank])
    CP(P, 'dve', peT[:], bank[:, 0:256].rearrange("p (a b) -> p a b", a=2)[:, :, 0:32], [bank], [peT])
    onesb = P.sb([1, 128], BF16, 'onesb')
    P.op('dve', lambda e: e.memset(onesb[:], 1.0), [], [onesb])
    cvec = P.sb([1, 2, 128], BF16, 'cvec')
    kcmpT = P.sb([128, 128], BF16, 'kcmpT')
    vcmp = P.sb([128, 128], BF16, 'vcmp')
    kcn = P.sb([128, 128], BF16, 'kcn')
    junk = P.sb([128, 128], F32, 'junk')
    stc = P.sb([128, 4], F32, 'stc')
    for a, (wt, src_idx) in enumerate(((wk, 0), (wv, 3))):
        cb_ = P.ps([2, 3])
        for li in range(32):
            MM(P, cb_[0:1, 0:128], peT[:, a, li:li + 1], wt[:, li, :], li == 0, li == 31, [peT, wt], [cb_])
        CP(P, 'dve', cvec[:, a, :], cb_[0:1, 0:128], [cb_], [cvec])
        kb_ = P.ps([2, 3])
        for li in range(32):
            MM(P, kb_[0:127, 0:128], kvT[:, src_idx, li:li + 16 * 126 + 1:16], wt[:, li, :], li == 0, False, [kvT, wt], [kb_])
        MM(P, kb_[0:127, 0:128], onesb[0:1, 0:127], cvec[0:1, a, :], False, True, [onesb, cvec], [kb_])
        if a == 0:
            ACT(P, junk[0:127, :], kb_[0:127, 0:128], AF.Square, [kb_], [junk, stc], accum_out=stc[0:127, 0:1])
            ACT(P, stc[0:127, 1:2], stc[0:127, 0:1], AF.Sqrt, [stc, C.epsT], [stc], scale=1.0 / 128, bias=C.epsT[0:127, 0:1])
            RECIP(P, stc[0:127, 2:3], stc[0:127, 1:2], [stc], [stc])
            P.op('dve', lambda e: e.memset(kcn[:], 0.0), [], [kcn])
            ACT(P, kcn[0:127, :], kb_[0:127, 0:128], AF.Copy, [kb_, stc, kcn], [kcn], scale=stc[0:127, 2:3])
            tb = P.ps([2, 3])
            tbb = tb[:].bitcast(BF16)
            TR(P, tbb[:, 0:128], kcn[:], identb[:], [kcn, identb], [tb])
            CP(P, 'dve', kcmpT[:], tbb[:, 0:128], [tb], [kcmpT])
        else:
            P.op('dve', lambda e: e.memset(vcmp[:], 0.0), [], [vcmp])
            CP(P, 'act', vcmp[0:127, :], kb_[0:127, 0:128], [kb_, vcmp], [vcmp])
    P.dbg('dbg_kcmpT', kcmpT, kcmpT[:], [128, 128], BF16)
    P.dbg('dbg_vcmp', vcmp, vcmp[:], [128, 128], BF16)
    P.dbg('dbg_kcT', kvT, kvT[:, 0, :], [128, S], BF16)
    cmask = [P.sb([128, 128], F32, 'cmask') for _ in range(2)]
    PT = [P.sb([128, 4, 128], BF16, 'PT') for _ in range(3)]
    msk = [P.sb([128, 128], BF16, 'msk') for _ in range(2)]
    acc = [P.sb([128, 4, 128], F32, 'acc') for _ in range(2)]
    zi = P.sb([128, 4, 34], F32, 'zi')
    wrk = P.sb([128, 4, 32], F32, 'wrk')
    imp = P.sb([128, 32], F32, 'imp')
    top8 = P.sb([128, 8], F32, 'top8')
    selm = P.sb([128, 32], BF16, 'selm')
    selT = P.sb([32, 128], BF16, 'selT')
    cf = P.sb([128, 3, 4], F32, 'cf')
    rz = P.sb([128, 3, 4], F32, 'rz')
    ostg = [P.sb([128, 4, 128], F32R, 'ostg') for _ in range(2)]
    pti = 0
    for i in range(NT):
        qTi = qT[:, i, :, :].rearrange("p h t -> p (h t)")
        ac = acc[i % 2]
        cm = cmask[i % 2]
        P.dma('sp', cm[0:127, :], C.c_cmpbase.ap[126 - 8 * i:126 - 8 * i + 127, :], writes=[cm])
        sb_ = P.ps([0, 1])
        MM(P, sb_[0:127, :], kcmpT[:, 0:127], qTi, True, True, [kcmpT, qT], [sb_])
        pt = PT[pti % 3]
        pti += 1
        ACT(P, pt[0:127].rearrange("p h t -> p (h t)"), sb_[0:127, :], AF.Exp, [sb_], [pt])
        TT(P, 'dve', pt[0:127], pt[0:127], cm[0:127, :].unsqueeze(1).broadcast_to([127, 4, 128]), ALU.mult, [pt, cm], [pt])
        ob = P.ps([2, 3])
        zb = P.ps([2, 3])
        for h in range(4):
            MM(P, ob[:, h * 128:(h + 1) * 128], pt[0:127, h, :], vcmp[0:127, :], True, True, [pt, vcmp], [ob])
        for h in range(4):
            MM(P, zb[:, h * 34:h * 34 + 34], pt[0:127, h, :], ovl[0:127, :], True, True, [pt, ovl], [zb])
        CP(P, 'dve', zi[:].rearrange("p a b -> p (a b)"), zb[:, 0:136], [zb], [zi])
        TS(P, 'dve', rz[:, 0, :], zi[:, :, 0], 1e-30, None, ALU.max, None, [zi], [rz])
        RECIP(P, rz[:, 0, :], rz[:, 0, :], [rz], [rz])
        TT(P, 'dve', wrk[:], zi[:, :, 1:33], rz[:, 0, :].unsqueeze(2).broadcast_to([128, 4, 32]), ALU.mult, [zi, rz], [wrk])
        P.op('dve', lambda e: e.tensor_reduce(out=imp[:], in_=wrk[:].rearrange("p h m -> p m h"), axis=AX.X, op=ALU.add), [wrk], [imp])
        TT(P, 'dve', cf[:, 0, :], gates[:, i, 0:12:3], rz[:, 0, :], ALU.mult, [gates, rz], [cf])
        for h in range(4):
            ACT(P, ac[:, h, :], ob[:, h * 128:(h + 1) * 128], AF.Copy, [ob, cf], [ac], scale=cf[:, 0, h:h + 1])
        TT(P, 'dve', imp[:], imp[:], selvalid[:, i, :], ALU.mult, [imp, selvalid], [imp])
        TT(P, 'dve', imp[:], imp[:], selcb[:, i, :], ALU.add, [imp, selcb], [imp])
        if i == 13:
            P.dbg('dbg_imp', imp, imp[:], [128, 32])
        P.op('dve', lambda e: e.max(out=top8[:], in_=imp[:]), [imp], [top8])
        TS(P, 'dve', selm[:], imp[:], top8[:, 7:8], None, ALU.is_ge, None, [imp, top8], [selm])
        tb = P.ps([2, 3])
        tbb = tb[:].bitcast(BF16)
        TR(P, tbb[0:32, 0:128], selm[:], identb[:], [selm, identb], [tb])
        CP(P, 'act', selT[:], tbb[0:32, 0:128], [tb], [selT])
        if i == 13:
            P.dbg('dbg_selm', selm, selm[:], [128, 32], BF16)
            P.dbg('dbg_top8', top8, top8[:], [128, 8])
        if i == 13:
            P.dbg('dbg_zi', zi, zi[:], [128, 4, 34])
            P.dbg('dbg_cf', cf, cf[:, 0, :], [128, 4])
            P.dbg('dbg_ac0', ac, ac[:], [128, 4, 128])
            P.dbg('dbg_pt', pt, pt[0:127], [127, 4, 128], BF16)
            P.dbg('dbg_cm', cm, cm[0:127, :], [127, 128])
        for br in (2, 1):
            kbs = [kb for kb in (i - 2, i - 1, i) if kb >= 0] if br == 2 else list(range(i + 1))
            A_, B_ = (P.psum[4], P.psum[5]) if br == 2 else (P.psum[6], P.psum[7])
            vt = vwb if br == 2 else vsb
            for n_, kb in enumerate(kbs):
                sb_ = P.ps([0, 1])
                MM(P, sb_[:, :], kvT[:, br, kb * 128:(kb + 1) * 128], qTi, True, True, [kvT, qT], [sb_])
                pt = PT[pti % 3]
                pti += 1
                ACT(P, pt[:].rearrange("p h t -> p (h t)"), sb_[:, :], AF.Exp, [sb_], [pt])
                if br == 2:
                    if kb == i:
                        TT(P, 'pool', pt[:], pt[:], tri[:].unsqueeze(1).broadcast_to([128, 4, 128]), ALU.mult, [pt, tri], [pt])
                    elif kb == i - 2:
                        TT(P, 'pool', pt[:], pt[:], trigt[:].unsqueeze(1).broadcast_to([128, 4, 128]), ALU.mult, [pt, trigt], [pt])
                else:
                    mb = P.ps([2, 3])
                    MM(P, mb[:, 0:128], eexp[:, kb, :], selT[:, :], True, True, [eexp, selT], [mb])
                    if kb == i:
                        mk = msk[n_ % 2]
                        TT(P, 'dve', mk[:], mb[:, 0:128], tri[:], ALU.mult, [mb, tri], [mk])
                        TT(P, 'dve', pt[:], pt[:], mk[:].unsqueeze(1).broadcast_to([128, 4, 128]), ALU.mult, [pt, mk], [pt])
                    else:
                        TT(P, 'dve', pt[:], pt[:], mb[:, 0:128].unsqueeze(1).broadcast_to([128, 4, 128]), ALU.mult, [pt, mb], [pt])
                for h in range(4):
                    bk = A_ if h < 2 else B_
                    o0 = (h % 2) * 130
                    MM(P, bk[:, o0:o0 + 129], pt[:, h, :], vt[:, kb, 0:129], (n_ == 0 and h % 2 == 0), n_ == len(kbs) - 1,
                       [pt, vt], [bk], skip_group_check=True)
            for h in range(4):
                bk = A_ if h < 2 else B_
                o0 = (h % 2) * 130
                RECIP(P, rz[:, br, h:h + 1], bk[:, o0 + 128:o0 + 129], [bk], [rz])
            TT(P, 'dve', cf[:, br, :], gates[:, i, br:12:3], rz[:, br, :], ALU.mult, [gates, rz], [cf])
            for h in range(4):
                bk = A_ if h < 2 else B_
                o0 = (h % 2) * 130
                STT(P, ac[:, h, :], bk[:, o0:o0 + 128], cf[:, br, h:h + 1], ac[:, h, :], ALU.mult, ALU.add, [bk, cf, ac], [ac])
        yb = P.ps([2, 3])
        for h in range(4):
            TR(P, yb[:, h * 128:(h + 1) * 128], ac[:, h, :], C.ident[:], [ac, C.ident], [yb])
        og = ostg[i % 2]
        CP(P, 'act', og[:].rearrange("p a b -> p (a b)"), yb[:], [yb], [og])
        P.dma('pool', C.mixedT.ap[1024:1536, i * 128:(i + 1) * 128].rearrange("(h e) t -> e h t", h=4), og[:], reads=[og])
    P.barrier()
    P.release(m0)


def phase_s5(P, C, l, banks=None):
    Lc = S5L
    NCH = S // Lc
    cosE = P.sb([128, 16, Lc], F32, 'cosE')
    sinE = P.sb([128, 16, Lc], F32, 'sinE')
    Bc = P.sb([128, 16, 2, 128], F32R, 'Bc')
    Cx = P.sb([128, 16, 2, 128], F32R, 'Cx')
    Dg = P.sb([128, 4, 128], F32R, 'Dg')
    wg = P.sb([128, 4, 512], F32R, 'wg')
    prm = P.sb([128, 16, 16], F32, 'prm')
    P.dma('pool', wg[:], C.s5_w_glu.ap[l].rearrange("(m p) c -> p m c", p=128), writes=[wg])
    LR, LI, LD, DT, A_, TH, R_, CT, ST, FR, FI, CL, SL, DEN, T0, T1 = range(16)
    m1 = P.mark()
    pad = P.sb([128, 3, 128], F32, 'pad')
    P.op('dve', lambda e: e.memset(pad[:], 0.0), [], [pad])
    P.dma('sp', pad[0:16, 0, :], C.s5_lambda_re.ap[l].rearrange("(j g) p -> j (g p)", g=2), reads=[pad], writes=[pad])
    P.dma('sp', pad[0:16, 1, :], C.s5_lambda_im.ap[l].rearrange("(j g) p -> j (g p)", g=2), reads=[pad], writes=[pad])
    ldt = P.sb([16, 2], F32, 'ldt')
    P.dma('sp', ldt[:], C.s5_log_dt.ap[l].rearrange("(j g) -> j g", g=2), writes=[ldt])
    CP(P, 'dve', pad[0:16, 2, :].rearrange("j (g p) -> j g p", g=2), ldt[:].unsqueeze(2).broadcast_to([16, 2, 64]), [ldt, pad], [pad])
    bank = P.ps(banks)
    for k in range(3):
        TR(P, bank[:, k * 128:(k + 1) * 128], pad[:, k, :], C.ident[:], [pad, C.ident], [bank])
    CP(P, 'dve', prm[:, 0:3, :], bank[:, 0:384].rearrange("p (k c) -> p k c", k=3)[:, :, 0:16], [bank], [prm])

    def pv(k):
        return prm[:, k, :]
    ACT(P, pv(DT), pv(LD), AF.Exp, [prm], [prm])
    TT(P, 'dve', pv(A_), pv(LR), pv(DT), ALU.mult, [prm], [prm])
    TT(P, 'dve', pv(TH), pv(LI), pv(DT), ALU.mult, [prm], [prm])
    ACT(P, pv(R_), pv(A_), AF.Exp, [prm], [prm])
    angs = P.sb([128, 2, 16], F32, 'angs')
    tqs = P.sb([128, 2, 16], F32, 'tqs')
    tis = P.sb([128, 2, 16], I32, 'tis')
    sins = P.sb([128, 2, 16], F32, 'sins')
    coss = P.sb([128, 2, 16], F32, 'coss')
    CP(P, 'dve', angs[:, 0, :], pv(TH), [prm], [angs])
    TS(P, 'dve', angs[:, 1, :], pv(TH), float(Lc), None, ALU.mult, None, [prm], [angs])
    range_reduce_sincos(P, angs, tqs, tis, sins, coss, None)
    CP(P, 'dve', pv(ST), sins[:, 0, :], [sins, coss], [prm])
    CP(P, 'dve', pv(SL), sins[:, 1, :], [sins, coss], [prm])
    CP(P, 'dve', pv(CT), coss[:, 0, :], [sins, coss], [prm])
    CP(P, 'dve', pv(CL), coss[:, 1, :], [sins, coss], [prm])
    TT(P, 'dve', pv(T0), pv(R_), pv(CT), ALU.mult, [prm], [prm])
    TT(P, 'dve', pv(T1), pv(R_), pv(ST), ALU.mult, [prm], [prm])
    TS(P, 'dve', pv(T0), pv(T0), -1.0, None, ALU.add, None, [prm], [prm])
    TT(P, 'dve', pv(DEN), pv(LR), pv(LR), ALU.mult, [prm], [prm])
    TT(P, 'dve', pv(FR), pv(LI), pv(LI), ALU.mult, [prm], [prm])
    TT(P, 'dve', pv(DEN), pv(DEN), pv(FR), ALU.add, [prm], [prm])
    RECIP(P, pv(DEN), pv(DEN), [prm], [prm])
    TT(P, 'dve', pv(FR), pv(T0), pv(LR), ALU.mult, [prm], [prm])
    TT(P, 'dve', pv(FI), pv(T1), pv(LI), ALU.mult, [prm], [prm])
    TT(P, 'dve', pv(FR), pv(FR), pv(FI), ALU.add, [prm], [prm])
    TT(P, 'dve', pv(FR), pv(FR), pv(DEN), ALU.mult, [prm], [prm])
    TT(P, 'dve', pv(FI), pv(T1), pv(LR), ALU.mult, [prm], [prm])
    TT(P, 'dve', pv(T1), pv(T0), pv(LI), ALU.mult, [prm], [prm])
    TT(P, 'dve', pv(FI), pv(FI), pv(T1), ALU.subtract, [prm], [prm])
    TT(P, 'dve', pv(FI), pv(FI), pv(DEN), ALU.mult, [prm], [prm])
    iota = P.sb([128, Lc], F32, 'iota')
    P.dma('sp', iota[:], C.c_iota.ap[:, :], writes=[iota])
    ang = P.sb([128, 16, Lc], F32, 'ang')
    tq = P.sb([128, 16, Lc], F32, 'tq')
    ti = P.sb([128, 16, Lc], I32, 'ti')
    for j in range(16):
        TS(P, 'dve' if j % 2 == 0 else 'pool', ang[:, j, :], iota[:], prm[:, TH, j:j + 1], None, ALU.mult, None, [iota, prm], [ang])
    range_reduce_sincos(P, ang, tq, ti, sinE, cosE, None)
    P.release(m1)
    m1 = P.mark()
    braw = P.sb([128, 2, 16, 16], F32, 'braw')
    P.dma('sp', braw[:, 0, :, :], C.s5_b_re.ap[l].rearrange("(j g) p h -> (g p) j h", g=2), writes=[braw])
    P.dma('sp', braw[:, 1, :, :], C.s5_b_im.ap[l].rearrange("(j g) p h -> (g p) j h", g=2), writes=[braw])
    bb = P.sb([128, 2, 16, 16], F32, 'bb')
    tb1 = P.sb([128, 16, 16], F32, 'tb1')
    tb2 = P.sb([128, 16, 16], F32, 'tb2')
    frb = prm[:, FR, :].unsqueeze(2).broadcast_to([128, 16, 16])
    fib = prm[:, FI, :].unsqueeze(2).broadcast_to([128, 16, 16])
    TT(P, 'dve', tb1[:], braw[:, 0], frb, ALU.mult, [braw, prm], [tb1])
    TT(P, 'dve', tb2[:], braw[:, 1], fib, ALU.mult, [braw, prm], [tb2])
    TT(P, 'dve', bb[:, 0], tb1[:], tb2[:], ALU.subtract, [tb1, tb2], [bb])
    TT(P, 'dve', tb1[:], braw[:, 1], frb, ALU.mult, [braw, prm], [tb1])
    TT(P, 'dve', tb2[:], braw[:, 0], fib, ALU.mult, [braw, prm], [tb2])
    TT(P, 'dve', bb[:, 1], tb1[:], tb2[:], ALU.add, [tb1, tb2], [bb])
    X = P.sb([128, 16, 2, 128], F32, 'X')
    P.op('pool', lambda e: e.memset(X[:], 0.0), [], [X])
    for g2 in range(2):
        for jj in range(4):
            for c in range(2):
                col = 32 * jj + 16 * g2
                CP(P, 'dve', X[g2 * 64:(g2 + 1) * 64, jj:16:4, c, col:col + 16], bb[g2 * 64:(g2 + 1) * 64, c, jj:16:4, :], [bb, X], [X])
    for j in range(16):
        bank = P.ps(banks)
        for c in range(2):
            TR(P, bank[:, c * 128:(c + 1) * 128], X[:, j, c, :], C.ident[:], [X, C.ident], [bank])
        CP(P, 'act' if j % 2 == 0 else 'dve', Bc[:, j, :, :].rearrange("p c k -> p (c k)"), bank[:, 0:256], [bank], [Bc])
    craw = P.sb([128, 4, 2, 128], F32, 'craw')
    for c, src in enumerate((C.s5_c_re, C.s5_c_im)):
        for dup in range(2):
            P.dma('sp', craw[:, :, c, dup * 64:(dup + 1) * 64], src.ap[l].rearrange("(m gl) n p -> (gl n) m p", gl=8), writes=[craw])
    P.op('pool', lambda e: e.memset(Cx[:].bitcast(F32), 0.0), [], [Cx])
    ctr = P.sb([128, 128], F32, 'ctr')
    for m in range(4):
        for c in range(2):
            bank = P.ps(banks)
            TR(P, bank[:, 0:128], craw[:, m, c, :], C.ident[:], [craw, C.ident], [bank])
            if c == 0:
                CP(P, 'act', ctr[:], bank[:, 0:128], [bank], [ctr])
            else:
                ACT(P, ctr[:], bank[:, 0:128], AF.Copy, [bank], [ctr], scale=-1.0)
            for gl in range(8):
                g2 = gl % 2
                j = 4 * m + gl // 2
                CP(P, 'dve', Cx[g2 * 64:(g2 + 1) * 64, j, c, 16 * gl:16 * gl + 16], ctr[g2 * 64:(g2 + 1) * 64, 16 * gl:16 * gl + 16], [ctr, Cx], [Cx])
    dcol = P.sb([128, 4], F32, 'dcol')
    P.dma('sp', dcol[:], C.s5_d.ap[l].rearrange("(m gl) n -> (gl n) m", gl=8), writes=[dcol], allow_slow_non_contiguous=True)
    for m in range(4):
        TS(P, 'dve', Dg[:, m, :], C.ident[:], dcol[:, m:m + 1], None, ALU.mult, None, [C.ident, dcol], [Dg])
    P.release(m1)
    uraw = [P.sb([128, 512], F32, 'uraw') for _ in range(2)]
    uT = [P.sb([128, 4, Lc], F32R, 'uT') for _ in range(1)]
    sre = P.sb([128, 16, Lc], F32R, 'sre')
    sim_ = P.sb([128, 16, Lc], F32R, 'sim')
    wk_ = [[P.sb([128, Lc], F32, 'wk') for _ in range(8)] for _ in range(4)]
    bsb = [P.sb([128, 2 * Lc], F32, 'bsb') for _ in range(1)]
    zl = P.sb([128, 2, 16], F32, 'zl')
    zin = P.sb([128, 2, 16], F32, 'zin')
    ztmp = P.sb([128, 2, 16], F32, 'ztmp')
    gT = [P.sb([128, 4, Lc], F32R, 'gT') for _ in range(1)]
    ge = [P.sb([128, Lc], F32, 'ge') for _ in range(3)]
    oT = [P.sb([128, Lc], F32R, 'oT') for _ in range(1)]
    c0 = OFF['u']
    ui = 0
    yield 'main'
    for ch in range(NCH):
        t0 = ch * Lc
        u_T = uT[0]
        for tt in range(Lc // 128):
            ur = uraw[ui % 2]
            ui += 1
            P.dma('sp', ur[:], C.proj.ap[t0 + tt * 128:t0 + (tt + 1) * 128, c0:c0 + 512], writes=[ur])
            bank = P.ps(banks)
            for m in range(4):
                TR(P, bank[:, m * 128:(m + 1) * 128], ur[:, m * 128:(m + 1) * 128], C.ident[:], [ur, C.ident], [bank])
            CP(P, 'act', u_T[:, :, tt * 128:(tt + 1) * 128], bank[:].rearrange("p (m t) -> p m t", m=4), [bank], [u_T])
        if ch > 0:
            clb, slb = prm[:, CL, :], prm[:, SL, :]
            TT(P, 'dve', ztmp[:, 0, :], zl[:, 0, :], clb, ALU.mult, [zl, prm], [ztmp])
            TT(P, 'dve', ztmp[:, 1, :], zl[:, 1, :], slb, ALU.mult, [zl, prm], [ztmp])
            TT(P, 'dve', zin[:, 0, :], ztmp[:, 0, :], ztmp[:, 1, :], ALU.subtract, [ztmp], [zin])
            TT(P, 'dve', ztmp[:, 0, :], zl[:, 0, :], slb, ALU.mult, [zl, prm], [ztmp])
            TT(P, 'dve', ztmp[:, 1, :], zl[:, 1, :], clb, ALU.mult, [zl, prm], [ztmp])
            TT(P, 'dve', zin[:, 1, :], ztmp[:, 0, :], ztmp[:, 1, :], ALU.add, [ztmp], [zin])
        def st_mm(j):
            bank = P.ps(banks)
            MM(P, bank[:, 0:Lc], Bc[:, j, 0, :], u_T[:, j // 4, :], True, True, [Bc, u_T], [bank])
            MM(P, bank[:, Lc:2 * Lc], Bc[:, j, 1, :], u_T[:, j // 4, :], True, True, [Bc, u_T], [bank])
            return bank

        def st_pre(j, e1, bre, bim, srcs):
            w = wk_[j % 4]
            cj, sj = cosE[:, j, :], sinE[:, j, :]
            TT(P, e1, w[0][:], bre, cj, ALU.mult, srcs + [cosE], [w[0]])
            TT(P, e1, w[1][:], bim, sj, ALU.mult, srcs + [sinE], [w[1]])
            TT(P, e1, w[2][:], w[0][:], w[1][:], ALU.add, [w[0], w[1]], [w[2]])
            TT(P, e1, w[3][:], bim, cj, ALU.mult, srcs + [cosE], [w[3]])
            TT(P, e1, w[4][:], bre, sj, ALU.mult, srcs + [sinE], [w[4]])
            TT(P, e1, w[5][:], w[3][:], w[4][:], ALU.subtract, [w[3], w[4]], [w[5]])

        def st_scan(j):
            w = wk_[j % 4]
            rb = prm[:, R_, j:j + 1].broadcast_to([128, Lc])
            for c_, (src, dst) in enumerate(((w[2], w[6]), (w[5], w[7]))):
                init = 0.0 if ch == 0 else zin[:, c_, j:j + 1]
                P.op('dve', lambda e, src=src, dst=dst, init=init, rb=rb: e.tensor_tensor_scan(
                    out=dst[:], data0=rb, data1=src[:], initial=init, op0=ALU.mult, op1=ALU.add),
                    [src, prm, zin], [dst])
                CP(P, 'act', zl[:, c_, j:j + 1], dst[:, Lc - 1:Lc], [dst], [zl])

        def st_post(j, e1):
            w = wk_[j % 4]
            cj, sj = cosE[:, j, :], sinE[:, j, :]
            zr, zi_ = w[6], w[7]
            TT(P, e1, w[0][:], zr[:], cj, ALU.mult, [zr, cosE], [w[0]])
            TT(P, e1, w[1][:], zi_[:], sj, ALU.mult, [zi_, sinE], [w[1]])
            TT(P, e1, w[3][:], zr[:], sj, ALU.mult, [zr, sinE], [w[3]])
            TT(P, e1, w[4][:], zi_[:], cj, ALU.mult, [zi_, cosE], [w[4]])

        def st_fin(j):
            w = wk_[j % 4]
            TT(P, 'dve', sre[:, j, :], w[0][:], w[1][:], ALU.subtract, [w[0], w[1]], [sre])
            TT(P, 'dve', sim_[:, j, :], w[3][:], w[4][:], ALU.add, [w[3], w[4]], [sim_])

        deferred = None
        for p_ in range(8):
            jo, je = 2 * p_ + 1, 2 * p_
            bko = st_mm(jo)
            bs = bsb[0]
            CP(P, 'act', bs[:], bko[:], [bko], [bs])
            st_pre(jo, 'pool', bs[:, 0:Lc], bs[:, Lc:2 * Lc], [bs])
            bke = st_mm(je)
            st_pre(je, 'dve', bke[:, 0:Lc], bke[:, Lc:2 * Lc], [bke])
            st_scan(je)
            st_post(je, 'dve')
            st_fin(je)
            if deferred is not None:
                st_fin(deferred)
            st_scan(jo)
            st_post(jo, 'pool')
            deferred = jo
            yield
        st_fin(deferred)
        g_T = gT[0]
        for m in range(4):
            bank = P.ps(banks)
            n_ = 0
            for j in range(4 * m, 4 * m + 4):
                for c_, st_ in enumerate((sre, sim_)):
                    MM(P, bank[:, 0:Lc], Cx[:, j, c_, :], st_[:, j, :], n_ == 0, False, [Cx, st_], [bank])
                    n_ += 1
            MM(P, bank[:, 0:Lc], Dg[:, m, :], u_T[:, m, :], False, True, [Dg, u_T], [bank])
            x_ = bank[:, 0:Lc]
            a, b, c_t = ge[0], ge[1], ge[2]
            ACT(P, a[:], x_, AF.Square, [bank], [a])
            TS(P, 'dve', a[:], a[:], 0.044715, 1.0, ALU.mult, ALU.add, [a], [a])
            TT(P, 'dve', b[:], a[:], x_, ALU.mult, [a, bank], [b])
            ACT(P, c_t[:], b[:], AF.Sigmoid, [b], [c_t], scale=1.5957691216057308)
            TT(P, 'dve', g_T[:, m, :], c_t[:], x_, ALU.mult, [c_t, bank], [g_T])
        for mo in range(4):
            bank = P.ps(banks)
            for m in range(4):
                MM(P, bank[:, 0:Lc], wg[:, m, mo * 128:(mo + 1) * 128], g_T[:, m, :], m == 0, m == 3, [wg, g_T], [bank])
            a = ge[mo % 3]
            ACT(P, a[:], bank[:, 0:Lc], AF.Sigmoid, [bank], [a])
            o_ = oT[0]
            TT(P, 'dve', o_[:], a[:], g_T[:, mo, :].bitcast(F32), ALU.mult, [a, g_T], [o_])
            P.dma('pool', C.mixedT.ap[mo * 128:(mo + 1) * 128, t0:t0 + Lc], o_[:], reads=[o_])
        yield


def run_alone(P, g):
    m0 = P.mark()
    for _ in g:
        pass
    P.release(m0)


def run_pair(P, gA, gB, ratio):
    m0 = P.mark()
    for g in (gA, gB):
        for v in g:
            if v == 'main':
                break
    live = {0: gA, 1: gB}
    while live:
        for k in (0, 1):
            if k not in live:
                continue
            for _ in range(ratio[k]):
                try:
                    next(live[k])
                except StopIteration:
                    del live[k]
                    break
    P.release(m0)


_CACHE = {}


def build_full(nl=L):
    nc = bass.Bass("TRN2", target_bir_lowering=False)
    P = Prog(nc)
    C = Ctx()
    setup_common(P, C, {})
    setup_rope(P, C)
    xin = C.x.ap
    for l in range(nl):
        phase_in_proj(P, C, l, xin)
        run_pair(P, phase_s5(P, C, l, [0, 1, 2, 3]), phase_ret(P, C, l, [4, 5, 6, 7]), (8, 1))
        phase_nsa(P, C, l)
        phase_diff(P, C, l)
        phase_out_proj(P, C, l, xin, C.xa.ap)
        xout = C.out.ap if l == nl - 1 else C.xb.ap
        phase_mlp(P, C, l, C.xa.ap, xout)
        xin = C.xb.ap
    P.barrier()
    P.finalize()
    return nc


def build_layer(lay):
    nc = bass.Bass("TRN2", target_bir_lowering=False)
    P = Prog(nc)
    C = Ctx()
    C.lbase = lay
    setup_common(P, C, {}, nlw=1)
    setup_rope(P, C)
    phase_in_proj(P, C, 0, C.x.ap)
    run_alone(P, phase_s5(P, C, 0))
    run_alone(P, phase_ret(P, C, 0))
    phase_nsa(P, C, 0)
    phase_diff(P, C, 0)
    phase_out_proj(P, C, 0, C.x.ap, C.xa.ap)
    phase_mlp(P, C, 0, C.xa.ap, C.out.ap)
    P.barrier()
    P.finalize()
    return nc


FUSED = True


def kernel(**inputs):
    x = np.ascontiguousarray(inputs['x'], dtype=np.float32)
    pos = np.ascontiguousarray(inputs['positions'], dtype=np.int32)
    B = x.shape[0]
    consts = host_consts()
    if FUSED:
        if 'nc' not in _CACHE:
            _CACHE['nc'] = build_full()
        nc = _CACHE['nc']
        shared = {name: np.ascontiguousarray(inputs[name], dtype=np.float32) for name, _, _ in WEIGHT_SPECS}
        shared.update(consts)
        in_maps = []
        for b in range(B):
            m = dict(shared)
            m['x'] = x[b]
            m['pos'] = pos[b]
            in_maps.append(m)
        res = run_bass_kernel_spmd(nc, in_maps, core_ids=list(range(B)))
        return np.stack([np.asarray(res.results[b]['out'], dtype=np.float32) for b in range(B)], axis=0)
    cur = [x[b] for b in range(B)]
    CPL = 4
    for lay in range(L):
        nc = build_layer(lay)
        shared = {name: np.ascontiguousarray(inputs[name][lay:lay + 1], dtype=np.float32) for name, _, _ in WEIGHT_SPECS}
        shared.update(consts)
        nxt = []
        for b0 in range(0, B, CPL):
            in_maps = []
            for b in range(b0, min(B, b0 + CPL)):
                m = dict(shared)
                m['x'] = cur[b]
                m['pos'] = pos[b]
                in_maps.append(m)
            res = run_bass_kernel_spmd(nc, in_maps, core_ids=list(range(len(in_maps))))
            nxt += [np.asarray(res.results[i]['out'], dtype=np.float32) for i in range(len(in_maps))]
        cur = nxt
    return np.stack(cur, axis=0)
```
